# Optimizing a Trainium2 kernel written in Bass

```python
import jax, jax.numpy as jnp
from jax import lax
import numpy as np

D_MODEL = 1024
BATCH = 2
SEQ = 16384
DEPTH = 2

GRID_W = 64
CTX_LEN = 256
N_MIXERS = 2
N_EVEN_LAYERS = (DEPTH + 1) // 2
N_ODD_LAYERS = DEPTH // 2
POOL_WINDOWS = (2, 4, 8, 16)
N_POOL_GROUPS = len(POOL_WINDOWS)
POOL_GROUP_DIM = D_MODEL // N_POOL_GROUPS
HEAD_DIM = 64
N_HEADS = D_MODEL // HEAD_DIM
N_KV_HEADS = 4
GROUP = N_HEADS // N_KV_HEADS
Q_DIM = N_HEADS * HEAD_DIM
KV_DIM = N_KV_HEADS * HEAD_DIM
ROPE_THETA = 10000.0
Q_BLOCK = 128
D_FF = 2816
N_EXPERTS = 8
TOP_K = 2
MOE_BLOCK = 256
NORM_EPS = 1e-6
N_MOD = 6

kernel_name = "hybrid_pool_gqa_moe_dit_block"


def rmsnorm(x, g):
    xf = x.astype(jnp.float32)
    y = xf * lax.rsqrt(jnp.mean(xf * xf, axis=-1, keepdims=True) + NORM_EPS)
    return (y * g.astype(jnp.float32)).astype(x.dtype)


def adaln(cvec, w, b):
    m = jax.nn.silu(cvec.astype(jnp.float32)) @ w.astype(jnp.float32) + b.astype(jnp.float32)
    m = m.astype(cvec.dtype)[:, None, :]
    return jnp.split(m, N_MOD, axis=-1)


def modulate(h, shift, scale):
    return h * (1.0 + scale) + shift


def pool_mix(h, w_grp, scale):
    B, L, D = h.shape
    h4 = h.reshape(B, L, N_POOL_GROUPS, POOL_GROUP_DIM)
    cs = jnp.cumsum(h4.astype(jnp.float32), axis=1)
    cs = jnp.concatenate([jnp.zeros_like(cs[:, :1]), cs], axis=1)
    t = jnp.arange(L, dtype=jnp.int32)[:, None]
    half = jnp.asarray(POOL_WINDOWS, dtype=jnp.int32) // 2
    lo = jnp.clip(t - half, 0, L)
    hi = jnp.clip(t + half, 0, L)
    gid = jnp.arange(N_POOL_GROUPS, dtype=jnp.int32)[None, :]
    win_sum = cs[:, hi, gid] - cs[:, lo, gid]
    mean = win_sum / (hi - lo).astype(jnp.float32)[None, :, :, None]
    d = (mean - h4.astype(jnp.float32)).astype(h.dtype)
    y = jnp.einsum('blgc,gce->blge', d, w_grp).reshape(B, L, D)
    return y * scale


def rope_1d(x, pos):
    half = x.shape[-1] // 2
    freqs = ROPE_THETA ** (-jnp.arange(half, dtype=jnp.float32) / half)
    ang = pos.astype(jnp.float32)[:, None] * freqs
    shape = (ang.shape[0],) + (1,) * (x.ndim - 3) + (half,)
    cos = jnp.cos(ang).reshape(shape)
    sin = jnp.sin(ang).reshape(shape)
    x1, x2 = x[..., :half], x[..., half:]
    return jnp.concatenate([x1 * cos - x2 * sin, x2 * cos + x1 * sin], axis=-1).astype(x.dtype)


def rope_2d(x, row, col):
    d = HEAD_DIM // 2
    return jnp.concatenate([rope_1d(x[..., :d], row), rope_1d(x[..., d:], col)], axis=-1)


def heads_q(q_flat, g):
    B, L = q_flat.shape[:2]
    return rmsnorm(q_flat.reshape(B, L, N_KV_HEADS, GROUP, HEAD_DIM), g)


def heads_kv(kv_flat, g):
    B, L = kv_flat.shape[:2]
    kv = kv_flat.reshape(B, L, 2, N_KV_HEADS, HEAD_DIM)
    return rmsnorm(kv[:, :, 0], g), kv[:, :, 1]


def attend(q, k, v):
    B, Lq = q.shape[:2]
    nb = Lq // Q_BLOCK
    qb = q.reshape(B, nb, Q_BLOCK, N_KV_HEADS, GROUP, HEAD_DIM).swapaxes(0, 1)
    sm_scale = HEAD_DIM ** -0.5

    def one_block(qblk):
        s = jnp.einsum('bqkgd,bnkd->bkgqn', qblk, k, preferred_element_type=jnp.float32) * sm_scale
        p = jax.nn.softmax(s, axis=-1)
        return jnp.einsum('bkgqn,bnkd->bqkgd', p.astype(v.dtype), v)

    o = lax.map(one_block, qb)
    return o.swapaxes(0, 1).reshape(B, Lq, Q_DIM)


def swiglu(h, w1, w3, w2):
    return (jax.nn.silu(h @ w1) * (h @ w3)) @ w2


def moe_swiglu(h, router_w, w1, w3, w2):
    B, L, D = h.shape
    T = B * L
    hf = h.reshape(T, D)
    logits = hf.astype(jnp.float32) @ router_w.astype(jnp.float32)
    top_logit, top_e = lax.top_k(logits, TOP_K)
    gates = jax.nn.softmax(top_logit, axis=-1)
    n_assign = T * TOP_K
    flat_e = top_e.reshape(-1)
    flat_tok = jnp.arange(n_assign, dtype=jnp.int32) // TOP_K
    flat_g = gates.reshape(-1)
    order = jnp.argsort(flat_e)
    e_sorted = flat_e[order]
    sizes = jnp.bincount(flat_e, length=N_EXPERTS)
    padded = (sizes + MOE_BLOCK - 1) // MOE_BLOCK * MOE_BLOCK
    start = jnp.cumsum(sizes) - sizes
    ends = jnp.cumsum(padded)
    pstart = ends - padded
    dest = pstart[e_sorted] + (jnp.arange(n_assign, dtype=jnp.int32) - start[e_sorted])
    cap = n_assign + N_EXPERTS * MOE_BLOCK
    n_blk = cap // MOE_BLOCK
    buf_tok = jnp.zeros((cap,), jnp.int32).at[dest].set(flat_tok[order])
    buf_gate = jnp.zeros((cap,), jnp.float32).at[dest].set(flat_g[order])
    blk_start = jnp.arange(n_blk, dtype=jnp.int32) * MOE_BLOCK
    blk_e = jnp.clip(jnp.searchsorted(ends, blk_start, side='right'), 0, N_EXPERTS - 1)

    def expert_block(args):
        tok, g, e = args
        xb = hf[tok]
        return swiglu(xb, w1[e], w3[e], w2[e]) * g[:, None].astype(xb.dtype)

    y = lax.map(expert_block, (buf_tok.reshape(n_blk, MOE_BLOCK),
                               buf_gate.reshape(n_blk, MOE_BLOCK), blk_e))
    out = jnp.zeros_like(hf).at[buf_tok].add(y.reshape(cap, D))
    return out.reshape(B, L, D)


def setup_inputs(seed: int = 0) -> dict:
    key = jax.random.key(seed)
    ks = jax.random.split(key, 21)

    def nrm(k, shape, s):
        return jax.random.normal(k, shape, jnp.float32) * s

    D = D_MODEL
    return {
        "x": nrm(ks[0], (BATCH, SEQ, D), 1.0),
        "c": nrm(ks[1], (BATCH, D), 1.0),
        "ctx": nrm(ks[2], (BATCH, CTX_LEN, D), 1.0),
        "c_ctx": nrm(ks[3], (D,), 1.0),
        "ada_w": nrm(ks[4], (DEPTH, D, N_MOD * D), 0.5 * D ** -0.5),
        "ada_b": nrm(ks[5], (DEPTH, N_MOD * D), 0.02),
        "norm_g": 1.0 + nrm(ks[6], (DEPTH, 2, D), 0.1),
        "pool_w": nrm(ks[7], (N_EVEN_LAYERS, N_POOL_GROUPS, POOL_GROUP_DIM, POOL_GROUP_DIM), POOL_GROUP_DIM ** -0.5),
        "pool_scale": 1.0 + nrm(ks[8], (N_EVEN_LAYERS, D), 0.1),
        "ffn_w1": nrm(ks[9], (N_EVEN_LAYERS, D, D_FF), D ** -0.5),
        "ffn_w3": nrm(ks[10], (N_EVEN_LAYERS, D, D_FF), D ** -0.5),
        "ffn_w2": nrm(ks[11], (N_EVEN_LAYERS, D_FF, D), D_FF ** -0.5),
        "w_qkv": nrm(ks[12], (N_ODD_LAYERS, D, Q_DIM + 2 * KV_DIM), D ** -0.5),
        "w_o": nrm(ks[13], (N_ODD_LAYERS, Q_DIM, D), Q_DIM ** -0.5),
        "q_norm_g": 1.0 + nrm(ks[14], (N_ODD_LAYERS, HEAD_DIM), 0.1),
        "k_norm_g": 1.0 + nrm(ks[15], (N_ODD_LAYERS, HEAD_DIM), 0.1),
        "router_w": nrm(ks[16], (N_ODD_LAYERS, D, N_EXPERTS), D ** -0.5),
        "moe_w1": nrm(ks[17], (N_ODD_LAYERS, N_EXPERTS, D, D_FF), D ** -0.5),
        "moe_w3": nrm(ks[18], (N_ODD_LAYERS, N_EXPERTS, D, D_FF), D ** -0.5),
        "moe_w2": nrm(ks[19], (N_ODD_LAYERS, N_EXPERTS, D_FF, D), D_FF ** -0.5),
        "final_g": 1.0 + nrm(ks[20], (D,), 0.1),
    }


def reference(x, c, ctx, c_ctx, ada_w, ada_b, norm_g, pool_w, pool_scale, ffn_w1, ffn_w3, ffn_w2,
              w_qkv, w_o, q_norm_g, k_norm_g, router_w, moe_w1, moe_w3, moe_w2, final_g):
    L = x.shape[1]
    ROWS = L // GRID_W
    t_row = jnp.repeat(jnp.arange(ROWS, dtype=jnp.int32), GRID_W)
    t_col = jnp.tile(jnp.arange(GRID_W, dtype=jnp.int32), ROWS)

    for i in range(DEPTH):
        j = i // N_MIXERS
        last = i == DEPTH - 1
        sh1, sc1, g1, sh2, sc2, g2 = adaln(c, ada_w[i], ada_b[i])
        ksh1, ksc1, kg1, ksh2, ksc2, kg2 = adaln(c_ctx[None, :], ada_w[i], ada_b[i])

        hx = modulate(rmsnorm(x, norm_g[i, 0]), sh1, sc1)
        hk = modulate(rmsnorm(ctx, norm_g[i, 0]), ksh1, ksc1)
        if i % N_MIXERS == 0:
            x = x + g1 * pool_mix(hx, pool_w[j], pool_scale[j])
            if not last:
                ctx = ctx + kg1 * pool_mix(hk, pool_w[j], pool_scale[j])
        else:
            qkv = hx @ w_qkv[j]
            qx = rope_2d(heads_q(qkv[..., :Q_DIM], q_norm_g[j]), t_row, t_col)
            kx, vx = heads_kv(qkv[..., Q_DIM:], k_norm_g[j])
            kx = rope_2d(kx, t_row, t_col)
            if last:
                kk, vk = heads_kv(hk @ w_qkv[j][:, Q_DIM:], k_norm_g[j])
            else:
                qkv_c = hk @ w_qkv[j]
                kk, vk = heads_kv(qkv_c[..., Q_DIM:], k_norm_g[j])
                qk = heads_q(qkv_c[..., :Q_DIM], q_norm_g[j])
                ctx = ctx + kg1 * (attend(qk, kk, vk) @ w_o[j])
            k_all = jnp.concatenate([kx, kk], axis=1)
            v_all = jnp.concatenate([vx, vk], axis=1)
            x = x + g1 * (attend(qx, k_all, v_all) @ w_o[j])

        hx = modulate(rmsnorm(x, norm_g[i, 1]), sh2, sc2)
        if i % N_MIXERS == 0:
            x = x + g2 * swiglu(hx, ffn_w1[j], ffn_w3[j], ffn_w2[j])
            if not last:
                hk = modulate(rmsnorm(ctx, norm_g[i, 1]), ksh2, ksc2)
                ctx = ctx + kg2 * swiglu(hk, ffn_w1[j], ffn_w3[j], ffn_w2[j])
        else:
            x = x + g2 * moe_swiglu(hx, router_w[j], moe_w1[j], moe_w3[j], moe_w2[j])
            if not last:
                hk = modulate(rmsnorm(ctx, norm_g[i, 1]), ksh2, ksc2)
                ctx = ctx + kg2 * moe_swiglu(hk, router_w[j], moe_w1[j], moe_w3[j], moe_w2[j])

    return rmsnorm(x, final_g)
```

```python
import numpy as np, os
DBG=os.environ.get('DBG','')
from contextlib import ExitStack
import concourse.bass as bass
import concourse.mybir as mybir
from concourse.bass_utils import run_bass_kernel_spmd

F32 = mybir.dt.float32; BF16 = mybir.dt.bfloat16
AF = mybir.ActivationFunctionType; ALU = mybir.AluOpType; AX = mybir.AxisListType

D = 1024; DFF = 2816; NF = 22; SEQ = 16384; CTX = 256; TPC = 4096; TT = 256; NT = TPC // TT
ENGS = ['sync', 'scalar', 'vector', 'gpsimd', 'tensor']
NDSEM = 8
SAME_SYNC = {'gpsimd', 'scalar', 'vector'}
_uid = [0]


class _Op:
    __slots__ = ('eng', 'fn', 'deps', 'dma', 'signal', 'sigval', 'dsem', 'dval', 'prev_dma')

    def __init__(s, eng, fn, dma):
        s.eng = eng; s.fn = fn; s.dma = dma; s.deps = []; s.signal = False; s.sigval = 0
        s.dsem = None; s.dval = 0; s.prev_dma = None


class Prog:
    def __init__(s, nc):
        s.nc = nc; s.ops = {e: [] for e in ENGS}; s.lastw = {}; s.readers = {}
        s.dma_count = {e: 0 for e in ENGS}; s.dma_hist = {e: [] for e in ENGS}

    def add(s, eng, fn, r=(), w=(), dma=False):
        op = _Op(eng, fn, dma); deps = []
        for k in r:
            d = s.lastw.get(k)
            if d is not None: deps.append(d)
        for k in w:
            d = s.lastw.get(k)
            if d is not None: deps.append(d)
            deps.extend(s.readers.get(k, ()))
        seen = set()
        for d in deps:
            if id(d) in seen: continue
            seen.add(id(d))
            if d.dma or d.eng != eng or eng in SAME_SYNC:
                op.deps.append(d)
                if not d.dma: d.signal = True
        for k in w:
            s.lastw[k] = op; s.readers[k] = []
        for k in r:
            s.readers.setdefault(k, []).append(op)
        if dma:
            n = s.dma_count[eng]; s.dma_count[eng] = n + 1
            op.dsem = n % NDSEM; op.dval = 16 * (n // NDSEM + 1)
            h = s.dma_hist[eng]
            if n >= NDSEM: op.prev_dma = h[n - NDSEM]
            h.append(op)
        s.ops[eng].append(op)
        return op

    def dma(s, eng, out, in_, r=(), w=(), **kw):
        return s.add(eng, lambda e: e.dma_start(out=out, in_=in_, **kw), r=r, w=w, dma=True)

    def emit(s, ctx):
        nc = s.nc; _uid[0] += 1; u = _uid[0]
        esem = {e: nc.alloc_semaphore(name=f"es{u}_{e}") for e in ENGS}
        dsem = {e: [nc.alloc_semaphore(name=f"ds{u}_{e}{i}") for i in range(NDSEM)]
                for e in ENGS if s.dma_count[e] > 0}
        allsems = list(esem.values()) + [h for l in dsem.values() for h in l]

        def _cleanup():
            nc.clear_and_free_semaphores(allsems)
            nc.all_engine_barrier()
        ctx.callback(_cleanup)
        for e in ENGS:
            c = 0
            for op in s.ops[e]:
                if op.signal and not op.dma:
                    c += 1; op.sigval = c
        block = ctx.enter_context(nc.Block())

        def mk(ename):
            def body(e):
                waited = {}

                def wait(sem, val, key):
                    if waited.get(key, 0) < val:
                        e.wait_ge(sem, val); waited[key] = val
                for op in s.ops[ename]:
                    for d in op.deps:
                        if d.dma: wait(dsem[d.eng][d.dsem], d.dval, ('d', d.eng, d.dsem))
                        else: wait(esem[d.eng], d.sigval, ('e', d.eng))
                    if op.dma and op.prev_dma is not None:
                        d = op.prev_dma; wait(dsem[d.eng][d.dsem], d.dval, ('d', d.eng, d.dsem))
                    ins = op.fn(e)
                    if op.dma: ins.then_inc(dsem[ename][op.dsem], 16)
                    elif op.signal: ins.then_inc(esem[ename], 1)
                if ename == 'sync':
                    for q in dsem:
                        for d in s.dma_hist[q][-NDSEM:]:
                            wait(dsem[q][d.dsem], d.dval, ('d', q, d.dsem))
            return body
        for ename in ENGS:
            if s.ops[ename] or ename == 'sync':
                getattr(block, ename)(mk(ename))


class Rec:
    def __init__(s): s.l = []; s.cur = None

    def _put(s, op):
        if s.cur is not None: s.cur.append(op)
        else: s.l.append([op])

    def add(s, *a, **k): s._put(('add', a, k))

    def dma(s, *a, **k): s._put(('dma', a, k))

    def atom(s):
        rec = s

        class _A:
            def __enter__(self_):
                assert rec.cur is None; rec.cur = []

            def __exit__(self_, *a):
                rec.l.append(rec.cur); rec.cur = None
        return _A()


def interleave(P, recs, W):
    active = {}; nxt = 0
    while nxt < len(recs) or active:
        while nxt < len(recs) and (nxt % W) not in active:
            active[nxt % W] = iter(recs[nxt].l); nxt += 1
        for sl in sorted(active, key=lambda q: q):
            unit = next(active[sl], None)
            if unit is None:
                del active[sl]; continue
            for op in unit:
                getattr(P, op[0])(*op[1], **op[2])


class _NoAtom:
    def __enter__(s): pass

    def __exit__(s, *a): pass


def atom_of(R):
    return R.atom() if isinstance(R, Rec) else _NoAtom()


class K:
    def __init__(s):
        s.nc = bass.Bass("TRN2", target_bir_lowering=False)
        s.dr = {}

    def din(s, name, shape, dt=F32):
        s.dr[name] = s.nc.dram_tensor(name, list(shape), dt, kind="ExternalInput").ap(); return s.dr[name]

    def dout(s, name, shape, dt=F32):
        s.dr[name] = s.nc.dram_tensor(name, list(shape), dt, kind="ExternalOutput").ap(); return s.dr[name]

    def dint(s, name, shape, dt=F32):
        s.dr[name] = s.nc.dram_tensor(name, list(shape), dt, kind="Internal").ap(); return s.dr[name]


def stage0(k):
    nc = k.nc; dr = k.dr
    with ExitStack() as ctx:
        T = lambda n, sh, dt=F32: ctx.enter_context(nc.sbuf_tensor("s0_" + n, sh, dt))
        cT = T("cT", [128, 8, 2]); scT = T("scT", [128, 8, 2])
        awt = [T(f"aw{i}", [128, 8, 512]) for i in range(2)]
        adab = T("adab", [2, 2, 6144]); modsb = T("mod", [2, 2, 6144])
        pm = [ctx.enter_context(nc.psum_tensor(f"s0_pm{i}", [128, 512], F32)) for i in range(2)]
        P = Prog(nc)
        for v in range(2):
            P.dma('sync', cT[:, :, v], dr['cvec'][v, :].rearrange("(c p) -> p c", p=128), w=['cT'], allow_slow_non_contiguous=True)
        for i in range(2):
            P.dma('gpsimd', adab[:, i, :], dr['ada_b'][i, :].partition_broadcast(2), w=[('adab', i)])
        P.add('scalar', lambda e: e.activation(out=scT[:], in_=cT[:], func=AF.Silu), r=['cT'], w=['scT'])
        n = 0
        for i in range(2):
            for cb in range(12):
                sl = n % 2; n += 1
                P.dma('sync', awt[sl][:], dr['ada_w'][i, :, cb * 512:(cb + 1) * 512].rearrange("(c p) n -> p c n", p=128),
                      w=[('aw', sl)])
                for kc in range(8):
                    P.add('tensor', lambda e, sl=sl, kc=kc: e.matmul(pm[sl][0:2, :], lhsT=scT[:, kc, :], rhs=awt[sl][:, kc, :],
                                                                     start=(kc == 0), stop=(kc == 7)),
                          r=['scT', ('aw', sl)], w=[('pm', sl)])
                P.add('vector', lambda e, sl=sl, i=i, cb=cb: e.tensor_tensor(out=modsb[:, i, cb * 512:(cb + 1) * 512], in0=pm[sl][0:2, :],
                                                                            in1=adab[:, i, cb * 512:(cb + 1) * 512], op=ALU.add),
                      r=[('pm', sl), ('adab', i)], w=['modsb'])
        P.dma('sync', dr['modD'].rearrange("i v n -> v i n"), modsb[:], r=['modsb'], w=['modD'])
        P.emit(ctx)


def load_AB(k, ctx, P, layer, pref):
    nc = k.nc; dr = k.dr
    T = lambda n, sh, dt=F32: ctx.enter_context(nc.sbuf_tensor(pref + n, sh, dt))
    modF = T("modF", [128, 2, 48]); gF = T("gF", [128, 2, 8]); AB = T("AB", [128, 2, 2, 2, 8])
    for v in range(2):
        P.dma('sync', modF[:, v, :], dr['modD'][layer, v, :].rearrange("(kc p) -> p kc", p=128), w=[('modF', v)],
              allow_slow_non_contiguous=True)
    for sub in range(2):
        P.dma('sync', gF[:, sub, :], dr['norm_g'][layer, sub, :].rearrange("(c p) -> p c", p=128), w=['gF'], allow_slow_non_contiguous=True)
    for v in range(2):
        for sub in range(2):
            sh = modF[:, v, (3 * sub) * 8:(3 * sub) * 8 + 8]; sc = modF[:, v, (3 * sub + 1) * 8:(3 * sub + 1) * 8 + 8]
            P.add('vector', lambda e, v=v, sub=sub, sc=sc: e.scalar_tensor_tensor(out=AB[:, v, sub, 0, :], in0=sc, scalar=1.0, in1=gF[:, sub, :],
                                                                              op0=ALU.add, op1=ALU.mult),
                  r=[('modF', v), 'gF'], w=['AB'])
            P.add('vector', lambda e, v=v, sub=sub, sh=sh: e.tensor_copy(out=AB[:, v, sub, 1, :], in_=sh), r=[('modF', v)], w=['AB'])
    return AB


def consts(k, ctx, P, pref):
    nc = k.nc
    ident = ctx.enter_context(nc.sbuf_tensor(pref + "ident", [128, 128], BF16))
    eps_t = ctx.enter_context(nc.sbuf_tensor(pref + "eps", [128, 1], F32))
    P.add('gpsimd', lambda e: e.memset(ident[:], 0.0), w=['ident'])
    P.add('gpsimd', lambda e: e.affine_select(out=ident[:], in_=ident[:], pattern=[[-1, 128]], compare_op=ALU.not_equal, fill=1.0,
                                              base=0, channel_multiplier=1), w=['ident'])
    P.add('gpsimd', lambda e: e.memset(eps_t[:], 1e-6), w=['eps'])
    return ident, eps_t


def norm_ops(P, xt_chunks, xkey, ssq, rstd, xn_chunks, eps_t, s):
    n = len(xt_chunks)
    for j, (xc, xo) in enumerate(zip(xt_chunks, xn_chunks)):
        np_ = xc.shape[0]
        P.add('scalar', lambda e, xc=xc, xo=xo, j=j, np_=np_: e.activation(out=xo, in_=xc, func=AF.Square, accum_out=ssq[0:np_, j:j + 1]),
              r=[xkey], w=[('xn', s), ('ssq', s)])
    P.add('scalar', lambda e: e.activation(out=rstd[:, 0:n], in_=ssq[:, 0:n], func=AF.Sqrt, scale=1.0 / D, bias=eps_t[:, 0:1]),
          r=[('ssq', s), 'eps'], w=[('rstd', s)])
    P.add('vector', lambda e: e.reciprocal(out=rstd[:, 0:n], in_=rstd[:, 0:n]), r=[('rstd', s)], w=[('rstd', s)])
    for j, (xc, xo) in enumerate(zip(xt_chunks, xn_chunks)):
        np_ = xc.shape[0]
        P.add('gpsimd', lambda e, xc=xc, xo=xo, j=j, np_=np_: e.tensor_scalar(out=xo, in0=xc, scalar1=rstd[0:np_, j:j + 1], scalar2=None, op0=ALU.mult),
              r=[xkey, ('rstd', s)], w=[('xn', s)])


def emit_norm_T(P, xcs, xkey, ssq, rstd, xos, eps_t, ident, pT, A, B, hx, nk):
    n = len(xcs)
    for j in range(n):
        P.add('scalar', lambda e, j=j: e.activation(out=xos[j], in_=xcs[j], func=AF.Square, accum_out=ssq[:, j:j + 1]),
              r=[xkey], w=[nk + ('xn',), nk + ('ssq',)])
    P.add('scalar', lambda e: e.activation(out=rstd[:, 0:n], in_=ssq[:, 0:n], func=AF.Sqrt, scale=1.0 / D, bias=eps_t[:, 0:1]),
          r=[nk + ('ssq',), 'eps'], w=[nk + ('rstd',)])
    P.add('vector', lambda e: e.reciprocal(out=rstd[:, 0:n], in_=rstd[:, 0:n]), r=[nk + ('rstd',)], w=[nk + ('rstd',)])
    for j in range(n):
        P.add('scalar', lambda e, j=j: e.activation(out=xos[j], in_=xcs[j], func=AF.Identity, scale=rstd[:, j:j + 1]),
              r=[xkey, nk + ('rstd',)], w=[nk + ('xn',)])
    for pr in range(4):
        bk = pr % 2
        with atom_of(P):
            for fc in (2 * pr, 2 * pr + 1):
                q = fc % 4
                for j in range(n):
                    P.add('tensor', lambda e, q=q, j=j, fc=fc: e.transpose(out=pT[:, q, j * 128:(j + 1) * 128], in_=xos[j][:, fc * 128:(fc + 1) * 128],
                                                                           identity=ident[:]),
                          r=[nk + ('xn',), 'ident'], w=[('pT', bk)])
            for fc in (2 * pr, 2 * pr + 1):
                q = fc % 4
                P.add('scalar', lambda e, fc=fc, q=q: e.activation(out=hx[:, fc, 0:128 * n], in_=pT[:, q, 0:128 * n], func=AF.Identity,
                                                                    scale=A[:, fc:fc + 1], bias=B[:, fc:fc + 1]),
                      r=[('pT', bk), 'AB'], w=[nk + ('hx',)])

def stage1a(k, upto=99, ntiles=None, NSEG=1, W=int(os.environ.get('W1A', '3'))):
    nc = k.nc; dr = k.dr
    with ExitStack() as ctx:
        T = lambda n, sh, dt=F32: ctx.enter_context(nc.sbuf_tensor("a_" + n, sh, dt))
        P = Prog(nc)
        ident, eps_t = consts(k, ctx, P, "a_")
        AB = load_AB(k, ctx, P, 0, "a_")
        gp_bc = [T(f"gpbc{v}", [128, 1024]) for v in range(2)]; ps_bc = T("psbc", [128, 1024])
        P.dma('sync', ps_bc[:], dr['pool_scale'][0, :].partition_broadcast(128), w=['psbc'])
        for v in range(2):
            P.dma('sync', gp_bc[v][:], dr['modD'][0, v, 2048:3072].partition_broadcast(128), w=[('gpbc', v)])
            P.add('gpsimd', lambda e, v=v: e.tensor_tensor(out=gp_bc[v][:], in0=gp_bc[v][:], in1=ps_bc[:], op=ALU.mult),
                  r=['psbc'], w=[('gpbc', v)])
        inv = T("inv", [128, NSEG + 1, 2, 4, 2, 8]); msk = T("msk", [128, 2 * (NSEG + 1)])
        for sg in range(NSEG + 1):
            P.dma('sync', inv[:, sg].rearrange("p b c d e -> p (b c d e)"), dr['inv'][sg, :].partition_broadcast(128), w=['inv'])
        P.dma('sync', msk[:], dr['msk'][0, :].partition_broadcast(128), w=['msk'])
        wg = T("wg", [128, 4, 2, 256], BF16)
        P.dma('gpsimd', wg[:], dr['pool_w'].rearrange("g (c p) e -> p g c e", p=128), w=['wg'])
        xt = [T(f"xt{s}", [128, 2, 1024]) for s in range(W)]; xh = [T(f"xh{s}", [128, 1024]) for s in range(W)]
        ssq = [T(f"ssq{s}", [128, 3]) for s in range(W)]; rstd = [T(f"rstd{s}", [128, 3]) for s in range(W)]
        xn = [T(f"xn{s}", [128, 3, 1024], BF16) for s in range(W)]
        hxT = [T(f"hxT{s}", [128, 8, 272], BF16) for s in range(W)]
        SA = [T(f"SA{s}", [128, 2, 272]) for s in range(W)]; SB = [T(f"SB{s}", [128, 2, 272]) for s in range(W)]
        te = [T(f"te{s}", [128, 2, 8]) for s in range(W)]
        dT = [T(f"dT{s}", [128, 8, 256], BF16) for s in range(W)]
        tmp = [T(f"tmp{s}", [128, 1024]) for s in range(2)]
        ssq2 = [T(f"ssq2{s}", [128, 2]) for s in range(W)]; rstd2 = [T(f"rstd2{s}", [128, 2]) for s in range(W)]
        xn2 = [T(f"xn2{s}", [128, 2, 1024], BF16) for s in range(W)]; hx2 = [T(f"hx2{s}", [128, 8, 256], BF16) for s in range(W)]
        pT = ctx.enter_context(nc.psum_tensor("a_pT", [128, 4, 512], BF16))
        pD = [ctx.enter_context(nc.psum_tensor(f"a_pD{s}", [128, 1024], F32)) for s in range(2)]
        for s in range(W):
            P.add('gpsimd', lambda e, s=s: e.memset(ssq[s][:], 1.0), w=[('ssq', s)])
            P.add('gpsimd', lambda e, s=s: e.memset(xh[s][:], 0.0), w=[('xh', s)])
            P.add('gpsimd', lambda e, s=s: e.memset(xn[s][:, 2, :], 0.0), w=[('xn', s)])
        tiles = [(0, sg, t) for sg in range(NSEG) for t in range(NT)] + [(1, NSEG, 0)]
        if ntiles: tiles = tiles[:ntiles]
        recs = []
        for ti, (v, sg, t) in enumerate(tiles):
            s = ti % W
            R = Rec(); recs.append(R)
            src = dr['xin'][sg] if v == 0 else dr['ctxin']; dst = dr['xaD'][sg * 4096:(sg + 1) * 4096, :] if v == 0 else dr['caD']
            r0 = 256 * t
            R.dma('sync', xt[s][:], src[r0 + 8:r0 + 264, :].rearrange("(j p) d -> p j d", p=128), w=[('xt', s)])
            R.dma('sync', xh[s][0:8, :], src[r0:r0 + 8, :], w=[('xh', s)])
            R.dma('sync', xh[s][32:40, :], src[r0 + 264:r0 + 272, :], w=[('xh', s)])
            xcs = [xt[s][:, 0, :], xt[s][:, 1, :], xh[s][:, :]]; xos = [xn[s][:, 0, :], xn[s][:, 1, :], xn[s][:, 2, :]]
            for j in range(3):
                np_ = xcs[j].shape[0]
                R.add('scalar', lambda e, j=j, np_=np_, s=s, xcs=xcs, xos=xos: e.activation(out=xos[j], in_=xcs[j], func=AF.Square,
                                                                                      accum_out=ssq[s][0:np_, j:j + 1]),
                      r=[('xt', s), ('xh', s)], w=[('xn', s), ('ssq', s)])
            R.add('scalar', lambda e, s=s: e.activation(out=rstd[s][:], in_=ssq[s][:], func=AF.Sqrt, scale=1.0 / D, bias=eps_t[:, 0:1]),
                  r=[('ssq', s), 'eps'], w=[('rstd', s)])
            R.add('vector', lambda e, s=s: e.reciprocal(out=rstd[s][:], in_=rstd[s][:]), r=[('rstd', s)], w=[('rstd', s)])
            for j in range(3):
                np_ = xcs[j].shape[0]
                R.add('scalar', lambda e, j=j, np_=np_, s=s, xcs=xcs, xos=xos: e.activation(out=xos[j], in_=xcs[j], func=AF.Identity,
                                                                                      scale=rstd[s][0:np_, j:j + 1]),
                      r=[('xt', s), ('xh', s), ('rstd', s)], w=[('xn', s)])
            if upto < 1: continue
            for pr in range(4):
              bk = pr % 2
              with R.atom():
                for fc in (2 * pr, 2 * pr + 1):
                    q = fc % 4
                    for jj in range(3):
                        R.add('tensor', lambda e, s=s, q=q, jj=jj, fc=fc: e.transpose(out=pT[:, q, jj * 128:(jj + 1) * 128],
                                                                                 in_=xn[s][:, jj, fc * 128:(fc + 1) * 128], identity=ident[:]),
                              r=[('xn', s), 'ident'], w=[('pT', bk)])
                for fc in (2 * pr, 2 * pr + 1):
                    q = fc % 4
                    A_ = AB[:, v, 0, 0, fc:fc + 1]; B_ = AB[:, v, 0, 1, fc:fc + 1]
                    R.add('scalar', lambda e, s=s, fc=fc, q=q, A_=A_, B_=B_: e.activation(out=hxT[s][:, fc, 8:264], in_=pT[:, q, 0:256], func=AF.Identity,
                                                                                      scale=A_, bias=B_),
                          r=[('pT', bk), 'AB'], w=[('hxT', s)])
                    for (o0, i0) in ((0, 256), (264, 288)):
                        R.add('vector', lambda e, s=s, fc=fc, q=q, A_=A_, B_=B_, o0=o0, i0=i0: e.tensor_scalar(out=hxT[s][:, fc, o0:o0 + 8], in0=pT[:, q, i0:i0 + 8],
                                                                                                     scalar1=A_, scalar2=B_, op0=ALU.mult, op1=ALU.add),
                              r=[('pT', bk), 'AB'], w=[('hxT', s)])
            if upto < 2: continue
            first = (t == 0); last = (v == 1) or (t == NT - 1)
            if first:
                R.add('vector', lambda e, s=s, sg=sg: e.tensor_scalar(out=hxT[s][:, :, 0:8], in0=hxT[s][:, :, 0:8], scalar1=msk[:, 2 * sg:2 * sg + 1],
                                                                     scalar2=None, op0=ALU.mult), r=['msk'], w=[('hxT', s)])
            if last:
                R.add('vector', lambda e, s=s, sg=sg: e.tensor_scalar(out=hxT[s][:, :, 264:272], in0=hxT[s][:, :, 264:272], scalar1=msk[:, 2 * sg + 1:2 * sg + 2],
                                                                     scalar2=None, op0=ALU.mult), r=['msk'], w=[('hxT', s)])
            for g in range(4):
                h = hxT[s][:, 2 * g:2 * g + 2, :]
                bufs = [SA[s], SB[s]]
                R.add('vector', lambda e, s=s, h=h: e.tensor_tensor(out=SA[s][:, :, 1:272], in0=h[:, :, 0:271], in1=h[:, :, 1:272], op=ALU.add),
                      r=[('hxT', s)], w=[('S', s)])
                cur = 0
                steps = [(1, 2, 271), (2, 4, 269), (4, 8, 265)]
                for (sh_, lo, hi) in steps[:g]:
                    a = bufs[cur]; b = bufs[1 - cur]
                    R.add('vector', lambda e, a=a, b=b, sh_=sh_, lo=lo, hi=hi: e.tensor_tensor(out=b[:, :, lo:hi], in0=a[:, :, lo - sh_:hi - sh_],
                                                                                         in1=a[:, :, lo + sh_:hi + sh_], op=ALU.add),
                          r=[('S', s)], w=[('S', s)])
                    cur = 1 - cur
                Sf = bufs[cur]; wv = 1.0 / (2 << g)
                R.add('vector', lambda e, s=s, g=g, Sf=Sf, h=h, wv=wv: e.scalar_tensor_tensor(out=dT[s][:, 2 * g:2 * g + 2, :], in0=Sf[:, :, 8:264], scalar=wv,
                                                                                         in1=h[:, :, 8:264], op0=ALU.mult, op1=ALU.subtract),
                      r=[('S', s), ('hxT', s)], w=[('dT', s)])
                for (flag, eidx, c0) in ((first, 0, 0), (last, 1, 248)):
                    if not flag: continue
                    R.add('vector', lambda e, s=s, g=g, Sf=Sf, sg=sg, eidx=eidx, c0=c0: e.tensor_tensor(out=te[s][:], in0=Sf[:, :, 8 + c0:16 + c0],
                                                                                                in1=inv[:, sg, eidx, g, :, :], op=ALU.mult),
                          r=[('S', s), 'inv'], w=[('te', s)])
                    R.add('vector', lambda e, s=s, g=g, h=h, c0=c0: e.tensor_tensor(out=dT[s][:, 2 * g:2 * g + 2, c0:c0 + 8], in0=te[s][:],
                                                                                in1=h[:, :, 8 + c0:16 + c0], op=ALU.subtract),
                          r=[('te', s), ('hxT', s)], w=[('dT', s)])
            if upto < 3: continue
            for j in range(2):
              with R.atom():
                for g in range(4):
                    for c in range(2):
                        R.add('tensor', lambda e, s=s, j=j, g=g, c=c: e.matmul(pD[j][:, g * 256:(g + 1) * 256], lhsT=dT[s][:, 2 * g + c, j * 128:(j + 1) * 128],
                                                                              rhs=wg[:, g, c, :], start=(c == 0), stop=(c == 1)),
                              r=[('dT', s), 'wg'], w=[('pD', j)])
                R.add('vector', lambda e, s=s, j=j, v=v: e.tensor_tensor(out=tmp[j][:], in0=pD[j][:], in1=gp_bc[v][:], op=ALU.mult),
                      r=[('pD', j), ('gpbc', v)], w=[('tmp', j)])
                R.add('vector', lambda e, s=s, j=j: e.tensor_tensor(out=xt[s][:, j, :], in0=xt[s][:, j, :], in1=tmp[j][:], op=ALU.add),
                      r=[('tmp', j)], w=[('xt', s)])
            R.dma('sync', dst[r0:r0 + 256, :].rearrange("(j p) d -> p j d", p=128), xt[s][:], r=[('xt', s)], w=['xaD'])
            hdst = dr['hxD'][:, :, sg * 4096:(sg + 1) * 4096] if v == 0 else dr['hcD']
            emit_norm_T(R, [xt[s][:, 0, :], xt[s][:, 1, :]], ('xt', s), ssq2[s], rstd2[s], [xn2[s][:, 0, :], xn2[s][:, 1, :]], eps_t, ident, pT,
                        AB[:, v, 1, 0, :], AB[:, v, 1, 1, :], hx2[s], ('n2', s))
            R.dma('sync', hdst[:, :, r0:r0 + 256].rearrange("c p t -> p c t"), hx2[s][:], r=[('n2', s, 'hx')], w=['hxD'])
        interleave(P, recs, W)
        P.emit(ctx)


def ffn_pass(k, pref, units, tiles, g2_specs, gatesD=None):
    nc = k.nc; dr = k.dr; HF = 11; HW = 1408
    with ExitStack() as ctx:
        T = lambda n, sh, dt=F32: ctx.enter_context(nc.sbuf_tensor(pref + n, sh, dt))
        P = Prog(nc)
        w1b = [T(f"w1b{i}", [128, 8, HW], BF16) for i in range(2)]; w3b = [T(f"w3b{i}", [128, 8, HW], BF16) for i in range(2)]
        w2b = [T(f"w2b{i}", [128, HF, 1024], BF16) for i in range(2)]
        g2bc = {v: T(f"g2bc{v}", [128, 1024]) for v in g2_specs}
        for v, src in g2_specs.items():
            P.dma('sync', g2bc[v][:], src.partition_broadcast(128), w=[('g2bc', v)])
        gates = None
        if gatesD is not None:
            gates = T("gates", [128, 32, 8])
            P.dma('sync', gates[:], gatesD.rearrange("(c p) e -> p c e", p=128), w=['gates'])
        hx = [T(f"hx{i}", [128, 8, 256], BF16) for i in range(3)]
        gT = T("gT", [128, HF, 256], BF16); sil = [T(f"sil{i}", [128, 256], BF16) for i in range(2)]
        tmp = [T(f"tmp{i}", [128, 512]) for i in range(4)]
        ph = [ctx.enter_context(nc.psum_tensor(f"{pref}ph{i}", [128, 2, 256], F32)) for i in range(2)]
        pD = [ctx.enter_context(nc.psum_tensor(f"{pref}pD{i}", [128, 512], F32)) for i in range(2)]
        nh = 0; nd = 0
        def loadw(u):
            (w1, w3, w2, ex) = units[u]; b = u % 2
            for dc in range(8):
                P.dma('gpsimd', w1b[b][:, dc, :], w1[dc * 128:(dc + 1) * 128, :], w=[('w1', b, dc)])
                P.dma('gpsimd', w3b[b][:, dc, :], w3[dc * 128:(dc + 1) * 128, :], w=[('w3', b, dc)])
            for f in range(HF):
                P.dma('gpsimd', w2b[b][:, f, :], w2[f * 128:(f + 1) * 128, :], w=[('w2', b, f)])
        loadw(0)
        for u, (w1, w3, w2, ex) in enumerate(units):
            b = u % 2
            if u + 1 < len(units): loadw(u + 1)
            for (hsrc, odst, v, ch0) in tiles:
                hs = nh % 3; nh += 1
                P.dma('sync', hx[hs][:], hsrc.rearrange("c p t -> p c t"), w=[('hx', hs)])
                for f in range(HF):
                    pb = f % 2
                    for (wb, wk, col) in ((w1b, 'w1', 0), (w3b, 'w3', 1)):
                        for dc in range(8):
                            P.add('tensor', lambda e, wb=wb, b=b, dc=dc, f=f, pb=pb, col=col, hs=hs: e.matmul(
                                ph[pb][:, col, :], lhsT=wb[b][:, dc, f * 128:(f + 1) * 128], rhs=hx[hs][:, dc, :], start=(dc == 0), stop=(dc == 7)),
                                r=[(wk, b, dc), ('hx', hs)], w=[('ph', pb)])
                    P.add('scalar', lambda e, pb=pb: e.activation(out=sil[pb][:], in_=ph[pb][:, 0, :], func=AF.Silu), r=[('ph', pb)], w=[('sil', pb)])
                    P.add('vector', lambda e, pb=pb, f=f: e.tensor_tensor(out=gT[:, f, :], in0=sil[pb][:], in1=ph[pb][:, 1, :], op=ALU.mult),
                          r=[('sil', pb), ('ph', pb)], w=[('gT', f)])
                for j in range(2):
                    for half in range(2):
                        db = nd % 2; ts = nd % 4; nd += 1
                        for f in range(HF):
                            P.add('tensor', lambda e, db=db, f=f, j=j, half=half, b=b: e.matmul(
                                pD[db][:], lhsT=gT[:, f, j * 128:(j + 1) * 128], rhs=w2b[b][:, f, half * 512:(half + 1) * 512], start=(f == 0), stop=(f == HF - 1)),
                                r=[('gT', f), ('w2', b, f)], w=[('pD', db)])
                        if gates is None:
                            P.add('vector', lambda e, db=db, ts=ts, half=half, v=v: e.tensor_tensor(out=tmp[ts][:], in0=pD[db][:],
                                                                                           in1=g2bc[v][:, half * 512:(half + 1) * 512], op=ALU.mult),
                                  r=[('pD', db), ('g2bc', v)], w=[('tmp', ts)])
                        else:
                            P.add('vector', lambda e, db=db, ts=ts, half=half, v=v, c=ch0 + j, ex=ex: e.scalar_tensor_tensor(
                                out=tmp[ts][:], in0=pD[db][:], scalar=gates[:, c, ex:ex + 1], in1=g2bc[v][:, half * 512:(half + 1) * 512],
                                op0=ALU.mult, op1=ALU.mult), r=[('pD', db), ('g2bc', v), 'gates'], w=[('tmp', ts)])
                        P.dma('gpsimd', odst[j * 128:(j + 1) * 128, half * 512:(half + 1) * 512], tmp[ts][:], r=[('tmp', ts)],
                              w=[('out', odst.name, ch0 + j, half)], accum_op=ALU.add)
        P.emit(ctx)


def stage1b(k, NSEG=1):
    dr = k.dr
    units = [(dr['ffn_w1'][:, h * 1408:(h + 1) * 1408], dr['ffn_w3'][:, h * 1408:(h + 1) * 1408], dr['ffn_w2'][h * 1408:(h + 1) * 1408, :], 0)
             for h in range(2)]
    tiles = [(dr['hxD'][:, :, 256 * t:256 * (t + 1)], dr['xaD'][256 * t:256 * (t + 1), :], 0, 2 * t) for t in range(NT * NSEG)]
    tiles.append((dr['hcD'][:, :, :], dr['caD'][:, :], 1, 2 * NT * NSEG))
    ffn_pass(k, "b_", units, tiles, {0: dr['modD'][0, 0, 5120:6144], 1: dr['modD'][0, 1, 5120:6144]})


def stage3(k, NQB=8, NKC=130, A_LIST=(0, 1)):
    nc = k.nc; dr = k.dr; NK = NKC * 128
    with ExitStack() as ctx:
        T = lambda n, sh, dt=F32: ctx.enter_context(nc.sbuf_tensor("c_" + n, sh, dt))
        P = Prog(nc)
        KT = T("KT", [128, NK], BF16); V = T("V", [128, NKC, 2, 65], BF16)
        wo = T("wo", [64, 16, 1024], BF16); wof = T("wof", [64, 2, 1024]); g1bc = T("g1bc", [64, 1024])
        QT = [T(f"QT{i}", [128, 4, 512], BF16) for i in range(2)]
        PT = [T(f"PT{i}", [128, 1024], BF16) for i in range(3)]
        oS = T("oS", [65, 2, 512]); attnT = T("attnT", [64, 8, 512], BF16)
        ones = T("ones", [65, 64]); tmp = [T(f"tmp{i}", [128, 512]) for i in range(2)]
        ps = [ctx.enter_context(nc.psum_tensor(f"c_ps{i}", [128, 1024], F32)) for i in range(2)]
        po = [ctx.enter_context(nc.psum_tensor(f"c_po{i}", [128, 512], F32)) for i in range(2)]
        pbc = ctx.enter_context(nc.psum_tensor("c_pbc", [128, 512], F32))
        pw = ctx.enter_context(nc.psum_tensor("c_pw", [128, 512], F32))
        P.add('gpsimd', lambda e: e.memset(ones[:], 1.0), w=['ones'])
        P.dma('sync', g1bc[:], dr['modD'][1, 0, 2048:3072].partition_broadcast(64), w=['g1bc'])
        for h in range(16):
            sl = h % 2
            P.dma('sync', wof[:, sl, :], dr['w_o'][h * 64:(h + 1) * 64, :], w=[('wof', sl)])
            P.add('vector', lambda e, h=h, sl=sl: e.tensor_tensor(out=wo[:, h, :], in0=wof[:, sl, :], in1=g1bc[:], op=ALU.mult),
                  r=[('wof', sl), 'g1bc'], w=['wo'])
        nq = 0; nps = 0; npt = 0; ntmp = 0
        for a in A_LIST:
            P.dma('sync', KT[:, :], dr['KTD'][2 * a:2 * a + 2, :, :].rearrange("b d t -> (b d) t"), w=['KT'])
            for c0 in range(0, NKC, 26):
                c1 = min(NKC, c0 + 26)
                P.dma('sync', V[:, c0:c1, :, :], dr['VD'][c0 * 128:c1 * 128, 2 * a:2 * a + 2, :].rearrange("(c p) g e -> p c g e", p=128), w=['V'])
            for qb in range(NQB):
                qs = nq % 2; nq += 1
                P.dma('sync', QT[qs][:], dr['QTD'][a * 8:(a + 1) * 8, :, qb * 512:(qb + 1) * 512].rearrange("(i b) d t -> (b d) i t", b=2), w=[('QT', qs)])
                for i in range(4):
                    def qk(kc, s):
                        for b in range(2):
                            P.add('tensor', lambda e, b=b, kc=kc, s=s, qs=qs, i=i: e.matmul(
                                ps[s][:, b * 512:(b + 1) * 512], lhsT=KT[b * 64:(b + 1) * 64, kc * 128:(kc + 1) * 128],
                                rhs=QT[qs][b * 64:(b + 1) * 64, i, :], start=True, stop=True),
                                r=['KT', ('QT', qs)], w=[('ps', s)])
                    s0 = nps % 2; nps += 1
                    qk(0, s0); cur = s0
                    for kc in range(NKC):
                        t = npt % 3; npt += 1
                        if kc + 1 < NKC:
                            s1 = nps % 2; nps += 1
                            qk(kc + 1, s1)
                        P.add('scalar', lambda e, cur=cur, t=t: e.activation(out=PT[t][:], in_=ps[cur][:], func=AF.Exp, scale=0.125),
                              r=[('ps', cur)], w=[('PT', t)])
                        for b in range(2):
                            P.add('tensor', lambda e, b=b, kc=kc, t=t: e.matmul(
                                po[b][0:65, :], lhsT=V[:, kc, b, :], rhs=PT[t][:, b * 512:(b + 1) * 512], start=(kc == 0), stop=(kc == NKC - 1)),
                                r=['V', ('PT', t)], w=[('po', b)])
                        if kc + 1 < NKC: cur = s1
                    for b in range(2):
                        P.add('vector', lambda e, b=b: e.tensor_copy(out=oS[:, b, :], in_=po[b][0:65, :]), r=[('po', b)], w=[('oS', b)])
                        P.add('vector', lambda e, b=b: e.reciprocal(out=oS[64:65, b, :], in_=oS[64:65, b, :]), r=[('oS', b)], w=[('oS', b)])
                        P.add('tensor', lambda e, b=b: e.matmul(pbc[0:64, :], lhsT=ones[64:65, :], rhs=oS[64:65, b, :], start=True, stop=True),
                              r=[('oS', b), 'ones'], w=['pbc'])
                        P.add('vector', lambda e, b=b, i=i: e.tensor_tensor(out=attnT[:, i * 2 + b, :], in0=oS[0:64, b, :], in1=pbc[0:64, :], op=ALU.mult),
                              r=[('oS', b), 'pbc'], w=[('attnT', i * 2 + b)])
                for j in range(4):
                    for half in range(2):
                        for hh in range(8):
                            i_, b_ = hh // 2, hh % 2; h = a * 8 + b_ * 4 + i_
                            P.add('tensor', lambda e, hh=hh, h=h, j=j, half=half: e.matmul(
                                pw[:, :], lhsT=attnT[:, hh, j * 128:(j + 1) * 128], rhs=wo[:, h, half * 512:(half + 1) * 512], start=(hh == 0), stop=(hh == 7)),
                                r=[('attnT', hh), 'wo'], w=['pw'])
                        ts = ntmp % 2; ntmp += 1
                        P.add('vector', lambda e, ts=ts: e.tensor_copy(out=tmp[ts][:], in_=pw[:, :]), r=['pw'], w=[('tmp', ts)])
                        r0 = qb * 512 + j * 128
                        P.dma('gpsimd', dr['xaD'][r0:r0 + 128, half * 512:(half + 1) * 512], tmp[ts][:], r=[('tmp', ts)],
                              w=[('xo', qb, j, half)], accum_op=ALU.add)
        P.emit(ctx)


def vw(t, off, dims):
    b = t[:]
    return bass.AP(tensor=b.tensor, offset=b.offset + off, ap=[list(b.ap[0])] + [list(d) for d in dims])


def stage2(k, ntiles=None, NSEG=1):
    nc = k.nc; dr = k.dr
    with ExitStack() as ctx:
        T = lambda n, sh, dt=F32: ctx.enter_context(nc.sbuf_tensor("q_" + n, sh, dt))
        P = Prog(nc)
        ident, eps_t = consts(k, ctx, P, "q_")
        AB = load_AB(k, ctx, P, 1, "q_")
        wq = T("wq", [128, 8, 1536], BF16)
        for dc in range(8):
            for a in range(2):
                for b in range(2):
                    P.dma('gpsimd', vw(wq, dc * 1536 + a * 512 + b * 64, [[128, 4], [1, 64]]),
                          dr['w_qkv'][dc * 128:(dc + 1) * 128, a * 512 + b * 256:a * 512 + (b + 1) * 256].rearrange("p (i d) -> p i d", d=64), w=[('wq', dc)])
            P.dma('gpsimd', wq[:, dc, 1024:1536], dr['w_qkv'][dc * 128:(dc + 1) * 128, 1024:1536], w=[('wq', dc)])
        gbc = T("gbc", [128, 1280])
        P.dma('sync', gbc[:], dr['qkg'][0, :].partition_broadcast(128), w=['gbc'])
        xt = [T(f"xt{s}", [128, 2, 1024]) for s in range(2)]
        ssq = [T(f"ssq{s}", [128, 2]) for s in range(2)]; rstd = [T(f"rstd{s}", [128, 2]) for s in range(2)]
        xn = [T(f"xn{s}", [128, 2, 1024], BF16) for s in range(2)]; hx = [T(f"hx{s}", [128, 8, 256], BF16) for s in range(2)]
        cs = [T(f"cs{s}", [128, 2, 2, 64]) for s in range(2)]
        qk = [T(f"qk{j}", [128, 1536]) for j in range(2)]; sq = [T(f"sq{j}", [128, 1280]) for j in range(2)]
        rh = [T(f"rh{j}", [128, 20]) for j in range(2)]; qn = [T(f"qn{j}", [128, 1280]) for j in range(2)]
        t2 = [T(f"t2{j}", [128, 1280]) for j in range(2)]; qr = [T(f"qr{j}", [128, 1280], BF16) for j in range(2)]
        vs = [T(f"vs{s}", [128, 2, 4, 65], BF16) for s in range(2)]
        qT = [T(f"qT{s}", [128, 10, 2, 128], BF16) for s in range(2)]
        pT = ctx.enter_context(nc.psum_tensor("q_pT", [128, 4, 512], BF16))
        pq = [ctx.enter_context(nc.psum_tensor(f"q_pq{i}", [128, 512], F32)) for i in range(3)]
        for s in range(2):
            P.add('gpsimd', lambda e, s=s: e.memset(vs[s][:], 1.0), w=[('vs', s)])
        tiles = [(0, t) for t in range(NT * NSEG)] + [(1, 0)]
        if ntiles: tiles = tiles[:ntiles - 1] + [(1, 0)]
        for ti, (v, t) in enumerate(tiles):
            s = ti % 2
            src = dr['xaD'] if v == 0 else dr['caD']
            r0 = 256 * t; k0 = r0 if v == 0 else 4096 * NSEG
            P.dma('sync', xt[s][:], src[r0:r0 + 256, :].rearrange("(j p) d -> p j d", p=128), w=[('xt', s)])
            P.dma('sync', cs[s][:, :, 0, :], dr['ropeC'][k0:k0 + 256, :].rearrange("(j p) e -> p j e", p=128), w=[('cs', s)])
            P.dma('sync', cs[s][:, :, 1, :], dr['ropeS'][k0:k0 + 256, :].rearrange("(j p) e -> p j e", p=128), w=[('cs', s)])
            emit_norm_T(P, [xt[s][:, 0, :], xt[s][:, 1, :]], ('xt', s), ssq[s], rstd[s], [xn[s][:, 0, :], xn[s][:, 1, :]], eps_t, ident, pT,
                        AB[:, v, 0, 0, :], AB[:, v, 0, 1, :], hx[s], ('n', s))
            for j in range(2):
                for cb in range(3):
                    for dc in range(8):
                        P.add('tensor', lambda e, s=s, j=j, cb=cb, dc=dc: e.matmul(pq[cb][:], lhsT=hx[s][:, dc, j * 128:(j + 1) * 128],
                                                                                 rhs=wq[:, dc, cb * 512:(cb + 1) * 512], start=(dc == 0), stop=(dc == 7)),
                              r=[('n', s, 'hx'), ('wq', dc)], w=[('pq', cb)])
                    P.add('scalar', lambda e, j=j, cb=cb: e.activation(out=qk[j][:, cb * 512:(cb + 1) * 512], in_=pq[cb][:], func=AF.Identity),
                          r=[('pq', cb)], w=[('qk', j)])
                Q = ('qk', j)
                P.add('scalar', lambda e, j=j: e.activation(out=sq[j][:], in_=qk[j][:, 0:1280], func=AF.Square), r=[Q], w=[('sq', j)])
                P.add('vector', lambda e, j=j: e.tensor_reduce(out=rh[j][:], in_=sq[j][:].rearrange("p (h d) -> p h d", d=64), axis=AX.X, op=ALU.add),
                      r=[('sq', j)], w=[('rh', j)])
                P.add('scalar', lambda e, j=j: e.activation(out=rh[j][:], in_=rh[j][:], func=AF.Sqrt, scale=1.0 / 64, bias=eps_t[:, 0:1]),
                      r=[('rh', j), 'eps'], w=[('rh', j)])
                P.add('vector', lambda e, j=j: e.reciprocal(out=rh[j][:], in_=rh[j][:]), r=[('rh', j)], w=[('rh', j)])
                P.add('vector', lambda e, j=j: e.tensor_tensor(out=qn[j][:].rearrange("p (h d) -> p h d", d=64), in0=qk[j][:, 0:1280].rearrange("p (h d) -> p h d", d=64),
                                                               in1=vw(rh[j], 0, [[1, 20], [0, 64]]), op=ALU.mult), r=[Q, ('rh', j)], w=[('qn', j)])
                P.add('vector', lambda e, j=j: e.tensor_tensor(out=qn[j][:], in0=qn[j][:], in1=gbc[:], op=ALU.mult), r=['gbc'], w=[('qn', j)])
                cofs = j * 128; sofs = j * 128 + 64
                P.add('vector', lambda e, j=j, s=s, sofs=sofs: e.tensor_tensor(out=vw(t2[j], 0, [[64, 20], [32, 2], [1, 16]]), in0=vw(qn[j], 16, [[64, 20], [32, 2], [1, 16]]),
                                                                         in1=vw(cs[s], sofs, [[0, 20], [32, 2], [1, 16]]), op=ALU.mult),
                      r=[('qn', j), ('cs', s)], w=[('t2', j)])
                P.add('vector', lambda e, j=j, s=s, sofs=sofs: e.tensor_tensor(out=vw(t2[j], 16, [[64, 20], [32, 2], [1, 16]]), in0=vw(qn[j], 0, [[64, 20], [32, 2], [1, 16]]),
                                                                         in1=vw(cs[s], sofs + 16, [[0, 20], [32, 2], [1, 16]]), op=ALU.mult),
                      r=[('qn', j), ('cs', s)], w=[('t2', j)])
                P.add('vector', lambda e, j=j, s=s, cofs=cofs: e.tensor_tensor(out=qn[j][:].rearrange("p (h d) -> p h d", d=64), in0=qn[j][:].rearrange("p (h d) -> p h d", d=64),
                                                                         in1=vw(cs[s], cofs, [[0, 20], [1, 64]]), op=ALU.mult), r=[('cs', s), ('t2', j)], w=[('qn', j)])
                P.add('vector', lambda e, j=j: e.tensor_tensor(out=qr[j][:], in0=qn[j][:], in1=t2[j][:], op=ALU.add), r=[('t2', j)], w=[('qn', j), ('qr', j)])
                P.add('scalar', lambda e, j=j, s=s: e.activation(out=vs[s][:, j, :, 0:64], in_=qk[j][:, 1280:1536].rearrange("p (g d) -> p g d", d=64), func=AF.Identity),
                      r=[Q], w=[('vs', s)])
                for grp in range(3):
                    rg = (0, 2, 1)[grp]; bk = rg // 2; n = 4 if grp < 2 else 2
                    for u in range(n):
                        sl = grp * 4 + u
                        P.add('tensor', lambda e, j=j, sl=sl, rg=rg, u=u: e.transpose(out=pT[:, rg, u * 128:(u + 1) * 128], in_=qr[j][:, sl * 128:(sl + 1) * 128],
                                                                                identity=ident[:]), r=[('qr', j), 'ident'], w=[('pT', bk)])
                    P.add('vector', lambda e, j=j, s=s, grp=grp, rg=rg, n=n: e.tensor_copy(out=qT[s][:, grp * 4:grp * 4 + n, j, :],
                                                                               in_=pT[:, rg, 0:n * 128].rearrange("p (u t) -> p u t", t=128)),
                          r=[('pT', bk)], w=[('qT', s)])
            for j in range(2):
                if v == 0 and t < NT:
                    P.dma('sync', dr['QTD'][:, :, r0 + j * 128:r0 + (j + 1) * 128].rearrange("(hp two) d t -> (two d) hp t", two=2), qT[s][:, 0:8, j, :],
                          r=[('qT', s)], w=['QTD'])
                P.dma('sync', dr['KTD'][:, :, k0 + j * 128:k0 + (j + 1) * 128].rearrange("(a b) d t -> (b d) a t", b=2), qT[s][:, 8:10, j, :],
                      r=[('qT', s)], w=['KTD'])
            P.dma('sync', dr['VD'][k0:k0 + 256, :, :].rearrange("(j p) g e -> p j g e", p=128), vs[s][:], r=[('vs', s)], w=['VD'])
        P.emit(ctx)


def stage3b(k, ntiles=NT):
    nc = k.nc; dr = k.dr
    with ExitStack() as ctx:
        T = lambda n, sh, dt=F32: ctx.enter_context(nc.sbuf_tensor("r_" + n, sh, dt))
        P = Prog(nc)
        ident, eps_t = consts(k, ctx, P, "r_")
        AB = load_AB(k, ctx, P, 1, "r_")
        RW = T("RW", [128, 8, 1024]); Abc = T("Abc", [128, 1024]); Bbc = T("Bbc", [128, 1024]); gbc = T("gbc", [128, 1024])
        cbc = T("cbc", [128, 8]); junk = T("junk", [128, 1024])
        P.dma('sync', Abc[:], dr['modD'][1, 0, 4096:5120].partition_broadcast(128), w=['Abc'])
        P.dma('sync', Bbc[:], dr['modD'][1, 0, 3072:4096].partition_broadcast(128), w=['Bbc'])
        P.dma('sync', gbc[:], dr['norm_g'][1, 1, :].partition_broadcast(128), w=['gbc'])
        P.add('vector', lambda e: e.scalar_tensor_tensor(out=Abc[:], in0=Abc[:], scalar=1.0, in1=gbc[:], op0=ALU.add, op1=ALU.mult), r=['gbc'], w=['Abc'])
        P.add('gpsimd', lambda e: e.memset(cbc[:], 0.0), w=['cbc'])
        for ex in range(8):
            P.dma('sync', RW[:, ex, :], dr['rwT'][ex, :].partition_broadcast(128), w=[('RW', ex)])
            P.add('vector', lambda e, ex=ex: e.scalar_tensor_tensor(out=junk[:], in0=Bbc[:], scalar=1.0, in1=RW[:, ex, :], op0=ALU.mult, op1=ALU.mult,
                                                                    accum_out=cbc[:, ex:ex + 1]), r=['Bbc', ('RW', ex), 'cbc'], w=['junk', 'cbc'])
            P.add('vector', lambda e, ex=ex: e.tensor_tensor(out=RW[:, ex, :], in0=RW[:, ex, :], in1=Abc[:], op=ALU.mult), r=['Abc'], w=[('RW', ex)])
        xt = [T(f"xt{s}", [128, 2, 1024]) for s in range(2)]
        ssq = [T(f"ssq{s}", [128, 2]) for s in range(2)]; rstd = [T(f"rstd{s}", [128, 2]) for s in range(2)]
        xn = [T(f"xn{s}", [128, 2, 1024], BF16) for s in range(2)]; hx = [T(f"hx{s}", [128, 8, 256], BF16) for s in range(2)]
        raw = [T(f"raw{s}", [128, 2, 8]) for s in range(2)]
        lg = T("lg", [128, 2, 8]); lg2 = T("lg2", [128, 2, 8]); mk1 = T("mk1", [128, 2, 8]); mk2 = T("mk2", [128, 2, 8])
        m1 = T("m1", [128, 2]); m2 = T("m2", [128, 2]); e2 = T("e2", [128, 2]); g1v = T("g1v", [128, 2]); g2v = T("g2v", [128, 2])
        gates = T("gates", [128, 32, 8])
        pT = ctx.enter_context(nc.psum_tensor("r_pT", [128, 4, 512], BF16))
        for s in range(2):
            P.add('gpsimd', lambda e, s=s: e.memset(raw[s][:], 0.0), w=[('raw', s)])
        for t in range(ntiles):
            s = t % 2; r0 = 256 * t
            P.dma('sync', xt[s][:], dr['xaD'][r0:r0 + 256, :].rearrange("(j p) d -> p j d", p=128), w=[('xt', s)])
            emit_norm_T(P, [xt[s][:, 0, :], xt[s][:, 1, :]], ('xt', s), ssq[s], rstd[s], [xn[s][:, 0, :], xn[s][:, 1, :]], eps_t, ident, pT,
                        AB[:, 0, 1, 0, :], AB[:, 0, 1, 1, :], hx[s], ('n', s))
            P.dma('sync', dr['hxD'][:, :, r0:r0 + 256].rearrange("c p t -> p c t"), hx[s][:], r=[('n', s, 'hx')], w=['hxD'])
            for j in range(2):
                for ex in range(8):
                    P.add('vector', lambda e, s=s, j=j, ex=ex: e.scalar_tensor_tensor(out=junk[:], in0=xt[s][:, j, :], scalar=1.0, in1=RW[:, ex, :],
                                                                                    op0=ALU.mult, op1=ALU.mult, accum_out=raw[s][:, j, ex:ex + 1]),
                          r=[('xt', s), ('RW', ex), ('raw', s)], w=['junk', ('raw', s)])
            V = 'vector'
            P.add(V, lambda e, s=s: e.tensor_tensor(out=lg[:], in0=raw[s][:], in1=vw(rstd[s], 0, [[1, 2], [0, 8]]), op=ALU.mult), r=[('raw', s), ('n', s, 'rstd')], w=['lg'])
            P.add('gpsimd', lambda e, s=s: e.memset(raw[s][:], 0.0), r=['lg'], w=[('raw', s)])
            P.add(V, lambda e: e.tensor_tensor(out=lg[:], in0=lg[:], in1=vw(cbc, 0, [[0, 2], [1, 8]]), op=ALU.add), r=['cbc'], w=['lg'])
            P.add(V, lambda e: e.tensor_reduce(out=m1[:], in_=lg[:], axis=AX.X, op=ALU.max), r=['lg'], w=['m1'])
            P.add(V, lambda e: e.tensor_tensor(out=mk1[:], in0=lg[:], in1=vw(m1, 0, [[1, 2], [0, 8]]), op=ALU.is_equal), r=['lg', 'm1'], w=['mk1'])
            P.add(V, lambda e: e.scalar_tensor_tensor(out=lg2[:], in0=mk1[:], scalar=-1e30, in1=lg[:], op0=ALU.mult, op1=ALU.add), r=['mk1', 'lg'], w=['lg2'])
            P.add(V, lambda e: e.tensor_reduce(out=m2[:], in_=lg2[:], axis=AX.X, op=ALU.max), r=['lg2'], w=['m2'])
            P.add(V, lambda e: e.tensor_tensor(out=mk2[:], in0=lg2[:], in1=vw(m2, 0, [[1, 2], [0, 8]]), op=ALU.is_equal), r=['lg2', 'm2'], w=['mk2'])
            P.add(V, lambda e: e.tensor_tensor(out=e2[:], in0=m2[:], in1=m1[:], op=ALU.subtract), r=['m1', 'm2'], w=['e2'])
            P.add('scalar', lambda e: e.activation(out=e2[:], in_=e2[:], func=AF.Exp), r=['e2'], w=['e2'])
            P.add(V, lambda e: e.tensor_scalar(out=g1v[:], in0=e2[:], scalar1=1.0, scalar2=None, op0=ALU.add), r=['e2'], w=['g1v'])
            P.add(V, lambda e: e.reciprocal(out=g1v[:], in_=g1v[:]), r=['g1v'], w=['g1v'])
            P.add(V, lambda e: e.tensor_tensor(out=g2v[:], in0=e2[:], in1=g1v[:], op=ALU.mult), r=['e2', 'g1v'], w=['g2v'])
            P.add(V, lambda e: e.tensor_tensor(out=mk1[:], in0=mk1[:], in1=vw(g1v, 0, [[1, 2], [0, 8]]), op=ALU.mult), r=['g1v'], w=['mk1'])
            P.add(V, lambda e: e.tensor_tensor(out=mk2[:], in0=mk2[:], in1=vw(g2v, 0, [[1, 2], [0, 8]]), op=ALU.mult), r=['g2v'], w=['mk2'])
            P.add(V, lambda e, t=t: e.tensor_tensor(out=gates[:, 2 * t:2 * t + 2, :], in0=mk1[:], in1=mk2[:], op=ALU.add), r=['mk1', 'mk2'], w=['gates'])
        P.dma('sync', dr['gatesD'][0:ntiles * 256, :].rearrange("(c p) e -> p c e", p=128), gates[:, 0:2 * ntiles, :], r=['gates'], w=['gatesD'])
        P.emit(ctx)


def stage4(k, nexp=8):
    dr = k.dr
    units = [(dr['moe_w1'][ex][:, h * 1408:(h + 1) * 1408], dr['moe_w3'][ex][:, h * 1408:(h + 1) * 1408], dr['moe_w2'][ex][h * 1408:(h + 1) * 1408, :], ex)
             for ex in range(nexp) for h in range(2)]
    tiles = [(dr['hxD'][:, :, 256 * t:256 * (t + 1)], dr['xaD'][256 * t:256 * (t + 1), :], 0, 2 * t) for t in range(NT)]
    ffn_pass(k, "m_", units, tiles, {0: dr['modD'][1, 0, 5120:6144]}, gatesD=dr['gatesD'])


def stage5(k, ntiles=NT):
    nc = k.nc; dr = k.dr
    with ExitStack() as ctx:
        T = lambda n, sh, dt=F32: ctx.enter_context(nc.sbuf_tensor("f_" + n, sh, dt))
        P = Prog(nc)
        eps_t = T("eps", [128, 1]); fg = T("fg", [128, 1024])
        P.add('gpsimd', lambda e: e.memset(eps_t[:], 1e-6), w=['eps'])
        P.dma('sync', fg[:], dr['final_g'][0, :].partition_broadcast(128), w=['fg'])
        xt = [T(f"xt{s}", [128, 2, 1024]) for s in range(2)]; ot = [T(f"ot{s}", [128, 2, 1024]) for s in range(2)]
        ssq = [T(f"ssq{s}", [128, 2]) for s in range(2)]; rstd = [T(f"rstd{s}", [128, 2]) for s in range(2)]
        for t in range(ntiles):
            s = t % 2; r0 = 256 * t
            P.dma('sync', xt[s][:], dr['xaD'][r0:r0 + 256, :].rearrange("(j p) d -> p j d", p=128), w=[('xt', s)])
            for j in range(2):
                P.add('scalar', lambda e, s=s, j=j: e.activation(out=ot[s][:, j, :], in_=xt[s][:, j, :], func=AF.Square, accum_out=ssq[s][:, j:j + 1]),
                      r=[('xt', s)], w=[('ot', s), ('ssq', s)])
            P.add('scalar', lambda e, s=s: e.activation(out=rstd[s][:], in_=ssq[s][:], func=AF.Sqrt, scale=1.0 / D, bias=eps_t[:, 0:1]),
                  r=[('ssq', s), 'eps'], w=[('rstd', s)])
            P.add('vector', lambda e, s=s: e.reciprocal(out=rstd[s][:], in_=rstd[s][:]), r=[('rstd', s)], w=[('rstd', s)])
            for j in range(2):
                P.add('vector', lambda e, s=s, j=j: e.scalar_tensor_tensor(out=ot[s][:, j, :], in0=xt[s][:, j, :], scalar=rstd[s][:, j:j + 1], in1=fg[:],
                                                                                              op0=ALU.mult, op1=ALU.mult),
                      r=[('xt', s), ('rstd', s), 'fg'], w=[('ot', s)])
            P.dma('sync', dr['out'][r0:r0 + 256, :].rearrange("(j p) d -> p j d", p=128), ot[s][:], r=[('ot', s)], w=['out'])
        P.emit(ctx)


POOLW=(2,4,8,16)
def edge_row(L, realfirst, reallast):
    inv=np.zeros((2,4,2,8),np.float32)
    for g,w in enumerate(POOLW):
        half=w//2
        for i in range(8):
            t=i; cnt=(min(t+half,L)-max(t-half,0)) if realfirst else w
            inv[0,g,:,i]=1.0/cnt
            t=L-8+i; cnt=(min(t+half,L)-max(t-half,0)) if reallast else w
            inv[1,g,:,i]=1.0/cnt
    return inv.reshape(128), [0.0 if realfirst else 1.0, 0.0 if reallast else 1.0]
def seg_quarters(r, NSEG):
    q=r%4
    if NSEG==1: return [q]
    if NSEG==4: return [q]+[o for o in range(4) if o!=q]
    m=r%2; p=(r%4)//2
    return [2*m+p, 2*m+1-p]
def core_inputs(r, inp, NSEG=1):
    b=r//4; qs=seg_quarters(r,NSEG); x=inp['x'][b]
    xin=np.zeros((NSEG,4112,1024),np.float32); inv=np.zeros((NSEG+1,128),np.float32); msk=np.zeros((1,2*(NSEG+1)),np.float32)
    for sg,q in enumerate(qs):
        lo=q*4096-8; hi=q*4096+4096+8; a=max(lo,0); z=min(hi,16384)
        xin[sg,a-lo:a-lo+(z-a)]=x[a:z]
        inv[sg],msk[0,2*sg:2*sg+2]=edge_row(16384,q==0,q==3)
    inv[NSEG],msk[0,2*NSEG:]=edge_row(256,True,True)
    ctxin=np.zeros((272,1024),np.float32); ctxin[8:264]=inp['ctx'][b]
    cvec=np.stack([inp['c'][b], inp['c_ctx']]).astype(np.float32)
    return dict(xin=xin, ctxin=ctxin, cvec=cvec, inv=inv, msk=msk)
def rope_gain(inp):
    return np.concatenate([np.tile(inp['q_norm_g'][0],16), np.tile(inp['k_norm_g'][0],4)]).astype(np.float32)[None,:]
def rope_tables(r, NSEG=1):
    n=NSEG*4096
    C=np.ones((n+256,64),np.float32); S=np.zeros((n+256,64),np.float32)
    fr=(10000.0**(-np.arange(16,dtype=np.float32)/16)).astype(np.float32)
    for sg,q in enumerate(seg_quarters(r,NSEG)):
        t=np.arange(q*4096,(q+1)*4096); row=(t//64).astype(np.float32); col=(t%64).astype(np.float32)
        ar=row[:,None]*fr; ac=col[:,None]*fr
        C[sg*4096:(sg+1)*4096]=np.concatenate([np.cos(ar),np.cos(ar),np.cos(ac),np.cos(ac)],1)
        S[sg*4096:(sg+1)*4096]=np.concatenate([-np.sin(ar),np.sin(ar),-np.sin(ac),np.sin(ac)],1)
    return {'ropeC':C,'ropeS':S}


NSEG = 4


def _build():
    k = K()
    k.din('xin', [NSEG, 4112, 1024]); k.din('ctxin', [272, 1024]); k.din('cvec', [2, 1024]); k.din('inv', [NSEG + 1, 128]); k.din('msk', [1, 2 * (NSEG + 1)])
    k.din('ada_w', [2, 1024, 6144]); k.din('ada_b', [2, 6144]); k.din('norm_g', [2, 2, 1024]); k.din('pool_w', [4, 256, 256]); k.din('pool_scale', [1, 1024])
    k.din('ffn_w1', [1024, 2816]); k.din('ffn_w3', [1024, 2816]); k.din('ffn_w2', [2816, 1024])
    k.din('w_qkv', [1024, 1536]); k.din('qkg', [1, 1280]); k.din('ropeC', [NSEG * 4096 + 256, 64]); k.din('ropeS', [NSEG * 4096 + 256, 64])
    k.din('w_o', [1024, 1024]); k.din('rwT', [8, 1024]); k.din('final_g', [1, 1024])
    k.din('moe_w1', [8, 1024, 2816]); k.din('moe_w3', [8, 1024, 2816]); k.din('moe_w2', [8, 2816, 1024])
    k.dint('modD', [2, 2, 6144]); k.dint('xaD', [NSEG * 4096, 1024]); k.dint('caD', [256, 1024])
    k.dint('hxD', [8, 128, NSEG * 4096], BF16); k.dint('hcD', [8, 128, 256], BF16)
    k.dint('QTD', [16, 64, 4096], BF16); k.dint('KTD', [4, 64, NSEG * 4096 + 256], BF16); k.dint('VD', [NSEG * 4096 + 256, 4, 65], BF16)
    k.dint('gatesD', [4096, 8]); k.dout('out', [4096, 1024])
    stage0(k); stage1a(k, NSEG=NSEG); stage1b(k, NSEG=NSEG); stage2(k, NSEG=NSEG)
    stage3(k, NKC=(NSEG * 4096 + 256) // 128); stage3b(k); stage4(k); stage5(k)
    return k.nc


def kernel(x, c, ctx, c_ctx, ada_w, ada_b, norm_g, pool_w, pool_scale, ffn_w1, ffn_w3, ffn_w2,
           w_qkv, w_o, q_norm_g, k_norm_g, router_w, moe_w1, moe_w3, moe_w2, final_g):
    f = lambda a: np.ascontiguousarray(np.asarray(a, dtype=np.float32))
    inp = dict(x=f(x), c=f(c), ctx=f(ctx), c_ctx=f(c_ctx), q_norm_g=f(q_norm_g), k_norm_g=f(k_norm_g))
    shared = dict(ada_w=f(ada_w), ada_b=f(ada_b), norm_g=f(norm_g), pool_w=f(pool_w)[0], pool_scale=f(pool_scale),
                  ffn_w1=f(ffn_w1)[0], ffn_w3=f(ffn_w3)[0], ffn_w2=f(ffn_w2)[0], w_qkv=f(w_qkv)[0], qkg=rope_gain(inp),
                  w_o=f(w_o)[0], rwT=np.ascontiguousarray(f(router_w)[0].T), final_g=f(final_g)[None, :],
                  moe_w1=f(moe_w1)[0], moe_w3=f(moe_w3)[0], moe_w2=f(moe_w2)[0])
    maps = [{**core_inputs(r, inp, NSEG), **rope_tables(r, NSEG), **shared} for r in range(8)]
    res = run_bass_kernel_spmd(_build(), maps, core_ids=list(range(8))).results
    out = np.stack([np.concatenate([np.asarray(res[4 * b + q]['out']) for q in range(4)], axis=0) for b in range(2)])
    return out.astype(np.float32)
```

```python
import numpy as np, os
DBG=os.environ.get('DBG','')
from contextlib import ExitStack
import concourse.bass as bass
import concourse.mybir as mybir
from concourse.bass_utils import run_bass_kernel_spmd

F32 = mybir.dt.float32; BF16 = mybir.dt.bfloat16
AF = mybir.ActivationFunctionType; ALU = mybir.AluOpType; AX = mybir.AxisListType

D = 1024; DFF = 2816; NF = 22; SEQ = 16384; CTX = 256; TPC = 4096; TT = 256; NT = TPC // TT
ENGS = ['sync', 'scalar', 'vector', 'gpsimd', 'tensor']
NDSEM = 8
SAME_SYNC = {'gpsimd', 'scalar', 'vector'}
_uid = [0]


class _Op:
    __slots__ = ('eng', 'fn', 'deps', 'dma', 'signal', 'sigval', 'dsem', 'dval', 'prev_dma')

    def __init__(s, eng, fn, dma):
        s.eng = eng; s.fn = fn; s.dma = dma; s.deps = []; s.signal = False; s.sigval = 0
        s.dsem = None; s.dval = 0; s.prev_dma = None


class Prog:
    def __init__(s, nc):
        s.nc = nc; s.ops = {e: [] for e in ENGS}; s.lastw = {}; s.readers = {}
        s.dma_count = {e: 0 for e in ENGS}; s.dma_hist = {e: [] for e in ENGS}

    def add(s, eng, fn, r=(), w=(), dma=False):
        op = _Op(eng, fn, dma); deps = []
        for k in r:
            d = s.lastw.get(k)
            if d is not None: deps.append(d)
        for k in w:
            d = s.lastw.get(k)
            if d is not None: deps.append(d)
            deps.extend(s.readers.get(k, ()))
        seen = set()
        for d in deps:
            if id(d) in seen: continue
            seen.add(id(d))
            if d.dma or d.eng != eng or eng in SAME_SYNC:
                op.deps.append(d)
                if not d.dma: d.signal = True
        for k in w:
            s.lastw[k] = op; s.readers[k] = []
        for k in r:
            s.readers.setdefault(k, []).append(op)
        if dma:
            n = s.dma_count[eng]; s.dma_count[eng] = n + 1
            op.dsem = n % NDSEM; op.dval = 16 * (n // NDSEM + 1)
            h = s.dma_hist[eng]
            if n >= NDSEM: op.prev_dma = h[n - NDSEM]
            h.append(op)
        s.ops[eng].append(op)
        return op

    def dma(s, eng, out, in_, r=(), w=(), **kw):
        return s.add(eng, lambda e: e.dma_start(out=out, in_=in_, **kw), r=r, w=w, dma=True)

    def emit(s, ctx):
        nc = s.nc; _uid[0] += 1; u = _uid[0]
        esem = {e: nc.alloc_semaphore(name=f"es{u}_{e}") for e in ENGS}
        dsem = {e: [nc.alloc_semaphore(name=f"ds{u}_{e}{i}") for i in range(NDSEM)]
                for e in ENGS if s.dma_count[e] > 0}
        allsems = list(esem.values()) + [h for l in dsem.values() for h in l]

        def _cleanup():
            nc.clear_and_free_semaphores(allsems)
            nc.all_engine_barrier()
        ctx.callback(_cleanup)
        for e in ENGS:
            c = 0
            for op in s.ops[e]:
                if op.signal and not op.dma:
                    c += 1; op.sigval = c
        block = ctx.enter_context(nc.Block())

        def mk(ename):
            def body(e):
                waited = {}

                def wait(sem, val, key):
                    if waited.get(key, 0) < val:
                        e.wait_ge(sem, val); waited[key] = val
                for op in s.ops[ename]:
                    for d in op.deps:
                        if d.dma: wait(dsem[d.eng][d.dsem], d.dval, ('d', d.eng, d.dsem))
                        else: wait(esem[d.eng], d.sigval, ('e', d.eng))
                    if op.dma and op.prev_dma is not None:
                        d = op.prev_dma; wait(dsem[d.eng][d.dsem], d.dval, ('d', d.eng, d.dsem))
                    ins = op.fn(e)
                    if op.dma: ins.then_inc(dsem[ename][op.dsem], 16)
                    elif op.signal: ins.then_inc(esem[ename], 1)
                if ename == 'sync':
                    for q in dsem:
                        for d in s.dma_hist[q][-NDSEM:]:
                            wait(dsem[q][d.dsem], d.dval, ('d', q, d.dsem))
            return body
        for ename in ENGS:
            if s.ops[ename] or ename == 'sync':
                getattr(block, ename)(mk(ename))


class Rec:
    def __init__(s): s.l = []; s.cur = None

    def _put(s, op):
        if s.cur is not None: s.cur.append(op)
        else: s.l.append([op])

    def add(s, *a, **k): s._put(('add', a, k))

    def dma(s, *a, **k): s._put(('dma', a, k))

    def atom(s):
        rec = s

        class _A:
            def __enter__(self_):
                assert rec.cur is None; rec.cur = []

            def __exit__(self_, *a):
                rec.l.append(rec.cur); rec.cur = None
        return _A()


def interleave(P, recs, W):
    active = {}; nxt = 0
    while nxt < len(recs) or active:
        while nxt < len(recs) and (nxt % W) not in active:
            active[nxt % W] = iter(recs[nxt].l); nxt += 1
        for sl in sorted(active, key=lambda q: q):
            unit = next(active[sl], None)
            if unit is None:
                del active[sl]; continue
            for op in unit:
                getattr(P, op[0])(*op[1], **op[2])


class _NoAtom:
    def __enter__(s): pass

    def __exit__(s, *a): pass


def atom_of(R):
    return R.atom() if isinstance(R, Rec) else _NoAtom()


class K:
    def __init__(s):
        s.nc = bass.Bass("TRN2", target_bir_lowering=False)
        s.dr = {}

    def din(s, name, shape, dt=F32):
        s.dr[name] = s.nc.dram_tensor(name, list(shape), dt, kind="ExternalInput").ap(); return s.dr[name]

    def dout(s, name, shape, dt=F32):
        s.dr[name] = s.nc.dram_tensor(name, list(shape), dt, kind="ExternalOutput").ap(); return s.dr[name]

    def dint(s, name, shape, dt=F32):
        s.dr[name] = s.nc.dram_tensor(name, list(shape), dt, kind="Internal").ap(); return s.dr[name]


def stage0(k):
    nc = k.nc; dr = k.dr
    with ExitStack() as ctx:
        T = lambda n, sh, dt=F32: ctx.enter_context(nc.sbuf_tensor("s0_" + n, sh, dt))
        cT = T("cT", [128, 8, 2]); scT = T("scT", [128, 8, 2])
        awt = [T(f"aw{i}", [128, 8, 512]) for i in range(2)]
        adab = T("adab", [2, 2, 6144]); modsb = T("mod", [2, 2, 6144])
        pm = [ctx.enter_context(nc.psum_tensor(f"s0_pm{i}", [128, 512], F32)) for i in range(2)]
        P = Prog(nc)
        for v in range(2):
            P.dma('sync', cT[:, :, v], dr['cvec'][v, :].rearrange("(c p) -> p c", p=128), w=['cT'], allow_slow_non_contiguous=True)
        for i in range(2):
            P.dma('gpsimd', adab[:, i, :], dr['ada_b'][i, :].partition_broadcast(2), w=[('adab', i)])
        P.add('scalar', lambda e: e.activation(out=scT[:], in_=cT[:], func=AF.Silu), r=['cT'], w=['scT'])
        n = 0
        for i in range(2):
            for cb in range(12):
                sl = n % 2; n += 1
                P.dma('sync', awt[sl][:], dr['ada_w'][i, :, cb * 512:(cb + 1) * 512].rearrange("(c p) n -> p c n", p=128),
                      w=[('aw', sl)])
                for kc in range(8):
                    P.add('tensor', lambda e, sl=sl, kc=kc: e.matmul(pm[sl][0:2, :], lhsT=scT[:, kc, :], rhs=awt[sl][:, kc, :],
                                                                     start=(kc == 0), stop=(kc == 7)),
                          r=['scT', ('aw', sl)], w=[('pm', sl)])
                P.add('vector', lambda e, sl=sl, i=i, cb=cb: e.tensor_tensor(out=modsb[:, i, cb * 512:(cb + 1) * 512], in0=pm[sl][0:2, :],
                                                                            in1=adab[:, i, cb * 512:(cb + 1) * 512], op=ALU.add),
                      r=[('pm', sl), ('adab', i)], w=['modsb'])
        P.dma('sync', dr['modD'].rearrange("i v n -> v i n"), modsb[:], r=['modsb'], w=['modD'])
        P.emit(ctx)


def load_AB(k, ctx, P, layer, pref):
    nc = k.nc; dr = k.dr
    T = lambda n, sh, dt=F32: ctx.enter_context(nc.sbuf_tensor(pref + n, sh, dt))
    modF = T("modF", [128, 2, 48]); gF = T("gF", [128, 2, 8]); AB = T("AB", [128, 2, 2, 2, 8])
    for v in range(2):
        P.dma('sync', modF[:, v, :], dr['modD'][layer, v, :].rearrange("(kc p) -> p kc", p=128), w=[('modF', v)],
              allow_slow_non_contiguous=True)
    for sub in range(2):
        P.dma('sync', gF[:, sub, :], dr['norm_g'][layer, sub, :].rearrange("(c p) -> p c", p=128), w=['gF'], allow_slow_non_contiguous=True)
    for v in range(2):
        for sub in range(2):
            sh = modF[:, v, (3 * sub) * 8:(3 * sub) * 8 + 8]; sc = modF[:, v, (3 * sub + 1) * 8:(3 * sub + 1) * 8 + 8]
            P.add('vector', lambda e, v=v, sub=sub, sc=sc: e.scalar_tensor_tensor(out=AB[:, v, sub, 0, :], in0=sc, scalar=1.0, in1=gF[:, sub, :],
                                                                              op0=ALU.add, op1=ALU.mult),
                  r=[('modF', v), 'gF'], w=['AB'])
            P.add('vector', lambda e, v=v, sub=sub, sh=sh: e.tensor_copy(out=AB[:, v, sub, 1, :], in_=sh), r=[('modF', v)], w=['AB'])
    return AB


def consts(k, ctx, P, pref):
    nc = k.nc
    ident = ctx.enter_context(nc.sbuf_tensor(pref + "ident", [128, 128], BF16))
    eps_t = ctx.enter_context(nc.sbuf_tensor(pref + "eps", [128, 1], F32))
    P.add('gpsimd', lambda e: e.memset(ident[:], 0.0), w=['ident'])
    P.add('gpsimd', lambda e: e.affine_select(out=ident[:], in_=ident[:], pattern=[[-1, 128]], compare_op=ALU.not_equal, fill=1.0,
                                              base=0, channel_multiplier=1), w=['ident'])
    P.add('gpsimd', lambda e: e.memset(eps_t[:], 1e-6), w=['eps'])
    return ident, eps_t


def norm_ops(P, xt_chunks, xkey, ssq, rstd, xn_chunks, eps_t, s):
    n = len(xt_chunks)
    for j, (xc, xo) in enumerate(zip(xt_chunks, xn_chunks)):
        np_ = xc.shape[0]
        P.add('scalar', lambda e, xc=xc, xo=xo, j=j, np_=np_: e.activation(out=xo, in_=xc, func=AF.Square, accum_out=ssq[0:np_, j:j + 1]),
              r=[xkey], w=[('xn', s), ('ssq', s)])
    P.add('scalar', lambda e: e.activation(out=rstd[:, 0:n], in_=ssq[:, 0:n], func=AF.Sqrt, scale=1.0 / D, bias=eps_t[:, 0:1]),
          r=[('ssq', s), 'eps'], w=[('rstd', s)])
    P.add('vector', lambda e: e.reciprocal(out=rstd[:, 0:n], in_=rstd[:, 0:n]), r=[('rstd', s)], w=[('rstd', s)])
    for j, (xc, xo) in enumerate(zip(xt_chunks, xn_chunks)):
        np_ = xc.shape[0]
        P.add('gpsimd', lambda e, xc=xc, xo=xo, j=j, np_=np_: e.tensor_scalar(out=xo, in0=xc, scalar1=rstd[0:np_, j:j + 1], scalar2=None, op0=ALU.mult),
              r=[xkey, ('rstd', s)], w=[('xn', s)])


def emit_norm_T(P, xcs, xkey, ssq, rstd, xos, eps_t, ident, pT, A, B, hx, nk):
    n = len(xcs)
    for j in range(n):
        P.add('scalar', lambda e, j=j: e.activation(out=xos[j], in_=xcs[j], func=AF.Square, accum_out=ssq[:, j:j + 1]),
              r=[xkey], w=[nk + ('xn',), nk + ('ssq',)])
    P.add('scalar', lambda e: e.activation(out=rstd[:, 0:n], in_=ssq[:, 0:n], func=AF.Sqrt, scale=1.0 / D, bias=eps_t[:, 0:1]),
          r=[nk + ('ssq',), 'eps'], w=[nk + ('rstd',)])
    P.add('vector', lambda e: e.reciprocal(out=rstd[:, 0:n], in_=rstd[:, 0:n]), r=[nk + ('rstd',)], w=[nk + ('rstd',)])
    for j in range(n):
        P.add('scalar', lambda e, j=j: e.activation(out=xos[j], in_=xcs[j], func=AF.Identity, scale=rstd[:, j:j + 1]),
              r=[xkey, nk + ('rstd',)], w=[nk + ('xn',)])
    for pr in range(4):
        bk = pr % 2
        with atom_of(P):
            for fc in (2 * pr, 2 * pr + 1):
                q = fc % 4
                for j in range(n):
                    P.add('tensor', lambda e, q=q, j=j, fc=fc: e.transpose(out=pT[:, q, j * 128:(j + 1) * 128], in_=xos[j][:, fc * 128:(fc + 1) * 128],
                                                                           identity=ident[:]),
                          r=[nk + ('xn',), 'ident'], w=[('pT', bk)])
            for fc in (2 * pr, 2 * pr + 1):
                q = fc % 4
                P.add('scalar', lambda e, fc=fc, q=q: e.activation(out=hx[:, fc, 0:128 * n], in_=pT[:, q, 0:128 * n], func=AF.Identity,
                                                                    scale=A[:, fc:fc + 1], bias=B[:, fc:fc + 1]),
                      r=[('pT', bk), 'AB'], w=[nk + ('hx',)])

def stage1a(k, upto=99, ntiles=None, NSEG=1, W=int(os.environ.get('W1A', '3'))):
    nc = k.nc; dr = k.dr
    with ExitStack() as ctx:
        T = lambda n, sh, dt=F32: ctx.enter_context(nc.sbuf_tensor("a_" + n, sh, dt))
        P = Prog(nc)
        ident, eps_t = consts(k, ctx, P, "a_")
        AB = load_AB(k, ctx, P, 0, "a_")
        gp_bc = [T(f"gpbc{v}", [128, 1024]) for v in range(2)]; ps_bc = T("psbc", [128, 1024])
        P.dma('sync', ps_bc[:], dr['pool_scale'][0, :].partition_broadcast(128), w=['psbc'])
        for v in range(2):
            P.dma('sync', gp_bc[v][:], dr['modD'][0, v, 2048:3072].partition_broadcast(128), w=[('gpbc', v)])
            P.add('gpsimd', lambda e, v=v: e.tensor_tensor(out=gp_bc[v][:], in0=gp_bc[v][:], in1=ps_bc[:], op=ALU.mult),
                  r=['psbc'], w=[('gpbc', v)])
        inv = T("inv", [128, NSEG + 1, 2, 4, 2, 8]); msk = T("msk", [128, 2 * (NSEG + 1)])
        for sg in range(NSEG + 1):
            P.dma('sync', inv[:, sg].rearrange("p b c d e -> p (b c d e)"), dr['inv'][sg, :].partition_broadcast(128), w=['inv'])
        P.dma('sync', msk[:], dr['msk'][0, :].partition_broadcast(128), w=['msk'])
        wg = T("wg", [128, 4, 2, 256], BF16)
        P.dma('gpsimd', wg[:], dr['pool_w'].rearrange("g (c p) e -> p g c e", p=128), w=['wg'])
        xt = [T(f"xt{s}", [128, 2, 1024]) for s in range(W)]; xh = [T(f"xh{s}", [128, 1024]) for s in range(W)]
        ssq = [T(f"ssq{s}", [128, 3]) for s in range(W)]; rstd = [T(f"rstd{s}", [128, 3]) for s in range(W)]
        xn = [T(f"xn{s}", [128, 3, 1024], BF16) for s in range(W)]
        hxT = [T(f"hxT{s}", [128, 8, 272], BF16) for s in range(W)]
        SA = [T(f"SA{s}", [128, 2, 272]) for s in range(W)]; SB = [T(f"SB{s}", [128, 2, 272]) for s in range(W)]
        te = [T(f"te{s}", [128, 2, 8]) for s in range(W)]
        dT = [T(f"dT{s}", [128, 8, 256], BF16) for s in range(W)]
        tmp = [T(f"tmp{s}", [128, 1024]) for s in range(2)]
        ssq2 = [T(f"ssq2{s}", [128, 2]) for s in range(W)]; rstd2 = [T(f"rstd2{s}", [128, 2]) for s in range(W)]
        xn2 = [T(f"xn2{s}", [128, 2, 1024], BF16) for s in range(W)]; hx2 = [T(f"hx2{s}", [128, 8, 256], BF16) for s in range(W)]
        pT = ctx.enter_context(nc.psum_tensor("a_pT", [128, 4, 512], BF16))
        pD = [ctx.enter_context(nc.psum_tensor(f"a_pD{s}", [128, 1024], F32)) for s in range(2)]
        for s in range(W):
            P.add('gpsimd', lambda e, s=s: e.memset(ssq[s][:], 1.0), w=[('ssq', s)])
            P.add('gpsimd', lambda e, s=s: e.memset(xh[s][:], 0.0), w=[('xh', s)])
            P.add('gpsimd', lambda e, s=s: e.memset(xn[s][:, 2, :], 0.0), w=[('xn', s)])
        tiles = [(0, sg, t) for sg in range(NSEG) for t in range(NT)] + [(1, NSEG, 0)]
        if ntiles: tiles = tiles[:ntiles]
        recs = []
        for ti, (v, sg, t) in enumerate(tiles):
            s = ti % W
            R = Rec(); recs.append(R)
            src = dr['xin'][sg] if v == 0 else dr['ctxin']; dst = dr['xaD'][sg * 4096:(sg + 1) * 4096, :] if v == 0 else dr['caD']
            r0 = 256 * t
            R.dma('sync', xt[s][:], src[r0 + 8:r0 + 264, :].rearrange("(j p) d -> p j d", p=128), w=[('xt', s)])
            R.dma('sync', xh[s][0:8, :], src[r0:r0 + 8, :], w=[('xh', s)])
            R.dma('sync', xh[s][32:40, :], src[r0 + 264:r0 + 272, :], w=[('xh', s)])
            xcs = [xt[s][:, 0, :], xt[s][:, 1, :], xh[s][:, :]]; xos = [xn[s][:, 0, :], xn[s][:, 1, :], xn[s][:, 2, :]]
            for j in range(3):
                np_ = xcs[j].shape[0]
                R.add('scalar', lambda e, j=j, np_=np_, s=s, xcs=xcs, xos=xos: e.activation(out=xos[j], in_=xcs[j], func=AF.Square,
                                                                                      accum_out=ssq[s][0:np_, j:j + 1]),
                      r=[('xt', s), ('xh', s)], w=[('xn', s), ('ssq', s)])
            R.add('scalar', lambda e, s=s: e.activation(out=rstd[s][:], in_=ssq[s][:], func=AF.Sqrt, scale=1.0 / D, bias=eps_t[:, 0:1]),
                  r=[('ssq', s), 'eps'], w=[('rstd', s)])
            R.add('vector', lambda e, s=s: e.reciprocal(out=rstd[s][:], in_=rstd[s][:]), r=[('rstd', s)], w=[('rstd', s)])
            for j in range(3):
                np_ = xcs[j].shape[0]
                R.add('scalar', lambda e, j=j, np_=np_, s=s, xcs=xcs, xos=xos: e.activation(out=xos[j], in_=xcs[j], func=AF.Identity,
                                                                                      scale=rstd[s][0:np_, j:j + 1]),
                      r=[('xt', s), ('xh', s), ('rstd', s)], w=[('xn', s)])
            if upto < 1: continue
            for pr in range(4):
              bk = pr % 2
              with R.atom():
                for fc in (2 * pr, 2 * pr + 1):
                    q = fc % 4
                    for jj in range(3):
                        R.add('tensor', lambda e, s=s, q=q, jj=jj, fc=fc: e.transpose(out=pT[:, q, jj * 128:(jj + 1) * 128],
                                                                                 in_=xn[s][:, jj, fc * 128:(fc + 1) * 128], identity=ident[:]),
                              r=[('xn', s), 'ident'], w=[('pT', bk)])
                for fc in (2 * pr, 2 * pr + 1):
                    q = fc % 4
                    A_ = AB[:, v, 0, 0, fc:fc + 1]; B_ = AB[:, v, 0, 1, fc:fc + 1]
                    R.add('scalar', lambda e, s=s, fc=fc, q=q, A_=A_, B_=B_: e.activation(out=hxT[s][:, fc, 8:264], in_=pT[:, q, 0:256], func=AF.Identity,
                                                                                      scale=A_, bias=B_),
                          r=[('pT', bk), 'AB'], w=[('hxT', s)])
                    for (o0, i0) in ((0, 256), (264, 288)):
                        R.add('vector', lambda e, s=s, fc=fc, q=q, A_=A_, B_=B_, o0=o0, i0=i0: e.tensor_scalar(out=hxT[s][:, fc, o0:o0 + 8], in0=pT[:, q, i0:i0 + 8],
                                                                                                     scalar1=A_, scalar2=B_, op0=ALU.mult, op1=ALU.add),
                              r=[('pT', bk), 'AB'], w=[('hxT', s)])
            if upto < 2: continue
            first = (t == 0); last = (v == 1) or (t == NT - 1)
            if first:
                R.add('vector', lambda e, s=s, sg=sg: e.tensor_scalar(out=hxT[s][:, :, 0:8], in0=hxT[s][:, :, 0:8], scalar1=msk[:, 2 * sg:2 * sg + 1],
                                                                     scalar2=None, op0=ALU.mult), r=['msk'], w=[('hxT', s)])
            if last:
                R.add('vector', lambda e, s=s, sg=sg: e.tensor_scalar(out=hxT[s][:, :, 264:272], in0=hxT[s][:, :, 264:272], scalar1=msk[:, 2 * sg + 1:2 * sg + 2],
                                                                     scalar2=None, op0=ALU.mult), r=['msk'], w=[('hxT', s)])
            for g in range(4):
                h = hxT[s][:, 2 * g:2 * g + 2, :]
                bufs = [SA[s], SB[s]]
                R.add('vector', lambda e, s=s, h=h: e.tensor_tensor(out=SA[s][:, :, 1:272], in0=h[:, :, 0:271], in1=h[:, :, 1:272], op=ALU.add),
                      r=[('hxT', s)], w=[('S', s)])
                cur = 0
                steps = [(1, 2, 271), (2, 4, 269), (4, 8, 265)]
                for (sh_, lo, hi) in steps[:g]:
                    a = bufs[cur]; b = bufs[1 - cur]
                    R.add('vector', lambda e, a=a, b=b, sh_=sh_, lo=lo, hi=hi: e.tensor_tensor(out=b[:, :, lo:hi], in0=a[:, :, lo - sh_:hi - sh_],
                                                                                         in1=a[:, :, lo + sh_:hi + sh_], op=ALU.add),
                          r=[('S', s)], w=[('S', s)])
                    cur = 1 - cur
                Sf = bufs[cur]; wv = 1.0 / (2 << g)
                R.add('vector', lambda e, s=s, g=g, Sf=Sf, h=h, wv=wv: e.scalar_tensor_tensor(out=dT[s][:, 2 * g:2 * g + 2, :], in0=Sf[:, :, 8:264], scalar=wv,
                                                                                         in1=h[:, :, 8:264], op0=ALU.mult, op1=ALU.subtract),
                      r=[('S', s), ('hxT', s)], w=[('dT', s)])
                for (flag, eidx, c0) in ((first, 0, 0), (last, 1, 248)):
                    if not flag: continue
                    R.add('vector', lambda e, s=s, g=g, Sf=Sf, sg=sg, eidx=eidx, c0=c0: e.tensor_tensor(out=te[s][:], in0=Sf[:, :, 8 + c0:16 + c0],
                                                                                                in1=inv[:, sg, eidx, g, :, :], op=ALU.mult),
                          r=[('S', s), 'inv'], w=[('te', s)])
                    R.add('vector', lambda e, s=s, g=g, h=h, c0=c0: e.tensor_tensor(out=dT[s][:, 2 * g:2 * g + 2, c0:c0 + 8], in0=te[s][:],
                                                                                in1=h[:, :, 8 + c0:16 + c0], op=ALU.subtract),
                          r=[('te', s), ('hxT', s)], w=[('dT', s)])
            if upto < 3: continue
            for j in range(2):
              with R.atom():
                for g in range(4):
                    for c in range(2):
                        R.add('tensor', lambda e, s=s, j=j, g=g, c=c: e.matmul(pD[j][:, g * 256:(g + 1) * 256], lhsT=dT[s][:, 2 * g + c, j * 128:(j + 1) * 128],
                                                                              rhs=wg[:, g, c, :], start=(c == 0), stop=(c == 1)),
                              r=[('dT', s), 'wg'], w=[('pD', j)])
                R.add('vector', lambda e, s=s, j=j, v=v: e.tensor_tensor(out=tmp[j][:], in0=pD[j][:], in1=gp_bc[v][:], op=ALU.mult),
                      r=[('pD', j), ('gpbc', v)], w=[('tmp', j)])
                R.add('vector', lambda e, s=s, j=j: e.tensor_tensor(out=xt[s][:, j, :], in0=xt[s][:, j, :], in1=tmp[j][:], op=ALU.add),
                      r=[('tmp', j)], w=[('xt', s)])
            R.dma('sync', dst[r0:r0 + 256, :].rearrange("(j p) d -> p j d", p=128), xt[s][:], r=[('xt', s)], w=['xaD'])
            hdst = dr['hxD'][:, :, sg * 4096:(sg + 1) * 4096] if v == 0 else dr['hcD']
            emit_norm_T(R, [xt[s][:, 0, :], xt[s][:, 1, :]], ('xt', s), ssq2[s], rstd2[s], [xn2[s][:, 0, :], xn2[s][:, 1, :]], eps_t, ident, pT,
                        AB[:, v, 1, 0, :], AB[:, v, 1, 1, :], hx2[s], ('n2', s))
            R.dma('sync', hdst[:, :, r0:r0 + 256].rearrange("c p t -> p c t"), hx2[s][:], r=[('n2', s, 'hx')], w=['hxD'])
        interleave(P, recs, W)
        P.emit(ctx)


def ffn_pass(k, pref, units, tiles, g2_specs, gatesD=None):
    nc = k.nc; dr = k.dr; HF = 11; HW = 1408
    with ExitStack() as ctx:
        T = lambda n, sh, dt=F32: ctx.enter_context(nc.sbuf_tensor(pref + n, sh, dt))
        P = Prog(nc)
        w1b = [T(f"w1b{i}", [128, 8, HW], BF16) for i in range(2)]; w3b = [T(f"w3b{i}", [128, 8, HW], BF16) for i in range(2)]
        w2b = [T(f"w2b{i}", [128, HF, 1024], BF16) for i in range(2)]
        g2bc = {v: T(f"g2bc{v}", [128, 1024]) for v in g2_specs}
        for v, src in g2_specs.items():
            P.dma('sync', g2bc[v][:], src.partition_broadcast(128), w=[('g2bc', v)])
        gates = None
        if gatesD is not None:
            gates = T("gates", [128, 32, 8])
            P.dma('sync', gates[:], gatesD.rearrange("(c p) e -> p c e", p=128), w=['gates'])
        hx = [T(f"hx{i}", [128, 8, 256], BF16) for i in range(3)]
        gT = T("gT", [128, HF, 256], BF16); sil = [T(f"sil{i}", [128, 256], BF16) for i in range(2)]
        tmp = [T(f"tmp{i}", [128, 512]) for i in range(4)]
        ph = [ctx.enter_context(nc.psum_tensor(f"{pref}ph{i}", [128, 2, 256], F32)) for i in range(2)]
        pD = [ctx.enter_context(nc.psum_tensor(f"{pref}pD{i}", [128, 512], F32)) for i in range(2)]
        nh = 0; nd = 0
        def loadw(u):
            (w1, w3, w2, ex) = units[u]; b = u % 2
            for dc in range(8):
                P.dma('gpsimd', w1b[b][:, dc, :], w1[dc * 128:(dc + 1) * 128, :], w=[('w1', b, dc)])
                P.dma('gpsimd', w3b[b][:, dc, :], w3[dc * 128:(dc + 1) * 128, :], w=[('w3', b, dc)])
            for f in range(HF):
                P.dma('gpsimd', w2b[b][:, f, :], w2[f * 128:(f + 1) * 128, :], w=[('w2', b, f)])
        loadw(0)
        for u, (w1, w3, w2, ex) in enumerate(units):
            b = u % 2
            if u + 1 < len(units): loadw(u + 1)
            for (hsrc, odst, v, ch0) in tiles:
                hs = nh % 3; nh += 1
                P.dma('sync', hx[hs][:], hsrc.rearrange("c p t -> p c t"), w=[('hx', hs)])
                for f in range(HF):
                    pb = f % 2
                    for (wb, wk, col) in ((w1b, 'w1', 0), (w3b, 'w3', 1)):
                        for dc in range(8):
                            P.add('tensor', lambda e, wb=wb, b=b, dc=dc, f=f, pb=pb, col=col, hs=hs: e.matmul(
                                ph[pb][:, col, :], lhsT=wb[b][:, dc, f * 128:(f + 1) * 128], rhs=hx[hs][:, dc, :], start=(dc == 0), stop=(dc == 7)),
                                r=[(wk, b, dc), ('hx', hs)], w=[('ph', pb)])
                    P.add('scalar', lambda e, pb=pb: e.activation(out=sil[pb][:], in_=ph[pb][:, 0, :], func=AF.Silu), r=[('ph', pb)], w=[('sil', pb)])
                    P.add('vector', lambda e, pb=pb, f=f: e.tensor_tensor(out=gT[:, f, :], in0=sil[pb][:], in1=ph[pb][:, 1, :], op=ALU.mult),
                          r=[('sil', pb), ('ph', pb)], w=[('gT', f)])
                for j in range(2):
                    for half in range(2):
                        db = nd % 2; ts = nd % 4; nd += 1
                        for f in range(HF):
                            P.add('tensor', lambda e, db=db, f=f, j=j, half=half, b=b: e.matmul(
                                pD[db][:], lhsT=gT[:, f, j * 128:(j + 1) * 128], rhs=w2b[b][:, f, half * 512:(half + 1) * 512], start=(f == 0), stop=(f == HF - 1)),
                                r=[('gT', f), ('w2', b, f)], w=[('pD', db)])
                        if gates is None:
                            P.add('vector', lambda e, db=db, ts=ts, half=half, v=v: e.tensor_tensor(out=tmp[ts][:], in0=pD[db][:],
                                                                                           in1=g2bc[v][:, half * 512:(half + 1) * 512], op=ALU.mult),
                                  r=[('pD', db), ('g2bc', v)], w=[('tmp', ts)])
                        else:
                            P.add('vector', lambda e, db=db, ts=ts, half=half, v=v, c=ch0 + j, ex=ex: e.scalar_tensor_tensor(
                                out=tmp[ts][:], in0=pD[db][:], scalar=gates[:, c, ex:ex + 1], in1=g2bc[v][:, half * 512:(half + 1) * 512],
                                op0=ALU.mult, op1=ALU.mult), r=[('pD', db), ('g2bc', v), 'gates'], w=[('tmp', ts)])
                        P.dma('gpsimd', odst[j * 128:(j + 1) * 128, half * 512:(half + 1) * 512], tmp[ts][:], r=[('tmp', ts)],
                              w=[('out', odst.name, ch0 + j, half)], accum_op=ALU.add)
        P.emit(ctx)


def stage1b(k, NSEG=1):
    dr = k.dr
    units = [(dr['ffn_w1'][:, h * 1408:(h + 1) * 1408], dr['ffn_w3'][:, h * 1408:(h + 1) * 1408], dr['ffn_w2'][h * 1408:(h + 1) * 1408, :], 0)
             for h in range(2)]
    tiles = [(dr['hxD'][:, :, 256 * t:256 * (t + 1)], dr['xaD'][256 * t:256 * (t + 1), :], 0, 2 * t) for t in range(NT * NSEG)]
    tiles.append((dr['hcD'][:, :, :], dr['caD'][:, :], 1, 2 * NT * NSEG))
    ffn_pass(k, "b_", units, tiles, {0: dr['modD'][0, 0, 5120:6144], 1: dr['modD'][0, 1, 5120:6144]})


def stage3(k, NQB=8, NKC=130, A_LIST=(0, 1)):
    nc = k.nc; dr = k.dr; NK = NKC * 128
    with ExitStack() as ctx:
        T = lambda n, sh, dt=F32: ctx.enter_context(nc.sbuf_tensor("c_" + n, sh, dt))
        P = Prog(nc)
        KT = T("KT", [128, NK], BF16); V = T("V", [128, NKC, 2, 65], BF16)
        wo = T("wo", [64, 16, 1024], BF16); wof = T("wof", [64, 2, 1024]); g1bc = T("g1bc", [64, 1024])
        QT = [T(f"QT{i}", [128, 4, 512], BF16) for i in range(2)]
        PT = [T(f"PT{i}", [128, 1024], BF16) for i in range(3)]
        oS = T("oS", [65, 2, 512]); attnT = T("attnT", [64, 8, 512], BF16)
        ones = T("ones", [65, 64]); tmp = [T(f"tmp{i}", [128, 512]) for i in range(2)]
        ps = [ctx.enter_context(nc.psum_tensor(f"c_ps{i}", [128, 1024], F32)) for i in range(2)]
        po = [ctx.enter_context(nc.psum_tensor(f"c_po{i}", [128, 512], F32)) for i in range(2)]
        pbc = ctx.enter_context(nc.psum_tensor("c_pbc", [128, 512], F32))
        pw = ctx.enter_context(nc.psum_tensor("c_pw", [128, 512], F32))
        P.add('gpsimd', lambda e: e.memset(ones[:], 1.0), w=['ones'])
        P.dma('sync', g1bc[:], dr['modD'][1, 0, 2048:3072].partition_broadcast(64), w=['g1bc'])
        for h in range(16):
            sl = h % 2
            P.dma('sync', wof[:, sl, :], dr['w_o'][h * 64:(h + 1) * 64, :], w=[('wof', sl)])
            P.add('vector', lambda e, h=h, sl=sl: e.tensor_tensor(out=wo[:, h, :], in0=wof[:, sl, :], in1=g1bc[:], op=ALU.mult),
                  r=[('wof', sl), 'g1bc'], w=['wo'])
        nq = 0; nps = 0; npt = 0; ntmp = 0
        for a in A_LIST:
            P.dma('sync', KT[:, :], dr['KTD'][2 * a:2 * a + 2, :, :].rearrange("b d t -> (b d) t"), w=['KT'])
            for c0 in range(0, NKC, 26):
                c1 = min(NKC, c0 + 26)
                P.dma('sync', V[:, c0:c1, :, :], dr['VD'][c0 * 128:c1 * 128, 2 * a:2 * a + 2, :].rearrange("(c p) g e -> p c g e", p=128), w=['V'])
            for qb in range(NQB):
                qs = nq % 2; nq += 1
                P.dma('sync', QT[qs][:], dr['QTD'][a * 8:(a + 1) * 8, :, qb * 512:(qb + 1) * 512].rearrange("(i b) d t -> (b d) i t", b=2), w=[('QT', qs)])
                for i in range(4):
                    def qk(kc, s):
                        for b in range(2):
                            P.add('tensor', lambda e, b=b, kc=kc, s=s, qs=qs, i=i: e.matmul(
                                ps[s][:, b * 512:(b + 1) * 512], lhsT=KT[b * 64:(b + 1) * 64, kc * 128:(kc + 1) * 128],
                                rhs=QT[qs][b * 64:(b + 1) * 64, i, :], start=True, stop=True),
                                r=['KT', ('QT', qs)], w=[('ps', s)])
                    s0 = nps % 2; nps += 1
                    qk(0, s0); cur = s0
                    for kc in range(NKC):
                        t = npt % 3; npt += 1
                        if kc + 1 < NKC:
                            s1 = nps % 2; nps += 1
                            qk(kc + 1, s1)
                        P.add('scalar', lambda e, cur=cur, t=t: e.activation(out=PT[t][:], in_=ps[cur][:], func=AF.Exp, scale=0.125),
                              r=[('ps', cur)], w=[('PT', t)])
                        for b in range(2):
                            P.add('tensor', lambda e, b=b, kc=kc, t=t: e.matmul(
                                po[b][0:65, :], lhsT=V[:, kc, b, :], rhs=PT[t][:, b * 512:(b + 1) * 512], start=(kc == 0), stop=(kc == NKC - 1)),
                                r=['V', ('PT', t)], w=[('po', b)])
                        if kc + 1 < NKC: cur = s1
                    for b in range(2):
                        P.add('vector', lambda e, b=b: e.tensor_copy(out=oS[:, b, :], in_=po[b][0:65, :]), r=[('po', b)], w=[('oS', b)])
                        P.add('vector', lambda e, b=b: e.reciprocal(out=oS[64:65, b, :], in_=oS[64:65, b, :]), r=[('oS', b)], w=[('oS', b)])
                        P.add('tensor', lambda e, b=b: e.matmul(pbc[0:64, :], lhsT=ones[64:65, :], rhs=oS[64:65, b, :], start=True, stop=True),
                              r=[('oS', b), 'ones'], w=['pbc'])
                        P.add('vector', lambda e, b=b, i=i: e.tensor_tensor(out=attnT[:, i * 2 + b, :], in0=oS[0:64, b, :], in1=pbc[0:64, :], op=ALU.mult),
                              r=[('oS', b), 'pbc'], w=[('attnT', i * 2 + b)])
                for j in range(4):
                    for half in range(2):
                        for hh in range(8):
                            i_, b_ = hh // 2, hh % 2; h = a * 8 + b_ * 4 + i_
                            P.add('tensor', lambda e, hh=hh, h=h, j=j, half=half: e.matmul(
                                pw[:, :], lhsT=attnT[:, hh, j * 128:(j + 1) * 128], rhs=wo[:, h, half * 512:(half + 1) * 512], start=(hh == 0), stop=(hh == 7)),
                                r=[('attnT', hh), 'wo'], w=['pw'])
                        ts = ntmp % 2; ntmp += 1
                        P.add('vector', lambda e, ts=ts: e.tensor_copy(out=tmp[ts][:], in_=pw[:, :]), r=['pw'], w=[('tmp', ts)])
                        r0 = qb * 512 + j * 128
                        P.dma('gpsimd', dr['xaD'][r0:r0 + 128, half * 512:(half + 1) * 512], tmp[ts][:], r=[('tmp', ts)],
                              w=[('xo', qb, j, half)], accum_op=ALU.add)
        P.emit(ctx)


def vw(t, off, dims):
    b = t[:]
    return bass.AP(tensor=b.tensor, offset=b.offset + off, ap=[list(b.ap[0])] + [list(d) for d in dims])


def stage2(k, ntiles=None, NSEG=1, W=2):
    nc = k.nc; dr = k.dr
    with ExitStack() as ctx:
        T = lambda n, sh, dt=F32: ctx.enter_context(nc.sbuf_tensor("q_" + n, sh, dt))
        P = Prog(nc)
        ident, eps_t = consts(k, ctx, P, "q_")
        AB = load_AB(k, ctx, P, 1, "q_")
        wq = T("wq", [128, 8, 1536], BF16)
        for dc in range(8):
            for a in range(2):
                for b in range(2):
                    P.dma('gpsimd', vw(wq, dc * 1536 + a * 512 + b * 64, [[128, 4], [1, 64]]),
                          dr['w_qkv'][dc * 128:(dc + 1) * 128, a * 512 + b * 256:a * 512 + (b + 1) * 256].rearrange("p (i d) -> p i d", d=64), w=[('wq', dc)])
            P.dma('gpsimd', wq[:, dc, 1024:1536], dr['w_qkv'][dc * 128:(dc + 1) * 128, 1024:1536], w=[('wq', dc)])
        gbc = T("gbc", [128, 1280])
        P.dma('sync', gbc[:], dr['qkg'][0, :].partition_broadcast(128), w=['gbc'])
        xt = [T(f"xt{s}", [128, 2, 1024]) for s in range(W)]
        ssq = [T(f"ssq{s}", [128, 2]) for s in range(W)]; rstd = [T(f"rstd{s}", [128, 2]) for s in range(W)]
        xn = [T(f"xn{s}", [128, 2, 1024], BF16) for s in range(W)]; hx = [T(f"hx{s}", [128, 8, 256], BF16) for s in range(W)]
        cs = [T(f"cs{s}", [128, 2, 2, 64]) for s in range(W)]
        qk_ = [[T(f"qk{s}{j}", [128, 1536]) for j in range(2)] for s in range(W)]; sq_ = [[T(f"sq{s}{j}", [128, 1280]) for j in range(2)] for s in range(W)]
        rh_ = [[T(f"rh{s}{j}", [128, 20]) for j in range(2)] for s in range(W)]; qn_ = [[T(f"qn{s}{j}", [128, 1280]) for j in range(2)] for s in range(W)]
        t2_ = [[T(f"t2{s}{j}", [128, 1280]) for j in range(2)] for s in range(W)]; qr_ = [[T(f"qr{s}{j}", [128, 1280], BF16) for j in range(2)] for s in range(W)]
        vs = [T(f"vs{s}", [128, 2, 4, 65], BF16) for s in range(W)]
        qT = [T(f"qT{s}", [128, 10, 2, 128], BF16) for s in range(W)]
        pT = ctx.enter_context(nc.psum_tensor("q_pT", [128, 4, 512], BF16))
        pq = [ctx.enter_context(nc.psum_tensor(f"q_pq{i}", [128, 512], F32)) for i in range(3)]
        for s in range(W):
            P.add('gpsimd', lambda e, s=s: e.memset(vs[s][:], 1.0), w=[('vs', s)])
        tiles = [(0, t) for t in range(NT * NSEG)] + [(1, 0)]
        if ntiles: tiles = tiles[:ntiles - 1] + [(1, 0)]
        def tile_body(P, ti, v, t):
            s = ti % W
            qk, sq, rh, qn, t2, qr = qk_[s], sq_[s], rh_[s], qn_[s], t2_[s], qr_[s]
            src = dr['xaD'] if v == 0 else dr['caD']
            r0 = 256 * t; k0 = r0 if v == 0 else 4096 * NSEG
            P.dma('sync', xt[s][:], src[r0:r0 + 256, :].rearrange("(j p) d -> p j d", p=128), w=[('xt', s)])
            P.dma('sync', cs[s][:, :, 0, :], dr['ropeC'][k0:k0 + 256, :].rearrange("(j p) e -> p j e", p=128), w=[('cs', s)])
            P.dma('sync', cs[s][:, :, 1, :], dr['ropeS'][k0:k0 + 256, :].rearrange("(j p) e -> p j e", p=128), w=[('cs', s)])
            emit_norm_T(P, [xt[s][:, 0, :], xt[s][:, 1, :]], ('xt', s), ssq[s], rstd[s], [xn[s][:, 0, :], xn[s][:, 1, :]], eps_t, ident, pT,
                        AB[:, v, 0, 0, :], AB[:, v, 0, 1, :], hx[s], ('n', s))
            full = (v == 0 and t < NT)
            c0 = 0 if full else 1024; h0 = c0 // 64; nh = 20 - h0; W_ = 1280 - c0
            for j in range(2):
                for cb in (range(3) if full else (2,)):
                  with P.atom():
                    for dc in range(8):
                        P.add('tensor', lambda e, s=s, j=j, cb=cb, dc=dc: e.matmul(pq[cb][:], lhsT=hx[s][:, dc, j * 128:(j + 1) * 128],
                                                                                 rhs=wq[:, dc, cb * 512:(cb + 1) * 512], start=(dc == 0), stop=(dc == 7)),
                              r=[('n', s, 'hx'), ('wq', dc)], w=[('pq', cb)])
                    P.add('scalar', lambda e, j=j, cb=cb: e.activation(out=qk[j][:, cb * 512:(cb + 1) * 512], in_=pq[cb][:], func=AF.Identity),
                          r=[('pq', cb)], w=[('qk', s, j)])
                Q = ('qk', s, j)
                hd = lambda ap: ap.rearrange("p (h d) -> p h d", d=64)
                P.add('scalar', lambda e, j=j, c0=c0: e.activation(out=sq[j][:, c0:1280], in_=qk[j][:, c0:1280], func=AF.Square), r=[Q], w=[('sq', s, j)])
                P.add('vector', lambda e, j=j, c0=c0, h0=h0: e.tensor_reduce(out=rh[j][:, h0:20], in_=hd(sq[j][:, c0:1280]), axis=AX.X, op=ALU.add),
                      r=[('sq', s, j)], w=[('rh', s, j)])
                P.add('scalar', lambda e, j=j, h0=h0: e.activation(out=rh[j][:, h0:20], in_=rh[j][:, h0:20], func=AF.Sqrt, scale=1.0 / 64, bias=eps_t[:, 0:1]),
                      r=[('rh', s, j), 'eps'], w=[('rh', s, j)])
                P.add('vector', lambda e, j=j, h0=h0: e.reciprocal(out=rh[j][:, h0:20], in_=rh[j][:, h0:20]), r=[('rh', s, j)], w=[('rh', s, j)])
                P.add('vector', lambda e, j=j, c0=c0, h0=h0, nh=nh: e.tensor_tensor(out=hd(qn[j][:, c0:1280]), in0=hd(qk[j][:, c0:1280]),
                                                                               in1=vw(rh[j], h0, [[1, nh], [0, 64]]), op=ALU.mult), r=[Q, ('rh', s, j)], w=[('qn', s, j)])
                P.add('vector', lambda e, j=j, c0=c0: e.tensor_tensor(out=qn[j][:, c0:1280], in0=qn[j][:, c0:1280], in1=gbc[:, c0:1280], op=ALU.mult),
                      r=['gbc'], w=[('qn', s, j)])
                cofs = j * 128; sofs = j * 128 + 64
                P.add('vector', lambda e, j=j, s=s, sofs=sofs, c0=c0, nh=nh: e.tensor_tensor(out=vw(t2[j], c0, [[64, nh], [32, 2], [1, 16]]),
                                                                                       in0=vw(qn[j], c0 + 16, [[64, nh], [32, 2], [1, 16]]),
                                                                                       in1=vw(cs[s], sofs, [[0, nh], [32, 2], [1, 16]]), op=ALU.mult),
                      r=[('qn', s, j), ('cs', s)], w=[('t2', s, j)])
                P.add('vector', lambda e, j=j, s=s, sofs=sofs, c0=c0, nh=nh: e.tensor_tensor(out=vw(t2[j], c0 + 16, [[64, nh], [32, 2], [1, 16]]),
                                                                                       in0=vw(qn[j], c0, [[64, nh], [32, 2], [1, 16]]),
                                                                                       in1=vw(cs[s], sofs + 16, [[0, nh], [32, 2], [1, 16]]), op=ALU.mult),
                      r=[('qn', s, j), ('cs', s)], w=[('t2', s, j)])
                P.add('vector', lambda e, j=j, s=s, cofs=cofs, c0=c0, nh=nh: e.tensor_tensor(out=hd(qn[j][:, c0:1280]), in0=hd(qn[j][:, c0:1280]),
                                                                                       in1=vw(cs[s], cofs, [[0, nh], [1, 64]]), op=ALU.mult),
                      r=[('cs', s), ('t2', s, j)], w=[('qn', s, j)])
                P.add('vector', lambda e, j=j, c0=c0: e.tensor_tensor(out=qr[j][:, c0:1280], in0=qn[j][:, c0:1280], in1=t2[j][:, c0:1280], op=ALU.add),
                      r=[('t2', s, j)], w=[('qn', s, j), ('qr', s, j)])
                P.add('scalar', lambda e, j=j, s=s: e.activation(out=vs[s][:, j, :, 0:64], in_=qk[j][:, 1280:1536].rearrange("p (g d) -> p g d", d=64), func=AF.Identity),
                      r=[Q], w=[('vs', s)])
                for grp in (range(3) if full else (2,)):
                  with P.atom():
                    rg = (0, 2, 1)[grp]; bk = rg // 2; n = 4 if grp < 2 else 2
                    for u in range(n):
                        sl = grp * 4 + u
                        P.add('tensor', lambda e, j=j, sl=sl, rg=rg, u=u: e.transpose(out=pT[:, rg, u * 128:(u + 1) * 128], in_=qr[j][:, sl * 128:(sl + 1) * 128],
                                                                                identity=ident[:]), r=[('qr', s, j), 'ident'], w=[('pT', bk)])
                    P.add('vector', lambda e, j=j, s=s, grp=grp, rg=rg, n=n: e.tensor_copy(out=qT[s][:, grp * 4:grp * 4 + n, j, :],
                                                                               in_=pT[:, rg, 0:n * 128].rearrange("p (u t) -> p u t", t=128)),
                          r=[('pT', bk)], w=[('qT', s)])
            for j in range(2):
                if v == 0 and t < NT:
                    P.dma('sync', dr['QTD'][:, :, r0 + j * 128:r0 + (j + 1) * 128].rearrange("(hp two) d t -> (two d) hp t", two=2), qT[s][:, 0:8, j, :],
                          r=[('qT', s)], w=['QTD'])
                P.dma('sync', dr['KTD'][:, :, k0 + j * 128:k0 + (j + 1) * 128].rearrange("(a b) d t -> (b d) a t", b=2), qT[s][:, 8:10, j, :],
                      r=[('qT', s)], w=['KTD'])
            P.dma('sync', dr['VD'][k0:k0 + 256, :, :].rearrange("(j p) g e -> p j g e", p=128), vs[s][:], r=[('vs', s)], w=['VD'])
        recs = []
        for ti, (v, t) in enumerate(tiles):
            R = Rec(); recs.append(R); tile_body(R, ti, v, t)
        interleave(P, recs, W)
        P.emit(ctx)


def stage3b(k, ntiles=NT):
    nc = k.nc; dr = k.dr
    with ExitStack() as ctx:
        T = lambda n, sh, dt=F32: ctx.enter_context(nc.sbuf_tensor("r_" + n, sh, dt))
        P = Prog(nc)
        ident, eps_t = consts(k, ctx, P, "r_")
        AB = load_AB(k, ctx, P, 1, "r_")
        RW = T("RW", [128, 8, 1024]); Abc = T("Abc", [128, 1024]); Bbc = T("Bbc", [128, 1024]); gbc = T("gbc", [128, 1024])
        cbc = T("cbc", [128, 8]); junk = T("junk", [128, 1024])
        P.dma('sync', Abc[:], dr['modD'][1, 0, 4096:5120].partition_broadcast(128), w=['Abc'])
        P.dma('sync', Bbc[:], dr['modD'][1, 0, 3072:4096].partition_broadcast(128), w=['Bbc'])
        P.dma('sync', gbc[:], dr['norm_g'][1, 1, :].partition_broadcast(128), w=['gbc'])
        P.add('vector', lambda e: e.scalar_tensor_tensor(out=Abc[:], in0=Abc[:], scalar=1.0, in1=gbc[:], op0=ALU.add, op1=ALU.mult), r=['gbc'], w=['Abc'])
        P.add('gpsimd', lambda e: e.memset(cbc[:], 0.0), w=['cbc'])
        for ex in range(8):
            P.dma('sync', RW[:, ex, :], dr['rwT'][ex, :].partition_broadcast(128), w=[('RW', ex)])
            P.add('vector', lambda e, ex=ex: e.scalar_tensor_tensor(out=junk[:], in0=Bbc[:], scalar=1.0, in1=RW[:, ex, :], op0=ALU.mult, op1=ALU.mult,
                                                                    accum_out=cbc[:, ex:ex + 1]), r=['Bbc', ('RW', ex), 'cbc'], w=['junk', 'cbc'])
            P.add('vector', lambda e, ex=ex: e.tensor_tensor(out=RW[:, ex, :], in0=RW[:, ex, :], in1=Abc[:], op=ALU.mult), r=['Abc'], w=[('RW', ex)])
        xt = [T(f"xt{s}", [128, 2, 1024]) for s in range(2)]
        ssq = [T(f"ssq{s}", [128, 2]) for s in range(2)]; rstd = [T(f"rstd{s}", [128, 2]) for s in range(2)]
        xn = [T(f"xn{s}", [128, 2, 1024], BF16) for s in range(2)]; hx = [T(f"hx{s}", [128, 8, 256], BF16) for s in range(2)]
        raw = [T(f"raw{s}", [128, 2, 8]) for s in range(2)]
        lg = T("lg", [128, 2, 8]); lg2 = T("lg2", [128, 2, 8]); mk1 = T("mk1", [128, 2, 8]); mk2 = T("mk2", [128, 2, 8])
        m1 = T("m1", [128, 2]); m2 = T("m2", [128, 2]); e2 = T("e2", [128, 2]); g1v = T("g1v", [128, 2]); g2v = T("g2v", [128, 2])
        gates = T("gates", [128, 32, 8])
        pT = ctx.enter_context(nc.psum_tensor("r_pT", [128, 4, 512], BF16))
        for s in range(2):
            P.add('gpsimd', lambda e, s=s: e.memset(raw[s][:], 0.0), w=[('raw', s)])
        for t in range(ntiles):
            s = t % 2; r0 = 256 * t
            P.dma('sync', xt[s][:], dr['xaD'][r0:r0 + 256, :].rearrange("(j p) d -> p j d", p=128), w=[('xt', s)])
            emit_norm_T(P, [xt[s][:, 0, :], xt[s][:, 1, :]], ('xt', s), ssq[s], rstd[s], [xn[s][:, 0, :], xn[s][:, 1, :]], eps_t, ident, pT,
                        AB[:, 0, 1, 0, :], AB[:, 0, 1, 1, :], hx[s], ('n', s))
            P.dma('sync', dr['hxD'][:, :, r0:r0 + 256].rearrange("c p t -> p c t"), hx[s][:], r=[('n', s, 'hx')], w=['hxD'])
            for j in range(2):
                for ex in range(8):
                    P.add('vector', lambda e, s=s, j=j, ex=ex: e.scalar_tensor_tensor(out=junk[:], in0=xt[s][:, j, :], scalar=1.0, in1=RW[:, ex, :],
                                                                                    op0=ALU.mult, op1=ALU.mult, accum_out=raw[s][:, j, ex:ex + 1]),
                          r=[('xt', s), ('RW', ex), ('raw', s)], w=['junk', ('raw', s)])
            V = 'vector'
            P.add(V, lambda e, s=s: e.tensor_tensor(out=lg[:], in0=raw[s][:], in1=vw(rstd[s], 0, [[1, 2], [0, 8]]), op=ALU.mult), r=[('raw', s), ('n', s, 'rstd')], w=['lg'])
            P.add('gpsimd', lambda e, s=s: e.memset(raw[s][:], 0.0), r=['lg'], w=[('raw', s)])
            P.add(V, lambda e: e.tensor_tensor(out=lg[:], in0=lg[:], in1=vw(cbc, 0, [[0, 2], [1, 8]]), op=ALU.add), r=['cbc'], w=['lg'])
            P.add(V, lambda e: e.tensor_reduce(out=m1[:], in_=lg[:], axis=AX.X, op=ALU.max), r=['lg'], w=['m1'])
            P.add(V, lambda e: e.tensor_tensor(out=mk1[:], in0=lg[:], in1=vw(m1, 0, [[1, 2], [0, 8]]), op=ALU.is_equal), r=['lg', 'm1'], w=['mk1'])
            P.add(V, lambda e: e.scalar_tensor_tensor(out=lg2[:], in0=mk1[:], scalar=-1e30, in1=lg[:], op0=ALU.mult, op1=ALU.add), r=['mk1', 'lg'], w=['lg2'])
            P.add(V, lambda e: e.tensor_reduce(out=m2[:], in_=lg2[:], axis=AX.X, op=ALU.max), r=['lg2'], w=['m2'])
            P.add(V, lambda e: e.tensor_tensor(out=mk2[:], in0=lg2[:], in1=vw(m2, 0, [[1, 2], [0, 8]]), op=ALU.is_equal), r=['lg2', 'm2'], w=['mk2'])
            P.add(V, lambda e: e.tensor_tensor(out=e2[:], in0=m2[:], in1=m1[:], op=ALU.subtract), r=['m1', 'm2'], w=['e2'])
            P.add('scalar', lambda e: e.activation(out=e2[:], in_=e2[:], func=AF.Exp), r=['e2'], w=['e2'])
            P.add(V, lambda e: e.tensor_scalar(out=g1v[:], in0=e2[:], scalar1=1.0, scalar2=None, op0=ALU.add), r=['e2'], w=['g1v'])
            P.add(V, lambda e: e.reciprocal(out=g1v[:], in_=g1v[:]), r=['g1v'], w=['g1v'])
            P.add(V, lambda e: e.tensor_tensor(out=g2v[:], in0=e2[:], in1=g1v[:], op=ALU.mult), r=['e2', 'g1v'], w=['g2v'])
            P.add(V, lambda e: e.tensor_tensor(out=mk1[:], in0=mk1[:], in1=vw(g1v, 0, [[1, 2], [0, 8]]), op=ALU.mult), r=['g1v'], w=['mk1'])
            P.add(V, lambda e: e.tensor_tensor(out=mk2[:], in0=mk2[:], in1=vw(g2v, 0, [[1, 2], [0, 8]]), op=ALU.mult), r=['g2v'], w=['mk2'])
            P.add(V, lambda e, t=t: e.tensor_tensor(out=gates[:, 2 * t:2 * t + 2, :], in0=mk1[:], in1=mk2[:], op=ALU.add), r=['mk1', 'mk2'], w=['gates'])
        P.dma('sync', dr['gatesD'][0:ntiles * 256, :].rearrange("(c p) e -> p c e", p=128), gates[:, 0:2 * ntiles, :], r=['gates'], w=['gatesD'])
        P.emit(ctx)


def stage4(k, nexp=8):
    dr = k.dr
    units = [(dr['moe_w1'][ex][:, h * 1408:(h + 1) * 1408], dr['moe_w3'][ex][:, h * 1408:(h + 1) * 1408], dr['moe_w2'][ex][h * 1408:(h + 1) * 1408, :], ex)
             for ex in range(nexp) for h in range(2)]
    tiles = [(dr['hxD'][:, :, 256 * t:256 * (t + 1)], dr['xaD'][256 * t:256 * (t + 1), :], 0, 2 * t) for t in range(NT)]
    ffn_pass(k, "m_", units, tiles, {0: dr['modD'][1, 0, 5120:6144]}, gatesD=dr['gatesD'])


def stage5(k, ntiles=NT):
    nc = k.nc; dr = k.dr
    with ExitStack() as ctx:
        T = lambda n, sh, dt=F32: ctx.enter_context(nc.sbuf_tensor("f_" + n, sh, dt))
        P = Prog(nc)
        eps_t = T("eps", [128, 1]); fg = T("fg", [128, 1024])
        P.add('gpsimd', lambda e: e.memset(eps_t[:], 1e-6), w=['eps'])
        P.dma('sync', fg[:], dr['final_g'][0, :].partition_broadcast(128), w=['fg'])
        xt = [T(f"xt{s}", [128, 2, 1024]) for s in range(2)]; ot = [T(f"ot{s}", [128, 2, 1024]) for s in range(2)]
        ssq = [T(f"ssq{s}", [128, 2]) for s in range(2)]; rstd = [T(f"rstd{s}", [128, 2]) for s in range(2)]
        for t in range(ntiles):
            s = t % 2; r0 = 256 * t
            P.dma('sync', xt[s][:], dr['xaD'][r0:r0 + 256, :].rearrange("(j p) d -> p j d", p=128), w=[('xt', s)])
            for j in range(2):
                P.add('scalar', lambda e, s=s, j=j: e.activation(out=ot[s][:, j, :], in_=xt[s][:, j, :], func=AF.Square, accum_out=ssq[s][:, j:j + 1]),
                      r=[('xt', s)], w=[('ot', s), ('ssq', s)])
            P.add('scalar', lambda e, s=s: e.activation(out=rstd[s][:], in_=ssq[s][:], func=AF.Sqrt, scale=1.0 / D, bias=eps_t[:, 0:1]),
                  r=[('ssq', s), 'eps'], w=[('rstd', s)])
            P.add('vector', lambda e, s=s: e.reciprocal(out=rstd[s][:], in_=rstd[s][:]), r=[('rstd', s)], w=[('rstd', s)])
            for j in range(2):
                P.add('vector', lambda e, s=s, j=j: e.scalar_tensor_tensor(out=ot[s][:, j, :], in0=xt[s][:, j, :], scalar=rstd[s][:, j:j + 1], in1=fg[:],
                                                                                              op0=ALU.mult, op1=ALU.mult),
                      r=[('xt', s), ('rstd', s), 'fg'], w=[('ot', s)])
            P.dma('sync', dr['out'][r0:r0 + 256, :].rearrange("(j p) d -> p j d", p=128), ot[s][:], r=[('ot', s)], w=['out'])
        P.emit(ctx)


POOLW=(2,4,8,16)
def edge_row(L, realfirst, reallast):
    inv=np.zeros((2,4,2,8),np.float32)
    for g,w in enumerate(POOLW):
        half=w//2
        for i in range(8):
            t=i; cnt=(min(t+half,L)-max(t-half,0)) if realfirst else w
            inv[0,g,:,i]=1.0/cnt
            t=L-8+i; cnt=(min(t+half,L)-max(t-half,0)) if reallast else w
            inv[1,g,:,i]=1.0/cnt
    return inv.reshape(128), [0.0 if realfirst else 1.0, 0.0 if reallast else 1.0]
def seg_quarters(r, NSEG):
    q=r%4
    if NSEG==1: return [q]
    if NSEG==4: return [q]+[o for o in range(4) if o!=q]
    m=r%2; p=(r%4)//2
    return [2*m+p, 2*m+1-p]
def core_inputs(r, inp, NSEG=1):
    b=r//4; qs=seg_quarters(r,NSEG); x=inp['x'][b]
    xin=np.zeros((NSEG,4112,1024),np.float32); inv=np.zeros((NSEG+1,128),np.float32); msk=np.zeros((1,2*(NSEG+1)),np.float32)
    for sg,q in enumerate(qs):
        lo=q*4096-8; hi=q*4096+4096+8; a=max(lo,0); z=min(hi,16384)
        xin[sg,a-lo:a-lo+(z-a)]=x[a:z]
        inv[sg],msk[0,2*sg:2*sg+2]=edge_row(16384,q==0,q==3)
    inv[NSEG],msk[0,2*NSEG:]=edge_row(256,True,True)
    ctxin=np.zeros((272,1024),np.float32); ctxin[8:264]=inp['ctx'][b]
    cvec=np.stack([inp['c'][b], inp['c_ctx']]).astype(np.float32)
    return dict(xin=xin, ctxin=ctxin, cvec=cvec, inv=inv, msk=msk)
def rope_gain(inp):
    return np.concatenate([np.tile(inp['q_norm_g'][0],16), np.tile(inp['k_norm_g'][0],4)]).astype(np.float32)[None,:]
def rope_tables(r, NSEG=1):
    n=NSEG*4096
    C=np.ones((n+256,64),np.float32); S=np.zeros((n+256,64),np.float32)
    fr=(10000.0**(-np.arange(16,dtype=np.float32)/16)).astype(np.float32)
    for sg,q in enumerate(seg_quarters(r,NSEG)):
        t=np.arange(q*4096,(q+1)*4096); row=(t//64).astype(np.float32); col=(t%64).astype(np.float32)
        ar=row[:,None]*fr; ac=col[:,None]*fr
        C[sg*4096:(sg+1)*4096]=np.concatenate([np.cos(ar),np.cos(ar),np.cos(ac),np.cos(ac)],1)
        S[sg*4096:(sg+1)*4096]=np.concatenate([-np.sin(ar),np.sin(ar),-np.sin(ac),np.sin(ac)],1)
    return {'ropeC':C,'ropeS':S}


NSEG = 4


def _build():
    k = K()
    k.din('xin', [NSEG, 4112, 1024]); k.din('ctxin', [272, 1024]); k.din('cvec', [2, 1024]); k.din('inv', [NSEG + 1, 128]); k.din('msk', [1, 2 * (NSEG + 1)])
    k.din('ada_w', [2, 1024, 6144]); k.din('ada_b', [2, 6144]); k.din('norm_g', [2, 2, 1024]); k.din('pool_w', [4, 256, 256]); k.din('pool_scale', [1, 1024])
    k.din('ffn_w1', [1024, 2816]); k.din('ffn_w3', [1024, 2816]); k.din('ffn_w2', [2816, 1024])
    k.din('w_qkv', [1024, 1536]); k.din('qkg', [1, 1280]); k.din('ropeC', [NSEG * 4096 + 256, 64]); k.din('ropeS', [NSEG * 4096 + 256, 64])
    k.din('w_o', [1024, 1024]); k.din('rwT', [8, 1024]); k.din('final_g', [1, 1024])
    k.din('moe_w1', [8, 1024, 2816]); k.din('moe_w3', [8, 1024, 2816]); k.din('moe_w2', [8, 2816, 1024])
    k.dint('modD', [2, 2, 6144]); k.dint('xaD', [NSEG * 4096, 1024]); k.dint('caD', [256, 1024])
    k.dint('hxD', [8, 128, NSEG * 4096], BF16); k.dint('hcD', [8, 128, 256], BF16)
    k.dint('QTD', [16, 64, 4096], BF16); k.dint('KTD', [4, 64, NSEG * 4096 + 256], BF16); k.dint('VD', [NSEG * 4096 + 256, 4, 65], BF16)
    k.dint('gatesD', [4096, 8]); k.dout('out', [4096, 1024])
    stage0(k); stage1a(k, NSEG=NSEG); stage1b(k, NSEG=NSEG); stage2(k, NSEG=NSEG)
    stage3(k, NKC=(NSEG * 4096 + 256) // 128); stage3b(k); stage4(k); stage5(k)
    return k.nc


def kernel(x, c, ctx, c_ctx, ada_w, ada_b, norm_g, pool_w, pool_scale, ffn_w1, ffn_w3, ffn_w2,
           w_qkv, w_o, q_norm_g, k_norm_g, router_w, moe_w1, moe_w3, moe_w2, final_g):
    f = lambda a: np.ascontiguousarray(np.asarray(a, dtype=np.float32))
    inp = dict(x=f(x), c=f(c), ctx=f(ctx), c_ctx=f(c_ctx), q_norm_g=f(q_norm_g), k_norm_g=f(k_norm_g))
    shared = dict(ada_w=f(ada_w), ada_b=f(ada_b), norm_g=f(norm_g), pool_w=f(pool_w)[0], pool_scale=f(pool_scale),
                  ffn_w1=f(ffn_w1)[0], ffn_w3=f(ffn_w3)[0], ffn_w2=f(ffn_w2)[0], w_qkv=f(w_qkv)[0], qkg=rope_gain(inp),
                  w_o=f(w_o)[0], rwT=np.ascontiguousarray(f(router_w)[0].T), final_g=f(final_g)[None, :],
                  moe_w1=f(moe_w1)[0], moe_w3=f(moe_w3)[0], moe_w2=f(moe_w2)[0])
    maps = [{**core_inputs(r, inp, NSEG), **rope_tables(r, NSEG), **shared} for r in range(8)]
    res = run_bass_kernel_spmd(_build(), maps, core_ids=list(range(8))).results
    out = np.stack([np.concatenate([np.asarray(res[4 * b + q]['out']) for q in range(4)], axis=0) for b in range(2)])
    return out.astype(np.float32)
```

```python
import numpy as np, os
DBG=os.environ.get('DBG','')
from contextlib import ExitStack
import concourse.bass as bass
import concourse.mybir as mybir
from concourse.bass_utils import run_bass_kernel_spmd

F32 = mybir.dt.float32; BF16 = mybir.dt.bfloat16
AF = mybir.ActivationFunctionType; ALU = mybir.AluOpType; AX = mybir.AxisListType

D = 1024; DFF = 2816; NF = 22; SEQ = 16384; CTX = 256; TPC = 4096; TT = 256; NT = TPC // TT
ENGS = ['sync', 'scalar', 'vector', 'gpsimd', 'tensor']
NDSEM = 8
SAME_SYNC = {'gpsimd', 'scalar', 'vector'}
_uid = [0]


class _Op:
    __slots__ = ('eng', 'fn', 'deps', 'dma', 'signal', 'sigval', 'dsem', 'dval', 'prev_dma')

    def __init__(s, eng, fn, dma):
        s.eng = eng; s.fn = fn; s.dma = dma; s.deps = []; s.signal = False; s.sigval = 0
        s.dsem = None; s.dval = 0; s.prev_dma = None


class Prog:
    def __init__(s, nc):
        s.nc = nc; s.ops = {e: [] for e in ENGS}; s.lastw = {}; s.readers = {}
        s.dma_count = {e: 0 for e in ENGS}; s.dma_hist = {e: [] for e in ENGS}

    def add(s, eng, fn, r=(), w=(), dma=False):
        op = _Op(eng, fn, dma); deps = []
        for k in r:
            d = s.lastw.get(k)
            if d is not None: deps.append(d)
        for k in w:
            d = s.lastw.get(k)
            if d is not None: deps.append(d)
            deps.extend(s.readers.get(k, ()))
        seen = set()
        for d in deps:
            if id(d) in seen: continue
            seen.add(id(d))
            if d.dma or d.eng != eng or eng in SAME_SYNC:
                op.deps.append(d)
                if not d.dma: d.signal = True
        for k in w:
            s.lastw[k] = op; s.readers[k] = []
        for k in r:
            s.readers.setdefault(k, []).append(op)
        if dma:
            n = s.dma_count[eng]; s.dma_count[eng] = n + 1
            op.dsem = n % NDSEM; op.dval = 16 * (n // NDSEM + 1)
            h = s.dma_hist[eng]
            if n >= NDSEM: op.prev_dma = h[n - NDSEM]
            h.append(op)
        s.ops[eng].append(op)
        return op

    def dma(s, eng, out, in_, r=(), w=(), **kw):
        return s.add(eng, lambda e: e.dma_start(out=out, in_=in_, **kw), r=r, w=w, dma=True)

    def emit(s, ctx):
        nc = s.nc; _uid[0] += 1; u = _uid[0]
        esem = {e: nc.alloc_semaphore(name=f"es{u}_{e}") for e in ENGS}
        dsem = {e: [nc.alloc_semaphore(name=f"ds{u}_{e}{i}") for i in range(NDSEM)]
                for e in ENGS if s.dma_count[e] > 0}
        allsems = list(esem.values()) + [h for l in dsem.values() for h in l]

        def _cleanup():
            nc.clear_and_free_semaphores(allsems)
            nc.all_engine_barrier()
        ctx.callback(_cleanup)
        for e in ENGS:
            c = 0
            for op in s.ops[e]:
                if op.signal and not op.dma:
                    c += 1; op.sigval = c
        block = ctx.enter_context(nc.Block())

        def mk(ename):
            def body(e):
                waited = {}

                def wait(sem, val, key):
                    if waited.get(key, 0) < val:
                        e.wait_ge(sem, val); waited[key] = val
                for op in s.ops[ename]:
                    for d in op.deps:
                        if d.dma: wait(dsem[d.eng][d.dsem], d.dval, ('d', d.eng, d.dsem))
                        else: wait(esem[d.eng], d.sigval, ('e', d.eng))
                    if op.dma and op.prev_dma is not None:
                        d = op.prev_dma; wait(dsem[d.eng][d.dsem], d.dval, ('d', d.eng, d.dsem))
                    ins = op.fn(e)
                    if op.dma: ins.then_inc(dsem[ename][op.dsem], 16)
                    elif op.signal: ins.then_inc(esem[ename], 1)
                if ename == 'sync':
                    for q in dsem:
                        for d in s.dma_hist[q][-NDSEM:]:
                            wait(dsem[q][d.dsem], d.dval, ('d', q, d.dsem))
            return body
        for ename in ENGS:
            if s.ops[ename] or ename == 'sync':
                getattr(block, ename)(mk(ename))


class Rec:
    def __init__(s): s.l = []; s.cur = None

    def _put(s, op):
        if s.cur is not None: s.cur.append(op)
        else: s.l.append([op])

    def add(s, *a, **k): s._put(('add', a, k))

    def dma(s, *a, **k): s._put(('dma', a, k))

    def atom(s):
        rec = s

        class _A:
            def __enter__(self_):
                assert rec.cur is None; rec.cur = []

            def __exit__(self_, *a):
                rec.l.append(rec.cur); rec.cur = None
        return _A()


def interleave(P, recs, W):
    active = {}; nxt = 0
    while nxt < len(recs) or active:
        while nxt < len(recs) and (nxt % W) not in active:
            active[nxt % W] = iter(recs[nxt].l); nxt += 1
        for sl in sorted(active, key=lambda q: q):
            unit = next(active[sl], None)
            if unit is None:
                del active[sl]; continue
            for op in unit:
                getattr(P, op[0])(*op[1], **op[2])


class _NoAtom:
    def __enter__(s): pass

    def __exit__(s, *a): pass


def atom_of(R):
    return R.atom() if isinstance(R, Rec) else _NoAtom()


class K:
    def __init__(s):
        s.nc = bass.Bass("TRN2", target_bir_lowering=False)
        s.dr = {}

    def din(s, name, shape, dt=F32):
        s.dr[name] = s.nc.dram_tensor(name, list(shape), dt, kind="ExternalInput").ap(); return s.dr[name]

    def dout(s, name, shape, dt=F32):
        s.dr[name] = s.nc.dram_tensor(name, list(shape), dt, kind="ExternalOutput").ap(); return s.dr[name]

    def dint(s, name, shape, dt=F32):
        s.dr[name] = s.nc.dram_tensor(name, list(shape), dt, kind="Internal").ap(); return s.dr[name]


def stage0(k):
    nc = k.nc; dr = k.dr
    with ExitStack() as ctx:
        T = lambda n, sh, dt=F32: ctx.enter_context(nc.sbuf_tensor("s0_" + n, sh, dt))
        cT = T("cT", [128, 8, 2]); scT = T("scT", [128, 8, 2])
        awt = [T(f"aw{i}", [128, 8, 512]) for i in range(2)]
        adab = T("adab", [2, 2, 6144]); modsb = T("mod", [2, 2, 6144])
        pm = [ctx.enter_context(nc.psum_tensor(f"s0_pm{i}", [128, 512], F32)) for i in range(2)]
        P = Prog(nc)
        for v in range(2):
            P.dma('sync', cT[:, :, v], dr['cvec'][v, :].rearrange("(c p) -> p c", p=128), w=['cT'], allow_slow_non_contiguous=True)
        for i in range(2):
            P.dma('gpsimd', adab[:, i, :], dr['ada_b'][i, :].partition_broadcast(2), w=[('adab', i)])
        P.add('scalar', lambda e: e.activation(out=scT[:], in_=cT[:], func=AF.Silu), r=['cT'], w=['scT'])
        n = 0
        for i in range(2):
            for cb in range(12):
                sl = n % 2; n += 1
                P.dma('sync', awt[sl][:], dr['ada_w'][i, :, cb * 512:(cb + 1) * 512].rearrange("(c p) n -> p c n", p=128),
                      w=[('aw', sl)])
                for kc in range(8):
                    P.add('tensor', lambda e, sl=sl, kc=kc: e.matmul(pm[sl][0:2, :], lhsT=scT[:, kc, :], rhs=awt[sl][:, kc, :],
                                                                     start=(kc == 0), stop=(kc == 7)),
                          r=['scT', ('aw', sl)], w=[('pm', sl)])
                P.add('vector', lambda e, sl=sl, i=i, cb=cb: e.tensor_tensor(out=modsb[:, i, cb * 512:(cb + 1) * 512], in0=pm[sl][0:2, :],
                                                                            in1=adab[:, i, cb * 512:(cb + 1) * 512], op=ALU.add),
                      r=[('pm', sl), ('adab', i)], w=['modsb'])
        P.dma('sync', dr['modD'].rearrange("i v n -> v i n"), modsb[:], r=['modsb'], w=['modD'])
        P.emit(ctx)


def load_AB(k, ctx, P, layer, pref):
    nc = k.nc; dr = k.dr
    T = lambda n, sh, dt=F32: ctx.enter_context(nc.sbuf_tensor(pref + n, sh, dt))
    modF = T("modF", [128, 2, 48]); gF = T("gF", [128, 2, 8]); AB = T("AB", [128, 2, 2, 2, 8])
    for v in range(2):
        P.dma('sync', modF[:, v, :], dr['modD'][layer, v, :].rearrange("(kc p) -> p kc", p=128), w=[('modF', v)],
              allow_slow_non_contiguous=True)
    for sub in range(2):
        P.dma('sync', gF[:, sub, :], dr['norm_g'][layer, sub, :].rearrange("(c p) -> p c", p=128), w=['gF'], allow_slow_non_contiguous=True)
    for v in range(2):
        for sub in range(2):
            sh = modF[:, v, (3 * sub) * 8:(3 * sub) * 8 + 8]; sc = modF[:, v, (3 * sub + 1) * 8:(3 * sub + 1) * 8 + 8]
            P.add('vector', lambda e, v=v, sub=sub, sc=sc: e.scalar_tensor_tensor(out=AB[:, v, sub, 0, :], in0=sc, scalar=1.0, in1=gF[:, sub, :],
                                                                              op0=ALU.add, op1=ALU.mult),
                  r=[('modF', v), 'gF'], w=['AB'])
            P.add('vector', lambda e, v=v, sub=sub, sh=sh: e.tensor_copy(out=AB[:, v, sub, 1, :], in_=sh), r=[('modF', v)], w=['AB'])
    return AB


def consts(k, ctx, P, pref):
    nc = k.nc
    ident = ctx.enter_context(nc.sbuf_tensor(pref + "ident", [128, 128], BF16))
    eps_t = ctx.enter_context(nc.sbuf_tensor(pref + "eps", [128, 1], F32))
    P.add('gpsimd', lambda e: e.memset(ident[:], 0.0), w=['ident'])
    P.add('gpsimd', lambda e: e.affine_select(out=ident[:], in_=ident[:], pattern=[[-1, 128]], compare_op=ALU.not_equal, fill=1.0,
                                              base=0, channel_multiplier=1), w=['ident'])
    P.add('gpsimd', lambda e: e.memset(eps_t[:], 1e-6), w=['eps'])
    return ident, eps_t


def norm_ops(P, xt_chunks, xkey, ssq, rstd, xn_chunks, eps_t, s):
    n = len(xt_chunks)
    for j, (xc, xo) in enumerate(zip(xt_chunks, xn_chunks)):
        np_ = xc.shape[0]
        P.add('scalar', lambda e, xc=xc, xo=xo, j=j, np_=np_: e.activation(out=xo, in_=xc, func=AF.Square, accum_out=ssq[0:np_, j:j + 1]),
              r=[xkey], w=[('xn', s), ('ssq', s)])
    P.add('scalar', lambda e: e.activation(out=rstd[:, 0:n], in_=ssq[:, 0:n], func=AF.Sqrt, scale=1.0 / D, bias=eps_t[:, 0:1]),
          r=[('ssq', s), 'eps'], w=[('rstd', s)])
    P.add('vector', lambda e: e.reciprocal(out=rstd[:, 0:n], in_=rstd[:, 0:n]), r=[('rstd', s)], w=[('rstd', s)])
    for j, (xc, xo) in enumerate(zip(xt_chunks, xn_chunks)):
        np_ = xc.shape[0]
        P.add('gpsimd', lambda e, xc=xc, xo=xo, j=j, np_=np_: e.tensor_scalar(out=xo, in0=xc, scalar1=rstd[0:np_, j:j + 1], scalar2=None, op0=ALU.mult),
              r=[xkey, ('rstd', s)], w=[('xn', s)])


def emit_norm_T(P, xcs, xkey, ssq, rstd, xos, eps_t, ident, pT, A, B, hx, nk):
    n = len(xcs)
    for j in range(n):
        P.add('scalar', lambda e, j=j: e.activation(out=xos[j], in_=xcs[j], func=AF.Square, accum_out=ssq[:, j:j + 1]),
              r=[xkey], w=[nk + ('xn',), nk + ('ssq',)])
    P.add('scalar', lambda e: e.activation(out=rstd[:, 0:n], in_=ssq[:, 0:n], func=AF.Sqrt, scale=1.0 / D, bias=eps_t[:, 0:1]),
          r=[nk + ('ssq',), 'eps'], w=[nk + ('rstd',)])
    P.add('vector', lambda e: e.reciprocal(out=rstd[:, 0:n], in_=rstd[:, 0:n]), r=[nk + ('rstd',)], w=[nk + ('rstd',)])
    for j in range(n):
        P.add('scalar', lambda e, j=j: e.activation(out=xos[j], in_=xcs[j], func=AF.Identity, scale=rstd[:, j:j + 1]),
              r=[xkey, nk + ('rstd',)], w=[nk + ('xn',)])
    for pr in range(4):
        bk = pr % 2
        with atom_of(P):
            for fc in (2 * pr, 2 * pr + 1):
                q = fc % 4
                for j in range(n):
                    P.add('tensor', lambda e, q=q, j=j, fc=fc: e.transpose(out=pT[:, q, j * 128:(j + 1) * 128], in_=xos[j][:, fc * 128:(fc + 1) * 128],
                                                                           identity=ident[:]),
                          r=[nk + ('xn',), 'ident'], w=[('pT', bk)])
            for fc in (2 * pr, 2 * pr + 1):
                q = fc % 4
                P.add('scalar', lambda e, fc=fc, q=q: e.activation(out=hx[:, fc, 0:128 * n], in_=pT[:, q, 0:128 * n], func=AF.Identity,
                                                                    scale=A[:, fc:fc + 1], bias=B[:, fc:fc + 1]),
                      r=[('pT', bk), 'AB'], w=[nk + ('hx',)])

def stage1a(k, upto=99, ntiles=None, NSEG=1, W=int(os.environ.get('W1A', '4'))):
    nc = k.nc; dr = k.dr
    with ExitStack() as ctx:
        T = lambda n, sh, dt=F32: ctx.enter_context(nc.sbuf_tensor("a_" + n, sh, dt))
        P = Prog(nc)
        ident, eps_t = consts(k, ctx, P, "a_")
        AB = load_AB(k, ctx, P, 0, "a_")
        gp_bc = [T(f"gpbc{v}", [128, 1024]) for v in range(2)]; ps_bc = T("psbc", [128, 1024])
        P.dma('sync', ps_bc[:], dr['pool_scale'][0, :].partition_broadcast(128), w=['psbc'])
        for v in range(2):
            P.dma('sync', gp_bc[v][:], dr['modD'][0, v, 2048:3072].partition_broadcast(128), w=[('gpbc', v)])
            P.add('gpsimd', lambda e, v=v: e.tensor_tensor(out=gp_bc[v][:], in0=gp_bc[v][:], in1=ps_bc[:], op=ALU.mult),
                  r=['psbc'], w=[('gpbc', v)])
        inv = T("inv", [128, NSEG + 1, 2, 4, 2, 8]); msk = T("msk", [128, 2 * (NSEG + 1)])
        for sg in range(NSEG + 1):
            P.dma('sync', inv[:, sg].rearrange("p b c d e -> p (b c d e)"), dr['inv'][sg, :].partition_broadcast(128), w=['inv'])
        P.dma('sync', msk[:], dr['msk'][0, :].partition_broadcast(128), w=['msk'])
        wg = T("wg", [128, 4, 2, 256], BF16)
        P.dma('gpsimd', wg[:], dr['pool_w'].rearrange("g (c p) e -> p g c e", p=128), w=['wg'])
        xt = [T(f"xt{s}", [128, 2, 1024]) for s in range(W)]; xh = [T(f"xh{s}", [128, 1024]) for s in range(W)]
        ssq = [T(f"ssq{s}", [128, 3]) for s in range(W)]; rstd = [T(f"rstd{s}", [128, 3]) for s in range(W)]
        xn = [T(f"xn{s}", [128, 3, 1024], BF16) for s in range(W)]
        hxT = [T(f"hxT{s}", [128, 8, 272], BF16) for s in range(W)]
        SA = [T(f"SA{s}", [128, 2, 272]) for s in range(W)]; SB = [T(f"SB{s}", [128, 2, 272]) for s in range(W)]
        te = [T(f"te{s}", [128, 2, 8]) for s in range(W)]
        dT = [T(f"dT{s}", [128, 8, 256], BF16) for s in range(W)]
        tmp = [T(f"tmp{s}", [128, 1024]) for s in range(2)]
        ssq2 = [T(f"ssq2{s}", [128, 2]) for s in range(W)]; rstd2 = [T(f"rstd2{s}", [128, 2]) for s in range(W)]
        xn2 = [T(f"xn2{s}", [128, 2, 1024], BF16) for s in range(W)]; hx2 = [T(f"hx2{s}", [128, 8, 256], BF16) for s in range(W)]
        pT = ctx.enter_context(nc.psum_tensor("a_pT", [128, 4, 512], BF16))
        pD = [ctx.enter_context(nc.psum_tensor(f"a_pD{s}", [128, 1024], F32)) for s in range(2)]
        for s in range(W):
            P.add('gpsimd', lambda e, s=s: e.memset(ssq[s][:], 1.0), w=[('ssq', s)])
            P.add('gpsimd', lambda e, s=s: e.memset(xh[s][:], 0.0), w=[('xh', s)])
            P.add('gpsimd', lambda e, s=s: e.memset(xn[s][:, 2, :], 0.0), w=[('xn', s)])
        tiles = [(0, sg, t) for sg in range(NSEG) for t in range(NT)] + [(1, NSEG, 0)]
        if ntiles: tiles = tiles[:ntiles]
        recs = []
        for ti, (v, sg, t) in enumerate(tiles):
            s = ti % W
            R = Rec(); recs.append(R)
            src = dr['xin'][sg] if v == 0 else dr['ctxin']; dst = dr['xaD'][sg * 4096:(sg + 1) * 4096, :] if v == 0 else dr['caD']
            r0 = 256 * t
            R.dma('sync', xt[s][:], src[r0 + 8:r0 + 264, :].rearrange("(j p) d -> p j d", p=128), w=[('xt', s)])
            R.dma('sync', xh[s][0:8, :], src[r0:r0 + 8, :], w=[('xh', s)])
            R.dma('sync', xh[s][32:40, :], src[r0 + 264:r0 + 272, :], w=[('xh', s)])
            xcs = [xt[s][:, 0, :], xt[s][:, 1, :], xh[s][:, :]]; xos = [xn[s][:, 0, :], xn[s][:, 1, :], xn[s][:, 2, :]]
            for j in range(3):
                np_ = xcs[j].shape[0]
                R.add('scalar', lambda e, j=j, np_=np_, s=s, xcs=xcs, xos=xos: e.activation(out=xos[j], in_=xcs[j], func=AF.Square,
                                                                                      accum_out=ssq[s][0:np_, j:j + 1]),
                      r=[('xt', s), ('xh', s)], w=[('xn', s), ('ssq', s)])
            R.add('scalar', lambda e, s=s: e.activation(out=rstd[s][:], in_=ssq[s][:], func=AF.Sqrt, scale=1.0 / D, bias=eps_t[:, 0:1]),
                  r=[('ssq', s), 'eps'], w=[('rstd', s)])
            R.add('vector', lambda e, s=s: e.reciprocal(out=rstd[s][:], in_=rstd[s][:]), r=[('rstd', s)], w=[('rstd', s)])
            for j in range(3):
                np_ = xcs[j].shape[0]
                R.add('scalar', lambda e, j=j, np_=np_, s=s, xcs=xcs, xos=xos: e.activation(out=xos[j], in_=xcs[j], func=AF.Identity,
                                                                                      scale=rstd[s][0:np_, j:j + 1]),
                      r=[('xt', s), ('xh', s), ('rstd', s)], w=[('xn', s)])
            if upto < 1: continue
            for pr in range(4):
              bk = pr % 2
              with R.atom():
                for fc in (2 * pr, 2 * pr + 1):
                    q = fc % 4
                    for jj in range(3):
                        R.add('tensor', lambda e, s=s, q=q, jj=jj, fc=fc: e.transpose(out=pT[:, q, jj * 128:(jj + 1) * 128],
                                                                                 in_=xn[s][:, jj, fc * 128:(fc + 1) * 128], identity=ident[:]),
                              r=[('xn', s), 'ident'], w=[('pT', bk)])
                for fc in (2 * pr, 2 * pr + 1):
                    q = fc % 4
                    A_ = AB[:, v, 0, 0, fc:fc + 1]; B_ = AB[:, v, 0, 1, fc:fc + 1]
                    R.add('scalar', lambda e, s=s, fc=fc, q=q, A_=A_, B_=B_: e.activation(out=hxT[s][:, fc, 8:264], in_=pT[:, q, 0:256], func=AF.Identity,
                                                                                      scale=A_, bias=B_),
                          r=[('pT', bk), 'AB'], w=[('hxT', s)])
                    for (o0, i0) in ((0, 256), (264, 288)):
                        R.add('scalar', lambda e, s=s, fc=fc, q=q, A_=A_, B_=B_, o0=o0, i0=i0: e.activation(out=hxT[s][:, fc, o0:o0 + 8], in_=pT[:, q, i0:i0 + 8],
                                                                                                  func=AF.Identity, scale=A_, bias=B_),
                              r=[('pT', bk), 'AB'], w=[('hxT', s)])
            if upto < 2: continue
            first = (t == 0); last = (v == 1) or (t == NT - 1)
            if first:
                R.add('vector', lambda e, s=s, sg=sg: e.tensor_scalar(out=hxT[s][:, :, 0:8], in0=hxT[s][:, :, 0:8], scalar1=msk[:, 2 * sg:2 * sg + 1],
                                                                     scalar2=None, op0=ALU.mult), r=['msk'], w=[('hxT', s)])
            if last:
                R.add('vector', lambda e, s=s, sg=sg: e.tensor_scalar(out=hxT[s][:, :, 264:272], in0=hxT[s][:, :, 264:272], scalar1=msk[:, 2 * sg + 1:2 * sg + 2],
                                                                     scalar2=None, op0=ALU.mult), r=['msk'], w=[('hxT', s)])
            for g in range(4):
                h = hxT[s][:, 2 * g:2 * g + 2, :]
                bufs = [SA[s], SB[s]]
                R.add('vector', lambda e, s=s, h=h: e.tensor_tensor(out=SA[s][:, :, 1:272], in0=h[:, :, 0:271], in1=h[:, :, 1:272], op=ALU.add),
                      r=[('hxT', s)], w=[('S', s)])
                cur = 0
                steps = [(1, 2, 271), (2, 4, 269), (4, 8, 265)]
                for (sh_, lo, hi) in steps[:g]:
                    a = bufs[cur]; b = bufs[1 - cur]
                    R.add('vector', lambda e, a=a, b=b, sh_=sh_, lo=lo, hi=hi: e.tensor_tensor(out=b[:, :, lo:hi], in0=a[:, :, lo - sh_:hi - sh_],
                                                                                         in1=a[:, :, lo + sh_:hi + sh_], op=ALU.add),
                          r=[('S', s)], w=[('S', s)])
                    cur = 1 - cur
                Sf = bufs[cur]; wv = 1.0 / (2 << g)
                R.add('vector', lambda e, s=s, g=g, Sf=Sf, h=h, wv=wv: e.scalar_tensor_tensor(out=dT[s][:, 2 * g:2 * g + 2, :], in0=Sf[:, :, 8:264], scalar=wv,
                                                                                         in1=h[:, :, 8:264], op0=ALU.mult, op1=ALU.subtract),
                      r=[('S', s), ('hxT', s)], w=[('dT', s)])
                for (flag, eidx, c0) in ((first, 0, 0), (last, 1, 248)):
                    if not flag: continue
                    R.add('vector', lambda e, s=s, g=g, Sf=Sf, sg=sg, eidx=eidx, c0=c0: e.tensor_tensor(out=te[s][:], in0=Sf[:, :, 8 + c0:16 + c0],
                                                                                                in1=inv[:, sg, eidx, g, :, :], op=ALU.mult),
                          r=[('S', s), 'inv'], w=[('te', s)])
                    R.add('vector', lambda e, s=s, g=g, h=h, c0=c0: e.tensor_tensor(out=dT[s][:, 2 * g:2 * g + 2, c0:c0 + 8], in0=te[s][:],
                                                                                in1=h[:, :, 8 + c0:16 + c0], op=ALU.subtract),
                          r=[('te', s), ('hxT', s)], w=[('dT', s)])
            if upto < 3: continue
            for j in range(2):
              with R.atom():
                for g in range(4):
                    for c in range(2):
                        R.add('tensor', lambda e, s=s, j=j, g=g, c=c: e.matmul(pD[j][:, g * 256:(g + 1) * 256], lhsT=dT[s][:, 2 * g + c, j * 128:(j + 1) * 128],
                                                                              rhs=wg[:, g, c, :], start=(c == 0), stop=(c == 1)),
                              r=[('dT', s), 'wg'], w=[('pD', j)])
                R.add('vector', lambda e, s=s, j=j, v=v: e.tensor_tensor(out=tmp[j][:], in0=pD[j][:], in1=gp_bc[v][:], op=ALU.mult),
                      r=[('pD', j), ('gpbc', v)], w=[('tmp', j)])
                R.add('vector', lambda e, s=s, j=j: e.tensor_tensor(out=xt[s][:, j, :], in0=xt[s][:, j, :], in1=tmp[j][:], op=ALU.add),
                      r=[('tmp', j)], w=[('xt', s)])
            R.dma('sync', dst[r0:r0 + 256, :].rearrange("(j p) d -> p j d", p=128), xt[s][:], r=[('xt', s)], w=['xaD'])
            hdst = dr['hxD'][:, :, sg * 4096:(sg + 1) * 4096] if v == 0 else dr['hcD']
            emit_norm_T(R, [xt[s][:, 0, :], xt[s][:, 1, :]], ('xt', s), ssq2[s], rstd2[s], [xn2[s][:, 0, :], xn2[s][:, 1, :]], eps_t, ident, pT,
                        AB[:, v, 1, 0, :], AB[:, v, 1, 1, :], hx2[s], ('n2', s))
            R.dma('sync', hdst[:, :, r0:r0 + 256].rearrange("c p t -> p c t"), hx2[s][:], r=[('n2', s, 'hx')], w=['hxD'])
        interleave(P, recs, W)
        P.emit(ctx)


def ffn_pass(k, pref, units, tiles, g2_specs, gatesD=None):
    nc = k.nc; dr = k.dr; HF = 11; HW = 1408
    with ExitStack() as ctx:
        T = lambda n, sh, dt=F32: ctx.enter_context(nc.sbuf_tensor(pref + n, sh, dt))
        P = Prog(nc)
        w1b = [T(f"w1b{i}", [128, 8, HW], BF16) for i in range(2)]; w3b = [T(f"w3b{i}", [128, 8, HW], BF16) for i in range(2)]
        w2b = [T(f"w2b{i}", [128, HF, 1024], BF16) for i in range(2)]
        g2bc = {v: T(f"g2bc{v}", [128, 1024]) for v in g2_specs}
        for v, src in g2_specs.items():
            P.dma('sync', g2bc[v][:], src.partition_broadcast(128), w=[('g2bc', v)])
        gates = None
        if gatesD is not None:
            gates = T("gates", [128, 32, 8])
            P.dma('sync', gates[:], gatesD.rearrange("(c p) e -> p c e", p=128), w=['gates'])
        hx = [T(f"hx{i}", [128, 8, 256], BF16) for i in range(3)]
        gT = T("gT", [128, HF, 256], BF16); sil = [T(f"sil{i}", [128, 256], BF16) for i in range(2)]
        tmp = [T(f"tmp{i}", [128, 512]) for i in range(4)]
        ph = [ctx.enter_context(nc.psum_tensor(f"{pref}ph{i}", [128, 2, 256], F32)) for i in range(2)]
        pD = [ctx.enter_context(nc.psum_tensor(f"{pref}pD{i}", [128, 512], F32)) for i in range(2)]
        nh = 0; nd = 0
        def loadw(u):
            (w1, w3, w2, ex) = units[u]; b = u % 2
            for dc in range(8):
                P.dma('gpsimd', w1b[b][:, dc, :], w1[dc * 128:(dc + 1) * 128, :], w=[('w1', b, dc)])
                P.dma('gpsimd', w3b[b][:, dc, :], w3[dc * 128:(dc + 1) * 128, :], w=[('w3', b, dc)])
            for f in range(HF):
                P.dma('gpsimd', w2b[b][:, f, :], w2[f * 128:(f + 1) * 128, :], w=[('w2', b, f)])
        loadw(0)
        for u, (w1, w3, w2, ex) in enumerate(units):
            b = u % 2
            if u + 1 < len(units): loadw(u + 1)
            for (hsrc, odst, v, ch0) in tiles:
                hs = nh % 3; nh += 1
                P.dma('sync', hx[hs][:], hsrc.rearrange("c p t -> p c t"), w=[('hx', hs)])
                for f in range(HF):
                    pb = f % 2
                    for (wb, wk, col) in ((w1b, 'w1', 0), (w3b, 'w3', 1)):
                        for dc in range(8):
                            P.add('tensor', lambda e, wb=wb, b=b, dc=dc, f=f, pb=pb, col=col, hs=hs: e.matmul(
                                ph[pb][:, col, :], lhsT=wb[b][:, dc, f * 128:(f + 1) * 128], rhs=hx[hs][:, dc, :], start=(dc == 0), stop=(dc == 7)),
                                r=[(wk, b, dc), ('hx', hs)], w=[('ph', pb)])
                    P.add('scalar', lambda e, pb=pb: e.activation(out=sil[pb][:], in_=ph[pb][:, 0, :], func=AF.Silu), r=[('ph', pb)], w=[('sil', pb)])
                    P.add('vector', lambda e, pb=pb, f=f: e.tensor_tensor(out=gT[:, f, :], in0=sil[pb][:], in1=ph[pb][:, 1, :], op=ALU.mult),
                          r=[('sil', pb), ('ph', pb)], w=[('gT', f)])
                for j in range(2):
                    for half in range(2):
                        db = nd % 2; ts = nd % 4; nd += 1
                        for f in range(HF):
                            P.add('tensor', lambda e, db=db, f=f, j=j, half=half, b=b: e.matmul(
                                pD[db][:], lhsT=gT[:, f, j * 128:(j + 1) * 128], rhs=w2b[b][:, f, half * 512:(half + 1) * 512], start=(f == 0), stop=(f == HF - 1)),
                                r=[('gT', f), ('w2', b, f)], w=[('pD', db)])
                        if gates is None:
                            P.add('vector', lambda e, db=db, ts=ts, half=half, v=v: e.tensor_tensor(out=tmp[ts][:], in0=pD[db][:],
                                                                                           in1=g2bc[v][:, half * 512:(half + 1) * 512], op=ALU.mult),
                                  r=[('pD', db), ('g2bc', v)], w=[('tmp', ts)])
                        else:
                            P.add('vector', lambda e, db=db, ts=ts, half=half, v=v, c=ch0 + j, ex=ex: e.scalar_tensor_tensor(
                                out=tmp[ts][:], in0=pD[db][:], scalar=gates[:, c, ex:ex + 1], in1=g2bc[v][:, half * 512:(half + 1) * 512],
                                op0=ALU.mult, op1=ALU.mult), r=[('pD', db), ('g2bc', v), 'gates'], w=[('tmp', ts)])
                        P.dma('gpsimd', odst[j * 128:(j + 1) * 128, half * 512:(half + 1) * 512], tmp[ts][:], r=[('tmp', ts)],
                              w=[('out', odst.name, ch0 + j, half)], accum_op=ALU.add)
        P.emit(ctx)


def stage1b(k, NSEG=1):
    dr = k.dr
    units = [(dr['ffn_w1'][:, h * 1408:(h + 1) * 1408], dr['ffn_w3'][:, h * 1408:(h + 1) * 1408], dr['ffn_w2'][h * 1408:(h + 1) * 1408, :], 0)
             for h in range(2)]
    tiles = [(dr['hxD'][:, :, 256 * t:256 * (t + 1)], dr['xaD'][256 * t:256 * (t + 1), :], 0, 2 * t) for t in range(NT * NSEG)]
    tiles.append((dr['hcD'][:, :, :], dr['caD'][:, :], 1, 2 * NT * NSEG))
    ffn_pass(k, "b_", units, tiles, {0: dr['modD'][0, 0, 5120:6144], 1: dr['modD'][0, 1, 5120:6144]})


def stage3(k, NQB=8, NKC=130, A_LIST=(0, 1)):
    nc = k.nc; dr = k.dr; NK = NKC * 128
    with ExitStack() as ctx:
        T = lambda n, sh, dt=F32: ctx.enter_context(nc.sbuf_tensor("c_" + n, sh, dt))
        P = Prog(nc)
        KT = T("KT", [128, NK], BF16); V = T("V", [128, NKC, 2, 65], BF16)
        wo = T("wo", [64, 16, 1024], BF16); wof = T("wof", [64, 2, 1024]); g1bc = T("g1bc", [64, 1024])
        QT = [T(f"QT{i}", [128, 4, 512], BF16) for i in range(2)]
        PT = [T(f"PT{i}", [128, 1024], BF16) for i in range(3)]
        oS = T("oS", [65, 2, 512]); attnT = T("attnT", [64, 8, 512], BF16)
        ones = T("ones", [65, 64]); tmp = [T(f"tmp{i}", [128, 512]) for i in range(2)]
        ps = [ctx.enter_context(nc.psum_tensor(f"c_ps{i}", [128, 1024], F32)) for i in range(2)]
        po = [ctx.enter_context(nc.psum_tensor(f"c_po{i}", [128, 512], F32)) for i in range(2)]
        pbc = ctx.enter_context(nc.psum_tensor("c_pbc", [128, 512], F32))
        pw = ctx.enter_context(nc.psum_tensor("c_pw", [128, 512], F32))
        P.add('gpsimd', lambda e: e.memset(ones[:], 1.0), w=['ones'])
        P.dma('sync', g1bc[:], dr['modD'][1, 0, 2048:3072].partition_broadcast(64), w=['g1bc'])
        for h in range(16):
            sl = h % 2
            P.dma('sync', wof[:, sl, :], dr['w_o'][h * 64:(h + 1) * 64, :], w=[('wof', sl)])
            P.add('vector', lambda e, h=h, sl=sl: e.tensor_tensor(out=wo[:, h, :], in0=wof[:, sl, :], in1=g1bc[:], op=ALU.mult),
                  r=[('wof', sl), 'g1bc'], w=['wo'])
        nq = 0; nps = 0; npt = 0; ntmp = 0
        for a in A_LIST:
            P.dma('sync', KT[:, :], dr['KTD'][2 * a:2 * a + 2, :, :].rearrange("b d t -> (b d) t"), w=['KT'])
            for c0 in range(0, NKC, 26):
                c1 = min(NKC, c0 + 26)
                P.dma('sync', V[:, c0:c1, :, :], dr['VD'][c0 * 128:c1 * 128, 2 * a:2 * a + 2, :].rearrange("(c p) g e -> p c g e", p=128), w=['V'])
            for qb in range(NQB):
                qs = nq % 2; nq += 1
                P.dma('sync', QT[qs][:], dr['QTD'][a * 8:(a + 1) * 8, :, qb * 512:(qb + 1) * 512].rearrange("(i b) d t -> (b d) i t", b=2), w=[('QT', qs)])
                for i in range(4):
                    def qk(kc, s):
                        for b in range(2):
                            P.add('tensor', lambda e, b=b, kc=kc, s=s, qs=qs, i=i: e.matmul(
                                ps[s][:, b * 512:(b + 1) * 512], lhsT=KT[b * 64:(b + 1) * 64, kc * 128:(kc + 1) * 128],
                                rhs=QT[qs][b * 64:(b + 1) * 64, i, :], start=True, stop=True),
                                r=['KT', ('QT', qs)], w=[('ps', s)])
                    s0 = nps % 2; nps += 1
                    qk(0, s0); cur = s0
                    for kc in range(NKC):
                        t = npt % 3; npt += 1
                        if kc + 1 < NKC:
                            s1 = nps % 2; nps += 1
                            qk(kc + 1, s1)
                        P.add('scalar', lambda e, cur=cur, t=t: e.activation(out=PT[t][:], in_=ps[cur][:], func=AF.Exp, scale=0.125),
                              r=[('ps', cur)], w=[('PT', t)])
                        for b in range(2):
                            P.add('tensor', lambda e, b=b, kc=kc, t=t: e.matmul(
                                po[b][0:65, :], lhsT=V[:, kc, b, :], rhs=PT[t][:, b * 512:(b + 1) * 512], start=(kc == 0), stop=(kc == NKC - 1)),
                                r=['V', ('PT', t)], w=[('po', b)])
                        if kc + 1 < NKC: cur = s1
                    for b in range(2):
                        P.add('vector', lambda e, b=b: e.tensor_copy(out=oS[:, b, :], in_=po[b][0:65, :]), r=[('po', b)], w=[('oS', b)])
                        P.add('vector', lambda e, b=b: e.reciprocal(out=oS[64:65, b, :], in_=oS[64:65, b, :]), r=[('oS', b)], w=[('oS', b)])
                        P.add('tensor', lambda e, b=b: e.matmul(pbc[0:64, :], lhsT=ones[64:65, :], rhs=oS[64:65, b, :], start=True, stop=True),
                              r=[('oS', b), 'ones'], w=['pbc'])
                        P.add('vector', lambda e, b=b, i=i: e.tensor_tensor(out=attnT[:, i * 2 + b, :], in0=oS[0:64, b, :], in1=pbc[0:64, :], op=ALU.mult),
                              r=[('oS', b), 'pbc'], w=[('attnT', i * 2 + b)])
                for j in range(4):
                    for half in range(2):
                        for hh in range(8):
                            i_, b_ = hh // 2, hh % 2; h = a * 8 + b_ * 4 + i_
                            P.add('tensor', lambda e, hh=hh, h=h, j=j, half=half: e.matmul(
                                pw[:, :], lhsT=attnT[:, hh, j * 128:(j + 1) * 128], rhs=wo[:, h, half * 512:(half + 1) * 512], start=(hh == 0), stop=(hh == 7)),
                                r=[('attnT', hh), 'wo'], w=['pw'])
                        ts = ntmp % 2; ntmp += 1
                        P.add('vector', lambda e, ts=ts: e.tensor_copy(out=tmp[ts][:], in_=pw[:, :]), r=['pw'], w=[('tmp', ts)])
                        r0 = qb * 512 + j * 128
                        P.dma('gpsimd', dr['xaD'][r0:r0 + 128, half * 512:(half + 1) * 512], tmp[ts][:], r=[('tmp', ts)],
                              w=[('xo', qb, j, half)], accum_op=ALU.add)
        P.emit(ctx)


def vw(t, off, dims):
    b = t[:]
    return bass.AP(tensor=b.tensor, offset=b.offset + off, ap=[list(b.ap[0])] + [list(d) for d in dims])


def stage2(k, ntiles=None, NSEG=1, W=2):
    nc = k.nc; dr = k.dr
    with ExitStack() as ctx:
        T = lambda n, sh, dt=F32: ctx.enter_context(nc.sbuf_tensor("q_" + n, sh, dt))
        P = Prog(nc)
        ident, eps_t = consts(k, ctx, P, "q_")
        AB = load_AB(k, ctx, P, 1, "q_")
        wq = T("wq", [128, 8, 1536], BF16)
        for dc in range(8):
            for a in range(2):
                for b in range(2):
                    P.dma('gpsimd', vw(wq, dc * 1536 + a * 512 + b * 64, [[128, 4], [1, 64]]),
                          dr['w_qkv'][dc * 128:(dc + 1) * 128, a * 512 + b * 256:a * 512 + (b + 1) * 256].rearrange("p (i d) -> p i d", d=64), w=[('wq', dc)])
            P.dma('gpsimd', wq[:, dc, 1024:1536], dr['w_qkv'][dc * 128:(dc + 1) * 128, 1024:1536], w=[('wq', dc)])
        gbc = T("gbc", [128, 1280])
        P.dma('sync', gbc[:], dr['qkg'][0, :].partition_broadcast(128), w=['gbc'])
        xt = [T(f"xt{s}", [128, 2, 1024]) for s in range(W)]
        ssq = [T(f"ssq{s}", [128, 2]) for s in range(W)]; rstd = [T(f"rstd{s}", [128, 2]) for s in range(W)]
        xn = [T(f"xn{s}", [128, 2, 1024], BF16) for s in range(W)]; hx = [T(f"hx{s}", [128, 8, 256], BF16) for s in range(W)]
        cs = [T(f"cs{s}", [128, 2, 2, 64]) for s in range(W)]
        qk_ = [[T(f"qk{s}{j}", [128, 1536]) for j in range(2)] for s in range(W)]; sq_ = [[T(f"sq{s}{j}", [128, 1280]) for j in range(2)] for s in range(W)]
        rh_ = [[T(f"rh{s}{j}", [128, 20]) for j in range(2)] for s in range(W)]; qn_ = [[T(f"qn{s}{j}", [128, 1280]) for j in range(2)] for s in range(W)]
        t2_ = [[T(f"t2{s}{j}", [128, 1280]) for j in range(2)] for s in range(W)]; qr_ = [[T(f"qr{s}{j}", [128, 1280], BF16) for j in range(2)] for s in range(W)]
        vs = [T(f"vs{s}", [128, 2, 4, 65], BF16) for s in range(W)]
        qT = [T(f"qT{s}", [128, 10, 2, 128], BF16) for s in range(W)]
        pT = ctx.enter_context(nc.psum_tensor("q_pT", [128, 4, 512], BF16))
        pq = [ctx.enter_context(nc.psum_tensor(f"q_pq{i}", [128, 512], F32)) for i in range(3)]
        for s in range(W):
            P.add('gpsimd', lambda e, s=s: e.memset(vs[s][:], 1.0), w=[('vs', s)])
        tiles = [(0, t) for t in range(NT * NSEG)] + [(1, 0)]
        if ntiles: tiles = tiles[:ntiles - 1] + [(1, 0)]
        def tile_body(P, ti, v, t):
            s = ti % W
            qk, sq, rh, qn, t2, qr = qk_[s], sq_[s], rh_[s], qn_[s], t2_[s], qr_[s]
            src = dr['xaD'] if v == 0 else dr['caD']
            r0 = 256 * t; k0 = r0 if v == 0 else 4096 * NSEG
            P.dma('sync', xt[s][:], src[r0:r0 + 256, :].rearrange("(j p) d -> p j d", p=128), w=[('xt', s)])
            P.dma('sync', cs[s][:, :, 0, :], dr['ropeC'][k0:k0 + 256, :].rearrange("(j p) e -> p j e", p=128), w=[('cs', s)])
            P.dma('sync', cs[s][:, :, 1, :], dr['ropeS'][k0:k0 + 256, :].rearrange("(j p) e -> p j e", p=128), w=[('cs', s)])
            emit_norm_T(P, [xt[s][:, 0, :], xt[s][:, 1, :]], ('xt', s), ssq[s], rstd[s], [xn[s][:, 0, :], xn[s][:, 1, :]], eps_t, ident, pT,
                        AB[:, v, 0, 0, :], AB[:, v, 0, 1, :], hx[s], ('n', s))
            full = (v == 0 and t < NT)
            c0 = 0 if full else 1024; h0 = c0 // 64; nh = 20 - h0; W_ = 1280 - c0
            for j in range(2):
                for cb in (range(3) if full else (2,)):
                  with P.atom():
                    for dc in range(8):
                        P.add('tensor', lambda e, s=s, j=j, cb=cb, dc=dc: e.matmul(pq[cb][:], lhsT=hx[s][:, dc, j * 128:(j + 1) * 128],
                                                                                 rhs=wq[:, dc, cb * 512:(cb + 1) * 512], start=(dc == 0), stop=(dc == 7)),
                              r=[('n', s, 'hx'), ('wq', dc)], w=[('pq', cb)])
                    P.add('scalar', lambda e, j=j, cb=cb: e.activation(out=qk[j][:, cb * 512:(cb + 1) * 512], in_=pq[cb][:], func=AF.Identity),
                          r=[('pq', cb)], w=[('qk', s, j)])
                Q = ('qk', s, j)
                hd = lambda ap: ap.rearrange("p (h d) -> p h d", d=64)
                P.add('scalar', lambda e, j=j, c0=c0: e.activation(out=sq[j][:, c0:1280], in_=qk[j][:, c0:1280], func=AF.Square), r=[Q], w=[('sq', s, j)])
                P.add('vector', lambda e, j=j, c0=c0, h0=h0: e.tensor_reduce(out=rh[j][:, h0:20], in_=hd(sq[j][:, c0:1280]), axis=AX.X, op=ALU.add),
                      r=[('sq', s, j)], w=[('rh', s, j)])
                P.add('scalar', lambda e, j=j, h0=h0: e.activation(out=rh[j][:, h0:20], in_=rh[j][:, h0:20], func=AF.Sqrt, scale=1.0 / 64, bias=eps_t[:, 0:1]),
                      r=[('rh', s, j), 'eps'], w=[('rh', s, j)])
                P.add('vector', lambda e, j=j, h0=h0: e.reciprocal(out=rh[j][:, h0:20], in_=rh[j][:, h0:20]), r=[('rh', s, j)], w=[('rh', s, j)])
                P.add('vector', lambda e, j=j, c0=c0, h0=h0, nh=nh: e.tensor_tensor(out=hd(qn[j][:, c0:1280]), in0=hd(qk[j][:, c0:1280]),
                                                                               in1=vw(rh[j], h0, [[1, nh], [0, 64]]), op=ALU.mult), r=[Q, ('rh', s, j)], w=[('qn', s, j)])
                P.add('vector', lambda e, j=j, c0=c0: e.tensor_tensor(out=qn[j][:, c0:1280], in0=qn[j][:, c0:1280], in1=gbc[:, c0:1280], op=ALU.mult),
                      r=['gbc'], w=[('qn', s, j)])
                cofs = j * 128; sofs = j * 128 + 64
                P.add('vector', lambda e, j=j, s=s, sofs=sofs, c0=c0, nh=nh: e.tensor_tensor(out=vw(t2[j], c0, [[64, nh], [32, 2], [1, 16]]),
                                                                                       in0=vw(qn[j], c0 + 16, [[64, nh], [32, 2], [1, 16]]),
                                                                                       in1=vw(cs[s], sofs, [[0, nh], [32, 2], [1, 16]]), op=ALU.mult),
                      r=[('qn', s, j), ('cs', s)], w=[('t2', s, j)])
                P.add('vector', lambda e, j=j, s=s, sofs=sofs, c0=c0, nh=nh: e.tensor_tensor(out=vw(t2[j], c0 + 16, [[64, nh], [32, 2], [1, 16]]),
                                                                                       in0=vw(qn[j], c0, [[64, nh], [32, 2], [1, 16]]),
                                                                                       in1=vw(cs[s], sofs + 16, [[0, nh], [32, 2], [1, 16]]), op=ALU.mult),
                      r=[('qn', s, j), ('cs', s)], w=[('t2', s, j)])
                P.add('vector', lambda e, j=j, s=s, cofs=cofs, c0=c0, nh=nh: e.tensor_tensor(out=hd(qn[j][:, c0:1280]), in0=hd(qn[j][:, c0:1280]),
                                                                                       in1=vw(cs[s], cofs, [[0, nh], [1, 64]]), op=ALU.mult),
                      r=[('cs', s), ('t2', s, j)], w=[('qn', s, j)])
                P.add('vector', lambda e, j=j, c0=c0: e.tensor_tensor(out=qr[j][:, c0:1280], in0=qn[j][:, c0:1280], in1=t2[j][:, c0:1280], op=ALU.add),
                      r=[('t2', s, j)], w=[('qn', s, j), ('qr', s, j)])
                P.add('scalar', lambda e, j=j, s=s: e.activation(out=vs[s][:, j, :, 0:64], in_=qk[j][:, 1280:1536].rearrange("p (g d) -> p g d", d=64), func=AF.Identity),
                      r=[Q], w=[('vs', s)])
                for grp in (range(3) if full else (2,)):
                  with P.atom():
                    rg = (0, 2, 1)[grp]; bk = rg // 2; n = 4 if grp < 2 else 2
                    for u in range(n):
                        sl = grp * 4 + u
                        P.add('tensor', lambda e, j=j, sl=sl, rg=rg, u=u: e.transpose(out=pT[:, rg, u * 128:(u + 1) * 128], in_=qr[j][:, sl * 128:(sl + 1) * 128],
                                                                                identity=ident[:]), r=[('qr', s, j), 'ident'], w=[('pT', bk)])
                    P.add('vector', lambda e, j=j, s=s, grp=grp, rg=rg, n=n: e.tensor_copy(out=qT[s][:, grp * 4:grp * 4 + n, j, :],
                                                                               in_=pT[:, rg, 0:n * 128].rearrange("p (u t) -> p u t", t=128)),
                          r=[('pT', bk)], w=[('qT', s)])
            for j in range(2):
                if v == 0 and t < NT:
                    P.dma('sync', dr['QTD'][:, :, r0 + j * 128:r0 + (j + 1) * 128].rearrange("(hp two) d t -> (two d) hp t", two=2), qT[s][:, 0:8, j, :],
                          r=[('qT', s)], w=['QTD'])
                P.dma('sync', dr['KTD'][:, :, k0 + j * 128:k0 + (j + 1) * 128].rearrange("(a b) d t -> (b d) a t", b=2), qT[s][:, 8:10, j, :],
                      r=[('qT', s)], w=['KTD'])
            P.dma('sync', dr['VD'][k0:k0 + 256, :, :].rearrange("(j p) g e -> p j g e", p=128), vs[s][:], r=[('vs', s)], w=['VD'])
        recs = []
        for ti, (v, t) in enumerate(tiles):
            R = Rec(); recs.append(R); tile_body(R, ti, v, t)
        interleave(P, recs, W)
        P.emit(ctx)


def stage3b(k, ntiles=NT):
    nc = k.nc; dr = k.dr
    with ExitStack() as ctx:
        T = lambda n, sh, dt=F32: ctx.enter_context(nc.sbuf_tensor("r_" + n, sh, dt))
        P = Prog(nc)
        ident, eps_t = consts(k, ctx, P, "r_")
        AB = load_AB(k, ctx, P, 1, "r_")
        RW = T("RW", [128, 8, 1024]); Abc = T("Abc", [128, 1024]); Bbc = T("Bbc", [128, 1024]); gbc = T("gbc", [128, 1024])
        cbc = T("cbc", [128, 8]); junk = T("junk", [128, 1024])
        P.dma('sync', Abc[:], dr['modD'][1, 0, 4096:5120].partition_broadcast(128), w=['Abc'])
        P.dma('sync', Bbc[:], dr['modD'][1, 0, 3072:4096].partition_broadcast(128), w=['Bbc'])
        P.dma('sync', gbc[:], dr['norm_g'][1, 1, :].partition_broadcast(128), w=['gbc'])
        P.add('vector', lambda e: e.scalar_tensor_tensor(out=Abc[:], in0=Abc[:], scalar=1.0, in1=gbc[:], op0=ALU.add, op1=ALU.mult), r=['gbc'], w=['Abc'])
        P.add('gpsimd', lambda e: e.memset(cbc[:], 0.0), w=['cbc'])
        for ex in range(8):
            P.dma('sync', RW[:, ex, :], dr['rwT'][ex, :].partition_broadcast(128), w=[('RW', ex)])
            P.add('vector', lambda e, ex=ex: e.scalar_tensor_tensor(out=junk[:], in0=Bbc[:], scalar=1.0, in1=RW[:, ex, :], op0=ALU.mult, op1=ALU.mult,
                                                                    accum_out=cbc[:, ex:ex + 1]), r=['Bbc', ('RW', ex), 'cbc'], w=['junk', 'cbc'])
            P.add('vector', lambda e, ex=ex: e.tensor_tensor(out=RW[:, ex, :], in0=RW[:, ex, :], in1=Abc[:], op=ALU.mult), r=['Abc'], w=[('RW', ex)])
        xt = [T(f"xt{s}", [128, 2, 1024]) for s in range(2)]
        ssq = [T(f"ssq{s}", [128, 2]) for s in range(2)]; rstd = [T(f"rstd{s}", [128, 2]) for s in range(2)]
        xn = [T(f"xn{s}", [128, 2, 1024], BF16) for s in range(2)]; hx = [T(f"hx{s}", [128, 8, 256], BF16) for s in range(2)]
        raw = [T(f"raw{s}", [128, 2, 8]) for s in range(2)]
        lg = T("lg", [128, 2, 8]); lg2 = T("lg2", [128, 2, 8]); mk1 = T("mk1", [128, 2, 8]); mk2 = T("mk2", [128, 2, 8])
        m1 = T("m1", [128, 2]); m2 = T("m2", [128, 2]); e2 = T("e2", [128, 2]); g1v = T("g1v", [128, 2]); g2v = T("g2v", [128, 2])
        gates = T("gates", [128, 32, 8])
        pT = ctx.enter_context(nc.psum_tensor("r_pT", [128, 4, 512], BF16))
        for s in range(2):
            P.add('gpsimd', lambda e, s=s: e.memset(raw[s][:], 0.0), w=[('raw', s)])
        for t in range(ntiles):
            s = t % 2; r0 = 256 * t
            P.dma('sync', xt[s][:], dr['xaD'][r0:r0 + 256, :].rearrange("(j p) d -> p j d", p=128), w=[('xt', s)])
            emit_norm_T(P, [xt[s][:, 0, :], xt[s][:, 1, :]], ('xt', s), ssq[s], rstd[s], [xn[s][:, 0, :], xn[s][:, 1, :]], eps_t, ident, pT,
                        AB[:, 0, 1, 0, :], AB[:, 0, 1, 1, :], hx[s], ('n', s))
            P.dma('sync', dr['hxD'][:, :, r0:r0 + 256].rearrange("c p t -> p c t"), hx[s][:], r=[('n', s, 'hx')], w=['hxD'])
            for j in range(2):
                for ex in range(8):
                    P.add('vector', lambda e, s=s, j=j, ex=ex: e.scalar_tensor_tensor(out=junk[:], in0=xt[s][:, j, :], scalar=1.0, in1=RW[:, ex, :],
                                                                                    op0=ALU.mult, op1=ALU.mult, accum_out=raw[s][:, j, ex:ex + 1]),
                          r=[('xt', s), ('RW', ex), ('raw', s)], w=['junk', ('raw', s)])
            V = 'vector'
            P.add(V, lambda e, s=s: e.tensor_tensor(out=lg[:], in0=raw[s][:], in1=vw(rstd[s], 0, [[1, 2], [0, 8]]), op=ALU.mult), r=[('raw', s), ('n', s, 'rstd')], w=['lg'])
            P.add('gpsimd', lambda e, s=s: e.memset(raw[s][:], 0.0), r=['lg'], w=[('raw', s)])
            P.add(V, lambda e: e.tensor_tensor(out=lg[:], in0=lg[:], in1=vw(cbc, 0, [[0, 2], [1, 8]]), op=ALU.add), r=['cbc'], w=['lg'])
            P.add(V, lambda e: e.tensor_reduce(out=m1[:], in_=lg[:], axis=AX.X, op=ALU.max), r=['lg'], w=['m1'])
            P.add(V, lambda e: e.tensor_tensor(out=mk1[:], in0=lg[:], in1=vw(m1, 0, [[1, 2], [0, 8]]), op=ALU.is_equal), r=['lg', 'm1'], w=['mk1'])
            P.add(V, lambda e: e.scalar_tensor_tensor(out=lg2[:], in0=mk1[:], scalar=-1e30, in1=lg[:], op0=ALU.mult, op1=ALU.add), r=['mk1', 'lg'], w=['lg2'])
            P.add(V, lambda e: e.tensor_reduce(out=m2[:], in_=lg2[:], axis=AX.X, op=ALU.max), r=['lg2'], w=['m2'])
            P.add(V, lambda e: e.tensor_tensor(out=mk2[:], in0=lg2[:], in1=vw(m2, 0, [[1, 2], [0, 8]]), op=ALU.is_equal), r=['lg2', 'm2'], w=['mk2'])
            P.add(V, lambda e: e.tensor_tensor(out=e2[:], in0=m2[:], in1=m1[:], op=ALU.subtract), r=['m1', 'm2'], w=['e2'])
            P.add('scalar', lambda e: e.activation(out=e2[:], in_=e2[:], func=AF.Exp), r=['e2'], w=['e2'])
            P.add(V, lambda e: e.tensor_scalar(out=g1v[:], in0=e2[:], scalar1=1.0, scalar2=None, op0=ALU.add), r=['e2'], w=['g1v'])
            P.add(V, lambda e: e.reciprocal(out=g1v[:], in_=g1v[:]), r=['g1v'], w=['g1v'])
            P.add(V, lambda e: e.tensor_tensor(out=g2v[:], in0=e2[:], in1=g1v[:], op=ALU.mult), r=['e2', 'g1v'], w=['g2v'])
            P.add(V, lambda e: e.tensor_tensor(out=mk1[:], in0=mk1[:], in1=vw(g1v, 0, [[1, 2], [0, 8]]), op=ALU.mult), r=['g1v'], w=['mk1'])
            P.add(V, lambda e: e.tensor_tensor(out=mk2[:], in0=mk2[:], in1=vw(g2v, 0, [[1, 2], [0, 8]]), op=ALU.mult), r=['g2v'], w=['mk2'])
            P.add(V, lambda e, t=t: e.tensor_tensor(out=gates[:, 2 * t:2 * t + 2, :], in0=mk1[:], in1=mk2[:], op=ALU.add), r=['mk1', 'mk2'], w=['gates'])
        P.dma('sync', dr['gatesD'][0:ntiles * 256, :].rearrange("(c p) e -> p c e", p=128), gates[:, 0:2 * ntiles, :], r=['gates'], w=['gatesD'])
        P.emit(ctx)


def stage4(k, nexp=8):
    dr = k.dr
    units = [(dr['moe_w1'][ex][:, h * 1408:(h + 1) * 1408], dr['moe_w3'][ex][:, h * 1408:(h + 1) * 1408], dr['moe_w2'][ex][h * 1408:(h + 1) * 1408, :], ex)
             for ex in range(nexp) for h in range(2)]
    tiles = [(dr['hxD'][:, :, 256 * t:256 * (t + 1)], dr['xaD'][256 * t:256 * (t + 1), :], 0, 2 * t) for t in range(NT)]
    ffn_pass(k, "m_", units, tiles, {0: dr['modD'][1, 0, 5120:6144]}, gatesD=dr['gatesD'])


def stage5(k, ntiles=NT):
    nc = k.nc; dr = k.dr
    with ExitStack() as ctx:
        T = lambda n, sh, dt=F32: ctx.enter_context(nc.sbuf_tensor("f_" + n, sh, dt))
        P = Prog(nc)
        eps_t = T("eps", [128, 1]); fg = T("fg", [128, 1024])
        P.add('gpsimd', lambda e: e.memset(eps_t[:], 1e-6), w=['eps'])
        P.dma('sync', fg[:], dr['final_g'][0, :].partition_broadcast(128), w=['fg'])
        xt = [T(f"xt{s}", [128, 2, 1024]) for s in range(2)]; ot = [T(f"ot{s}", [128, 2, 1024]) for s in range(2)]
        ssq = [T(f"ssq{s}", [128, 2]) for s in range(2)]; rstd = [T(f"rstd{s}", [128, 2]) for s in range(2)]
        for t in range(ntiles):
            s = t % 2; r0 = 256 * t
            P.dma('sync', xt[s][:], dr['xaD'][r0:r0 + 256, :].rearrange("(j p) d -> p j d", p=128), w=[('xt', s)])
            for j in range(2):
                P.add('scalar', lambda e, s=s, j=j: e.activation(out=ot[s][:, j, :], in_=xt[s][:, j, :], func=AF.Square, accum_out=ssq[s][:, j:j + 1]),
                      r=[('xt', s)], w=[('ot', s), ('ssq', s)])
            P.add('scalar', lambda e, s=s: e.activation(out=rstd[s][:], in_=ssq[s][:], func=AF.Sqrt, scale=1.0 / D, bias=eps_t[:, 0:1]),
                  r=[('ssq', s), 'eps'], w=[('rstd', s)])
            P.add('vector', lambda e, s=s: e.reciprocal(out=rstd[s][:], in_=rstd[s][:]), r=[('rstd', s)], w=[('rstd', s)])
            for j in range(2):
                P.add('vector', lambda e, s=s, j=j: e.scalar_tensor_tensor(out=ot[s][:, j, :], in0=xt[s][:, j, :], scalar=rstd[s][:, j:j + 1], in1=fg[:],
                                                                                              op0=ALU.mult, op1=ALU.mult),
                      r=[('xt', s), ('rstd', s), 'fg'], w=[('ot', s)])
            P.dma('sync', dr['out'][r0:r0 + 256, :].rearrange("(j p) d -> p j d", p=128), ot[s][:], r=[('ot', s)], w=['out'])
        P.emit(ctx)


POOLW=(2,4,8,16)
def edge_row(L, realfirst, reallast):
    inv=np.zeros((2,4,2,8),np.float32)
    for g,w in enumerate(POOLW):
        half=w//2
        for i in range(8):
            t=i; cnt=(min(t+half,L)-max(t-half,0)) if realfirst else w
            inv[0,g,:,i]=1.0/cnt
            t=L-8+i; cnt=(min(t+half,L)-max(t-half,0)) if reallast else w
            inv[1,g,:,i]=1.0/cnt
    return inv.reshape(128), [0.0 if realfirst else 1.0, 0.0 if reallast else 1.0]
def seg_quarters(r, NSEG):
    q=r%4
    if NSEG==1: return [q]
    if NSEG==4: return [q]+[o for o in range(4) if o!=q]
    m=r%2; p=(r%4)//2
    return [2*m+p, 2*m+1-p]
def core_inputs(r, inp, NSEG=1):
    b=r//4; qs=seg_quarters(r,NSEG); x=inp['x'][b]
    xin=np.zeros((NSEG,4112,1024),np.float32); inv=np.zeros((NSEG+1,128),np.float32); msk=np.zeros((1,2*(NSEG+1)),np.float32)
    for sg,q in enumerate(qs):
        lo=q*4096-8; hi=q*4096+4096+8; a=max(lo,0); z=min(hi,16384)
        xin[sg,a-lo:a-lo+(z-a)]=x[a:z]
        inv[sg],msk[0,2*sg:2*sg+2]=edge_row(16384,q==0,q==3)
    inv[NSEG],msk[0,2*NSEG:]=edge_row(256,True,True)
    ctxin=np.zeros((272,1024),np.float32); ctxin[8:264]=inp['ctx'][b]
    cvec=np.stack([inp['c'][b], inp['c_ctx']]).astype(np.float32)
    return dict(xin=xin, ctxin=ctxin, cvec=cvec, inv=inv, msk=msk)
def rope_gain(inp):
    return np.concatenate([np.tile(inp['q_norm_g'][0],16), np.tile(inp['k_norm_g'][0],4)]).astype(np.float32)[None,:]
def rope_tables(r, NSEG=1):
    n=NSEG*4096
    C=np.ones((n+256,64),np.float32); S=np.zeros((n+256,64),np.float32)
    fr=(10000.0**(-np.arange(16,dtype=np.float32)/16)).astype(np.float32)
    for sg,q in enumerate(seg_quarters(r,NSEG)):
        t=np.arange(q*4096,(q+1)*4096); row=(t//64).astype(np.float32); col=(t%64).astype(np.float32)
        ar=row[:,None]*fr; ac=col[:,None]*fr
        C[sg*4096:(sg+1)*4096]=np.concatenate([np.cos(ar),np.cos(ar),np.cos(ac),np.cos(ac)],1)
        S[sg*4096:(sg+1)*4096]=np.concatenate([-np.sin(ar),np.sin(ar),-np.sin(ac),np.sin(ac)],1)
    return {'ropeC':C,'ropeS':S}


NSEG = 4


def _build():
    k = K()
    k.din('xin', [NSEG, 4112, 1024]); k.din('ctxin', [272, 1024]); k.din('cvec', [2, 1024]); k.din('inv', [NSEG + 1, 128]); k.din('msk', [1, 2 * (NSEG + 1)])
    k.din('ada_w', [2, 1024, 6144]); k.din('ada_b', [2, 6144]); k.din('norm_g', [2, 2, 1024]); k.din('pool_w', [4, 256, 256]); k.din('pool_scale', [1, 1024])
    k.din('ffn_w1', [1024, 2816]); k.din('ffn_w3', [1024, 2816]); k.din('ffn_w2', [2816, 1024])
    k.din('w_qkv', [1024, 1536]); k.din('qkg', [1, 1280]); k.din('ropeC', [NSEG * 4096 + 256, 64]); k.din('ropeS', [NSEG * 4096 + 256, 64])
    k.din('w_o', [1024, 1024]); k.din('rwT', [8, 1024]); k.din('final_g', [1, 1024])
    k.din('moe_w1', [8, 1024, 2816]); k.din('moe_w3', [8, 1024, 2816]); k.din('moe_w2', [8, 2816, 1024])
    k.dint('modD', [2, 2, 6144]); k.dint('xaD', [NSEG * 4096, 1024]); k.dint('caD', [256, 1024])
    k.dint('hxD', [8, 128, NSEG * 4096], BF16); k.dint('hcD', [8, 128, 256], BF16)
    k.dint('QTD', [16, 64, 4096], BF16); k.dint('KTD', [4, 64, NSEG * 4096 + 256], BF16); k.dint('VD', [NSEG * 4096 + 256, 4, 65], BF16)
    k.dint('gatesD', [4096, 8]); k.dout('out', [4096, 1024])
    stage0(k); stage1a(k, NSEG=NSEG); stage1b(k, NSEG=NSEG); stage2(k, NSEG=NSEG)
    stage3(k, NKC=(NSEG * 4096 + 256) // 128); stage3b(k); stage4(k); stage5(k)
    return k.nc


def kernel(x, c, ctx, c_ctx, ada_w, ada_b, norm_g, pool_w, pool_scale, ffn_w1, ffn_w3, ffn_w2,
           w_qkv, w_o, q_norm_g, k_norm_g, router_w, moe_w1, moe_w3, moe_w2, final_g):
    f = lambda a: np.ascontiguousarray(np.asarray(a, dtype=np.float32))
    inp = dict(x=f(x), c=f(c), ctx=f(ctx), c_ctx=f(c_ctx), q_norm_g=f(q_norm_g), k_norm_g=f(k_norm_g))
    shared = dict(ada_w=f(ada_w), ada_b=f(ada_b), norm_g=f(norm_g), pool_w=f(pool_w)[0], pool_scale=f(pool_scale),
                  ffn_w1=f(ffn_w1)[0], ffn_w3=f(ffn_w3)[0], ffn_w2=f(ffn_w2)[0], w_qkv=f(w_qkv)[0], qkg=rope_gain(inp),
                  w_o=f(w_o)[0], rwT=np.ascontiguousarray(f(router_w)[0].T), final_g=f(final_g)[None, :],
                  moe_w1=f(moe_w1)[0], moe_w3=f(moe_w3)[0], moe_w2=f(moe_w2)[0])
    maps = [{**core_inputs(r, inp, NSEG), **rope_tables(r, NSEG), **shared} for r in range(8)]
    res = run_bass_kernel_spmd(_build(), maps, core_ids=list(range(8))).results
    out = np.stack([np.concatenate([np.asarray(res[4 * b + q]['out']) for q in range(4)], axis=0) for b in range(2)])
    return out.astype(np.float32)
```

```python
import numpy as np, os
DBG=os.environ.get('DBG','')
from contextlib import ExitStack
import concourse.bass as bass
import concourse.mybir as mybir
from concourse.bass_utils import run_bass_kernel_spmd

F32 = mybir.dt.float32; BF16 = mybir.dt.bfloat16
AF = mybir.ActivationFunctionType; ALU = mybir.AluOpType; AX = mybir.AxisListType

D = 1024; DFF = 2816; NF = 22; SEQ = 16384; CTX = 256; TPC = 4096; TT = 256; NT = TPC // TT
ENGS = ['sync', 'scalar', 'vector', 'gpsimd', 'tensor']
NDSEM = 8
SAME_SYNC = {'gpsimd', 'scalar', 'vector'}
_uid = [0]


class _Op:
    __slots__ = ('eng', 'fn', 'deps', 'dma', 'signal', 'sigval', 'dsem', 'dval', 'prev_dma')

    def __init__(s, eng, fn, dma):
        s.eng = eng; s.fn = fn; s.dma = dma; s.deps = []; s.signal = False; s.sigval = 0
        s.dsem = None; s.dval = 0; s.prev_dma = None


class Prog:
    def __init__(s, nc):
        s.nc = nc; s.ops = {e: [] for e in ENGS}; s.lastw = {}; s.readers = {}
        s.dma_count = {e: 0 for e in ENGS}; s.dma_hist = {e: [] for e in ENGS}

    def add(s, eng, fn, r=(), w=(), dma=False):
        op = _Op(eng, fn, dma); deps = []
        for k in r:
            d = s.lastw.get(k)
            if d is not None: deps.append(d)
        for k in w:
            d = s.lastw.get(k)
            if d is not None: deps.append(d)
            deps.extend(s.readers.get(k, ()))
        seen = set()
        for d in deps:
            if id(d) in seen: continue
            seen.add(id(d))
            if d.dma or d.eng != eng or eng in SAME_SYNC:
                op.deps.append(d)
                if not d.dma: d.signal = True
        for k in w:
            s.lastw[k] = op; s.readers[k] = []
        for k in r:
            s.readers.setdefault(k, []).append(op)
        if dma:
            n = s.dma_count[eng]; s.dma_count[eng] = n + 1
            op.dsem = n % NDSEM; op.dval = 16 * (n // NDSEM + 1)
            h = s.dma_hist[eng]
            if n >= NDSEM: op.prev_dma = h[n - NDSEM]
            h.append(op)
        s.ops[eng].append(op)
        return op

    def dma(s, eng, out, in_, r=(), w=(), **kw):
        return s.add(eng, lambda e: e.dma_start(out=out, in_=in_, **kw), r=r, w=w, dma=True)

    def emit(s, ctx):
        nc = s.nc; _uid[0] += 1; u = _uid[0]
        esem = {e: nc.alloc_semaphore(name=f"es{u}_{e}") for e in ENGS}
        dsem = {e: [nc.alloc_semaphore(name=f"ds{u}_{e}{i}") for i in range(NDSEM)]
                for e in ENGS if s.dma_count[e] > 0}
        allsems = list(esem.values()) + [h for l in dsem.values() for h in l]

        def _cleanup():
            nc.clear_and_free_semaphores(allsems)
            nc.all_engine_barrier()
        ctx.callback(_cleanup)
        for e in ENGS:
            c = 0
            for op in s.ops[e]:
                if op.signal and not op.dma:
                    c += 1; op.sigval = c
        block = ctx.enter_context(nc.Block())

        def mk(ename):
            def body(e):
                waited = {}

                def wait(sem, val, key):
                    if waited.get(key, 0) < val:
                        e.wait_ge(sem, val); waited[key] = val
                for op in s.ops[ename]:
                    for d in op.deps:
                        if d.dma: wait(dsem[d.eng][d.dsem], d.dval, ('d', d.eng, d.dsem))
                        else: wait(esem[d.eng], d.sigval, ('e', d.eng))
                    if op.dma and op.prev_dma is not None:
                        d = op.prev_dma; wait(dsem[d.eng][d.dsem], d.dval, ('d', d.eng, d.dsem))
                    ins = op.fn(e)
                    if op.dma: ins.then_inc(dsem[ename][op.dsem], 16)
                    elif op.signal: ins.then_inc(esem[ename], 1)
                if ename == 'sync':
                    for q in dsem:
                        for d in s.dma_hist[q][-NDSEM:]:
                            wait(dsem[q][d.dsem], d.dval, ('d', q, d.dsem))
            return body
        for ename in ENGS:
            if s.ops[ename] or ename == 'sync':
                getattr(block, ename)(mk(ename))


class Rec:
    def __init__(s): s.l = []; s.cur = None

    def _put(s, op):
        if s.cur is not None: s.cur.append(op)
        else: s.l.append([op])

    def add(s, *a, **k): s._put(('add', a, k))

    def dma(s, *a, **k): s._put(('dma', a, k))

    def atom(s):
        rec = s

        class _A:
            def __enter__(self_):
                assert rec.cur is None; rec.cur = []

            def __exit__(self_, *a):
                rec.l.append(rec.cur); rec.cur = None
        return _A()


def interleave(P, recs, W):
    active = {}; nxt = 0
    while nxt < len(recs) or active:
        while nxt < len(recs) and (nxt % W) not in active:
            active[nxt % W] = iter(recs[nxt].l); nxt += 1
        for sl in sorted(active, key=lambda q: q):
            unit = next(active[sl], None)
            if unit is None:
                del active[sl]; continue
            for op in unit:
                getattr(P, op[0])(*op[1], **op[2])


class _NoAtom:
    def __enter__(s): pass

    def __exit__(s, *a): pass


def atom_of(R):
    return R.atom() if isinstance(R, Rec) else _NoAtom()


class K:
    def __init__(s):
        s.nc = bass.Bass("TRN2", target_bir_lowering=False)
        s.dr = {}

    def din(s, name, shape, dt=F32):
        s.dr[name] = s.nc.dram_tensor(name, list(shape), dt, kind="ExternalInput").ap(); return s.dr[name]

    def dout(s, name, shape, dt=F32):
        s.dr[name] = s.nc.dram_tensor(name, list(shape), dt, kind="ExternalOutput").ap(); return s.dr[name]

    def dint(s, name, shape, dt=F32):
        s.dr[name] = s.nc.dram_tensor(name, list(shape), dt, kind="Internal").ap(); return s.dr[name]


def stage0(k):
    nc = k.nc; dr = k.dr
    with ExitStack() as ctx:
        T = lambda n, sh, dt=F32: ctx.enter_context(nc.sbuf_tensor("s0_" + n, sh, dt))
        cT = T("cT", [128, 8, 2]); scT = T("scT", [128, 8, 2])
        awt = [T(f"aw{i}", [128, 8, 512]) for i in range(2)]
        adab = T("adab", [2, 2, 6144]); modsb = T("mod", [2, 2, 6144])
        pm = [ctx.enter_context(nc.psum_tensor(f"s0_pm{i}", [128, 512], F32)) for i in range(2)]
        P = Prog(nc)
        for v in range(2):
            P.dma('sync', cT[:, :, v], dr['cvec'][v, :].rearrange("(c p) -> p c", p=128), w=['cT'], allow_slow_non_contiguous=True)
        for i in range(2):
            P.dma('gpsimd', adab[:, i, :], dr['ada_b'][i, :].partition_broadcast(2), w=[('adab', i)])
        P.add('scalar', lambda e: e.activation(out=scT[:], in_=cT[:], func=AF.Silu), r=['cT'], w=['scT'])
        n = 0
        for i in range(2):
            for cb in range(12):
                sl = n % 2; n += 1
                P.dma('sync', awt[sl][:], dr['ada_w'][i, :, cb * 512:(cb + 1) * 512].rearrange("(c p) n -> p c n", p=128),
                      w=[('aw', sl)])
                for kc in range(8):
                    P.add('tensor', lambda e, sl=sl, kc=kc: e.matmul(pm[sl][0:2, :], lhsT=scT[:, kc, :], rhs=awt[sl][:, kc, :],
                                                                     start=(kc == 0), stop=(kc == 7)),
                          r=['scT', ('aw', sl)], w=[('pm', sl)])
                P.add('vector', lambda e, sl=sl, i=i, cb=cb: e.tensor_tensor(out=modsb[:, i, cb * 512:(cb + 1) * 512], in0=pm[sl][0:2, :],
                                                                            in1=adab[:, i, cb * 512:(cb + 1) * 512], op=ALU.add),
                      r=[('pm', sl), ('adab', i)], w=['modsb'])
        P.dma('sync', dr['modD'].rearrange("i v n -> v i n"), modsb[:], r=['modsb'], w=['modD'])
        P.emit(ctx)


def load_AB(k, ctx, P, layer, pref):
    nc = k.nc; dr = k.dr
    T = lambda n, sh, dt=F32: ctx.enter_context(nc.sbuf_tensor(pref + n, sh, dt))
    modF = T("modF", [128, 2, 48]); gF = T("gF", [128, 2, 8]); AB = T("AB", [128, 2, 2, 2, 8])
    for v in range(2):
        P.dma('sync', modF[:, v, :], dr['modD'][layer, v, :].rearrange("(kc p) -> p kc", p=128), w=[('modF', v)],
              allow_slow_non_contiguous=True)
    for sub in range(2):
        P.dma('sync', gF[:, sub, :], dr['norm_g'][layer, sub, :].rearrange("(c p) -> p c", p=128), w=['gF'], allow_slow_non_contiguous=True)
    for v in range(2):
        for sub in range(2):
            sh = modF[:, v, (3 * sub) * 8:(3 * sub) * 8 + 8]; sc = modF[:, v, (3 * sub + 1) * 8:(3 * sub + 1) * 8 + 8]
            P.add('vector', lambda e, v=v, sub=sub, sc=sc: e.scalar_tensor_tensor(out=AB[:, v, sub, 0, :], in0=sc, scalar=1.0, in1=gF[:, sub, :],
                                                                              op0=ALU.add, op1=ALU.mult),
                  r=[('modF', v), 'gF'], w=['AB'])
            P.add('vector', lambda e, v=v, sub=sub, sh=sh: e.tensor_copy(out=AB[:, v, sub, 1, :], in_=sh), r=[('modF', v)], w=['AB'])
    return AB


def consts(k, ctx, P, pref):
    nc = k.nc
    ident = ctx.enter_context(nc.sbuf_tensor(pref + "ident", [128, 128], BF16))
    eps_t = ctx.enter_context(nc.sbuf_tensor(pref + "eps", [128, 1], F32))
    P.add('gpsimd', lambda e: e.memset(ident[:], 0.0), w=['ident'])
    P.add('gpsimd', lambda e: e.affine_select(out=ident[:], in_=ident[:], pattern=[[-1, 128]], compare_op=ALU.not_equal, fill=1.0,
                                              base=0, channel_multiplier=1), w=['ident'])
    P.add('gpsimd', lambda e: e.memset(eps_t[:], 1e-6), w=['eps'])
    return ident, eps_t


def norm_ops(P, xt_chunks, xkey, ssq, rstd, xn_chunks, eps_t, s):
    n = len(xt_chunks)
    for j, (xc, xo) in enumerate(zip(xt_chunks, xn_chunks)):
        np_ = xc.shape[0]
        P.add('scalar', lambda e, xc=xc, xo=xo, j=j, np_=np_: e.activation(out=xo, in_=xc, func=AF.Square, accum_out=ssq[0:np_, j:j + 1]),
              r=[xkey], w=[('xn', s), ('ssq', s)])
    P.add('scalar', lambda e: e.activation(out=rstd[:, 0:n], in_=ssq[:, 0:n], func=AF.Sqrt, scale=1.0 / D, bias=eps_t[:, 0:1]),
          r=[('ssq', s), 'eps'], w=[('rstd', s)])
    P.add('vector', lambda e: e.reciprocal(out=rstd[:, 0:n], in_=rstd[:, 0:n]), r=[('rstd', s)], w=[('rstd', s)])
    for j, (xc, xo) in enumerate(zip(xt_chunks, xn_chunks)):
        np_ = xc.shape[0]
        P.add('gpsimd', lambda e, xc=xc, xo=xo, j=j, np_=np_: e.tensor_scalar(out=xo, in0=xc, scalar1=rstd[0:np_, j:j + 1], scalar2=None, op0=ALU.mult),
              r=[xkey, ('rstd', s)], w=[('xn', s)])


def emit_norm_T(P, xcs, xkey, ssq, rstd, xos, eps_t, ident, pT, A, B, hx, nk):
    n = len(xcs)
    for j in range(n):
        P.add('scalar', lambda e, j=j: e.activation(out=xos[j], in_=xcs[j], func=AF.Square, accum_out=ssq[:, j:j + 1]),
              r=[xkey], w=[nk + ('xn',), nk + ('ssq',)])
    P.add('scalar', lambda e: e.activation(out=rstd[:, 0:n], in_=ssq[:, 0:n], func=AF.Sqrt, scale=1.0 / D, bias=eps_t[:, 0:1]),
          r=[nk + ('ssq',), 'eps'], w=[nk + ('rstd',)])
    P.add('vector', lambda e: e.reciprocal(out=rstd[:, 0:n], in_=rstd[:, 0:n]), r=[nk + ('rstd',)], w=[nk + ('rstd',)])
    for j in range(n):
        P.add('scalar', lambda e, j=j: e.activation(out=xos[j], in_=xcs[j], func=AF.Identity, scale=rstd[:, j:j + 1]),
              r=[xkey, nk + ('rstd',)], w=[nk + ('xn',)])
    for pr in range(4):
        bk = pr % 2
        with atom_of(P):
            for fc in (2 * pr, 2 * pr + 1):
                q = fc % 4
                for j in range(n):
                    P.add('tensor', lambda e, q=q, j=j, fc=fc: e.transpose(out=pT[:, q, j * 128:(j + 1) * 128], in_=xos[j][:, fc * 128:(fc + 1) * 128],
                                                                           identity=ident[:]),
                          r=[nk + ('xn',), 'ident'], w=[('pT', bk)])
            for fc in (2 * pr, 2 * pr + 1):
                q = fc % 4
                P.add('scalar', lambda e, fc=fc, q=q: e.activation(out=hx[:, fc, 0:128 * n], in_=pT[:, q, 0:128 * n], func=AF.Identity,
                                                                    scale=A[:, fc:fc + 1], bias=B[:, fc:fc + 1]),
                      r=[('pT', bk), 'AB'], w=[nk + ('hx',)])

def stage1a(k, upto=99, ntiles=None, NSEG=1, W=int(os.environ.get('W1A', '4'))):
    nc = k.nc; dr = k.dr
    with ExitStack() as ctx:
        T = lambda n, sh, dt=F32: ctx.enter_context(nc.sbuf_tensor("a_" + n, sh, dt))
        P = Prog(nc)
        ident, eps_t = consts(k, ctx, P, "a_")
        AB = load_AB(k, ctx, P, 0, "a_")
        gp_bc = [T(f"gpbc{v}", [128, 1024]) for v in range(2)]; ps_bc = T("psbc", [128, 1024])
        P.dma('sync', ps_bc[:], dr['pool_scale'][0, :].partition_broadcast(128), w=['psbc'])
        for v in range(2):
            P.dma('sync', gp_bc[v][:], dr['modD'][0, v, 2048:3072].partition_broadcast(128), w=[('gpbc', v)])
            P.add('gpsimd', lambda e, v=v: e.tensor_tensor(out=gp_bc[v][:], in0=gp_bc[v][:], in1=ps_bc[:], op=ALU.mult),
                  r=['psbc'], w=[('gpbc', v)])
        inv = T("inv", [128, NSEG + 1, 2, 4, 2, 8]); msk = T("msk", [128, 2 * (NSEG + 1)])
        for sg in range(NSEG + 1):
            P.dma('sync', inv[:, sg].rearrange("p b c d e -> p (b c d e)"), dr['inv'][sg, :].partition_broadcast(128), w=['inv'])
        P.dma('sync', msk[:], dr['msk'][0, :].partition_broadcast(128), w=['msk'])
        wg = T("wg", [128, 4, 2, 256], BF16)
        P.dma('gpsimd', wg[:], dr['pool_w'].rearrange("g (c p) e -> p g c e", p=128), w=['wg'])
        xt = [T(f"xt{s}", [128, 2, 1024]) for s in range(W)]; xh = [T(f"xh{s}", [128, 1024]) for s in range(W)]
        ssq = [T(f"ssq{s}", [128, 3]) for s in range(W)]; rstd = [T(f"rstd{s}", [128, 3]) for s in range(W)]
        xn = [T(f"xn{s}", [128, 3, 1024], BF16) for s in range(W)]
        hxT = [T(f"hxT{s}", [128, 8, 272], BF16) for s in range(W)]
        SA = [T(f"SA{s}", [128, 2, 272]) for s in range(W)]; SB = [T(f"SB{s}", [128, 2, 272]) for s in range(W)]
        te = [T(f"te{s}", [128, 2, 8]) for s in range(W)]
        dT = [T(f"dT{s}", [128, 8, 256], BF16) for s in range(W)]
        tmp = [T(f"tmp{s}", [128, 1024]) for s in range(2)]
        ssq2 = [T(f"ssq2{s}", [128, 2]) for s in range(W)]; rstd2 = [T(f"rstd2{s}", [128, 2]) for s in range(W)]
        xn2 = [T(f"xn2{s}", [128, 2, 1024], BF16) for s in range(W)]; hx2 = [T(f"hx2{s}", [128, 8, 256], BF16) for s in range(W)]
        pT = ctx.enter_context(nc.psum_tensor("a_pT", [128, 4, 512], BF16))
        pD = [ctx.enter_context(nc.psum_tensor(f"a_pD{s}", [128, 1024], F32)) for s in range(2)]
        for s in range(W):
            P.add('gpsimd', lambda e, s=s: e.memset(ssq[s][:], 1.0), w=[('ssq', s)])
            P.add('gpsimd', lambda e, s=s: e.memset(xh[s][:], 0.0), w=[('xh', s)])
            P.add('gpsimd', lambda e, s=s: e.memset(xn[s][:, 2, :], 0.0), w=[('xn', s)])
        tiles = [(0, sg, t) for sg in range(NSEG) for t in range(NT)] + [(1, NSEG, 0)]
        if ntiles: tiles = tiles[:ntiles]
        recs = []
        for ti, (v, sg, t) in enumerate(tiles):
            s = ti % W
            R = Rec(); recs.append(R)
            src = dr['xin'][sg] if v == 0 else dr['ctxin']; dst = dr['xaD'][sg * 4096:(sg + 1) * 4096, :] if v == 0 else dr['caD']
            r0 = 256 * t
            R.dma('sync', xt[s][:], src[r0 + 8:r0 + 264, :].rearrange("(j p) d -> p j d", p=128), w=[('xt', s)])
            R.dma('sync', xh[s][0:8, :], src[r0:r0 + 8, :], w=[('xh', s)])
            R.dma('sync', xh[s][32:40, :], src[r0 + 264:r0 + 272, :], w=[('xh', s)])
            xcs = [xt[s][:, 0, :], xt[s][:, 1, :], xh[s][:, :]]; xos = [xn[s][:, 0, :], xn[s][:, 1, :], xn[s][:, 2, :]]
            for j in range(3):
                np_ = xcs[j].shape[0]
                R.add('scalar', lambda e, j=j, np_=np_, s=s, xcs=xcs, xos=xos: e.activation(out=xos[j], in_=xcs[j], func=AF.Square,
                                                                                      accum_out=ssq[s][0:np_, j:j + 1]),
                      r=[('xt', s), ('xh', s)], w=[('xn', s), ('ssq', s)])
            R.add('scalar', lambda e, s=s: e.activation(out=rstd[s][:], in_=ssq[s][:], func=AF.Sqrt, scale=1.0 / D, bias=eps_t[:, 0:1]),
                  r=[('ssq', s), 'eps'], w=[('rstd', s)])
            R.add('vector', lambda e, s=s: e.reciprocal(out=rstd[s][:], in_=rstd[s][:]), r=[('rstd', s)], w=[('rstd', s)])
            for j in range(3):
                np_ = xcs[j].shape[0]
                R.add('scalar', lambda e, j=j, np_=np_, s=s, xcs=xcs, xos=xos: e.activation(out=xos[j], in_=xcs[j], func=AF.Identity,
                                                                                      scale=rstd[s][0:np_, j:j + 1]),
                      r=[('xt', s), ('xh', s), ('rstd', s)], w=[('xn', s)])
            if upto < 1: continue
            for pr in range(4):
              bk = pr % 2
              with R.atom():
                for fc in (2 * pr, 2 * pr + 1):
                    q = fc % 4
                    for jj in range(3):
                        R.add('tensor', lambda e, s=s, q=q, jj=jj, fc=fc: e.transpose(out=pT[:, q, jj * 128:(jj + 1) * 128],
                                                                                 in_=xn[s][:, jj, fc * 128:(fc + 1) * 128], identity=ident[:]),
                              r=[('xn', s), 'ident'], w=[('pT', bk)])
                for fc in (2 * pr, 2 * pr + 1):
                    q = fc % 4
                    A_ = AB[:, v, 0, 0, fc:fc + 1]; B_ = AB[:, v, 0, 1, fc:fc + 1]
                    R.add('scalar', lambda e, s=s, fc=fc, q=q, A_=A_, B_=B_: e.activation(out=hxT[s][:, fc, 8:264], in_=pT[:, q, 0:256], func=AF.Identity,
                                                                                      scale=A_, bias=B_),
                          r=[('pT', bk), 'AB'], w=[('hxT', s)])
                    for (o0, i0) in ((0, 256), (264, 288)):
                        R.add('scalar', lambda e, s=s, fc=fc, q=q, A_=A_, B_=B_, o0=o0, i0=i0: e.activation(out=hxT[s][:, fc, o0:o0 + 8], in_=pT[:, q, i0:i0 + 8],
                                                                                                  func=AF.Identity, scale=A_, bias=B_),
                              r=[('pT', bk), 'AB'], w=[('hxT', s)])
            if upto < 2: continue
            first = (t == 0); last = (v == 1) or (t == NT - 1)
            if first:
                R.add('vector', lambda e, s=s, sg=sg: e.tensor_scalar(out=hxT[s][:, :, 0:8], in0=hxT[s][:, :, 0:8], scalar1=msk[:, 2 * sg:2 * sg + 1],
                                                                     scalar2=None, op0=ALU.mult), r=['msk'], w=[('hxT', s)])
            if last:
                R.add('vector', lambda e, s=s, sg=sg: e.tensor_scalar(out=hxT[s][:, :, 264:272], in0=hxT[s][:, :, 264:272], scalar1=msk[:, 2 * sg + 1:2 * sg + 2],
                                                                     scalar2=None, op0=ALU.mult), r=['msk'], w=[('hxT', s)])
            for g in range(4):
                h = hxT[s][:, 2 * g:2 * g + 2, :]
                bufs = [SA[s], SB[s]]
                R.add('vector', lambda e, s=s, h=h: e.tensor_tensor(out=SA[s][:, :, 1:272], in0=h[:, :, 0:271], in1=h[:, :, 1:272], op=ALU.add),
                      r=[('hxT', s)], w=[('S', s)])
                cur = 0
                steps = [(1, 2, 271), (2, 4, 269), (4, 8, 265)]
                for (sh_, lo, hi) in steps[:g]:
                    a = bufs[cur]; b = bufs[1 - cur]
                    R.add('vector', lambda e, a=a, b=b, sh_=sh_, lo=lo, hi=hi: e.tensor_tensor(out=b[:, :, lo:hi], in0=a[:, :, lo - sh_:hi - sh_],
                                                                                         in1=a[:, :, lo + sh_:hi + sh_], op=ALU.add),
                          r=[('S', s)], w=[('S', s)])
                    cur = 1 - cur
                Sf = bufs[cur]; wv = 1.0 / (2 << g)
                R.add('vector', lambda e, s=s, g=g, Sf=Sf, h=h, wv=wv: e.scalar_tensor_tensor(out=dT[s][:, 2 * g:2 * g + 2, :], in0=Sf[:, :, 8:264], scalar=wv,
                                                                                         in1=h[:, :, 8:264], op0=ALU.mult, op1=ALU.subtract),
                      r=[('S', s), ('hxT', s)], w=[('dT', s)])
                for (flag, eidx, c0) in ((first, 0, 0), (last, 1, 248)):
                    if not flag: continue
                    R.add('vector', lambda e, s=s, g=g, Sf=Sf, sg=sg, eidx=eidx, c0=c0: e.tensor_tensor(out=te[s][:], in0=Sf[:, :, 8 + c0:16 + c0],
                                                                                                in1=inv[:, sg, eidx, g, :, :], op=ALU.mult),
                          r=[('S', s), 'inv'], w=[('te', s)])
                    R.add('vector', lambda e, s=s, g=g, h=h, c0=c0: e.tensor_tensor(out=dT[s][:, 2 * g:2 * g + 2, c0:c0 + 8], in0=te[s][:],
                                                                                in1=h[:, :, 8 + c0:16 + c0], op=ALU.subtract),
                          r=[('te', s), ('hxT', s)], w=[('dT', s)])
            if upto < 3: continue
            for j in range(2):
              with R.atom():
                for g in range(4):
                    for c in range(2):
                        R.add('tensor', lambda e, s=s, j=j, g=g, c=c: e.matmul(pD[j][:, g * 256:(g + 1) * 256], lhsT=dT[s][:, 2 * g + c, j * 128:(j + 1) * 128],
                                                                              rhs=wg[:, g, c, :], start=(c == 0), stop=(c == 1)),
                              r=[('dT', s), 'wg'], w=[('pD', j)])
                R.add('vector', lambda e, s=s, j=j, v=v: e.tensor_tensor(out=tmp[j][:], in0=pD[j][:], in1=gp_bc[v][:], op=ALU.mult),
                      r=[('pD', j), ('gpbc', v)], w=[('tmp', j)])
                R.add('vector', lambda e, s=s, j=j: e.tensor_tensor(out=xt[s][:, j, :], in0=xt[s][:, j, :], in1=tmp[j][:], op=ALU.add),
                      r=[('tmp', j)], w=[('xt', s)])
            R.dma('sync', dst[r0:r0 + 256, :].rearrange("(j p) d -> p j d", p=128), xt[s][:], r=[('xt', s)], w=['xaD'])
            hdst = dr['hxD'][:, :, sg * 4096:(sg + 1) * 4096] if v == 0 else dr['hcD']
            emit_norm_T(R, [xt[s][:, 0, :], xt[s][:, 1, :]], ('xt', s), ssq2[s], rstd2[s], [xn2[s][:, 0, :], xn2[s][:, 1, :]], eps_t, ident, pT,
                        AB[:, v, 1, 0, :], AB[:, v, 1, 1, :], hx2[s], ('n2', s))
            R.dma('sync', hdst[:, :, r0:r0 + 256].rearrange("c p t -> p c t"), hx2[s][:], r=[('n2', s, 'hx')], w=['hxD'])
        interleave(P, recs, W)
        P.emit(ctx)


def ffn_pass(k, pref, units, tiles, g2_specs, gatesD=None):
    nc = k.nc; dr = k.dr; HF = 11; HW = 1408
    with ExitStack() as ctx:
        T = lambda n, sh, dt=F32: ctx.enter_context(nc.sbuf_tensor(pref + n, sh, dt))
        P = Prog(nc)
        w1b = [T(f"w1b{i}", [128, 8, HW], BF16) for i in range(2)]; w3b = [T(f"w3b{i}", [128, 8, HW], BF16) for i in range(2)]
        w2b = [T(f"w2b{i}", [128, HF, 1024], BF16) for i in range(2)]
        g2bc = {v: T(f"g2bc{v}", [128, 1024]) for v in g2_specs}
        for v, src in g2_specs.items():
            P.dma('sync', g2bc[v][:], src.partition_broadcast(128), w=[('g2bc', v)])
        gates = None
        if gatesD is not None:
            gates = T("gates", [128, 32, 8])
            P.dma('sync', gates[:], gatesD.rearrange("(c p) e -> p c e", p=128), w=['gates'])
        hx = [T(f"hx{i}", [128, 8, 256], BF16) for i in range(3)]
        gT = T("gT", [128, HF, 256], BF16); sil = [T(f"sil{i}", [128, 256], BF16) for i in range(2)]
        tmp = [T(f"tmp{i}", [128, 512]) for i in range(4)]
        ph = [ctx.enter_context(nc.psum_tensor(f"{pref}ph{i}", [128, 2, 256], F32)) for i in range(2)]
        pD = [ctx.enter_context(nc.psum_tensor(f"{pref}pD{i}", [128, 512], F32)) for i in range(2)]
        nh = 0; nd = 0
        def loadw(u):
            (w1, w3, w2, ex) = units[u]; b = u % 2
            for dc in range(8):
                P.dma('gpsimd', w1b[b][:, dc, :], w1[dc * 128:(dc + 1) * 128, :], w=[('w1', b, dc)])
                P.dma('gpsimd', w3b[b][:, dc, :], w3[dc * 128:(dc + 1) * 128, :], w=[('w3', b, dc)])
            for f in range(HF):
                P.dma('gpsimd', w2b[b][:, f, :], w2[f * 128:(f + 1) * 128, :], w=[('w2', b, f)])
        loadw(0)
        for u, (w1, w3, w2, ex) in enumerate(units):
            b = u % 2
            if u + 1 < len(units): loadw(u + 1)
            for (hsrc, odst, v, ch0) in tiles:
                hs = nh % 3; nh += 1
                P.dma('sync', hx[hs][:], hsrc.rearrange("c p t -> p c t"), w=[('hx', hs)])
                for f in range(HF):
                    pb = f % 2
                    for (wb, wk, col) in ((w1b, 'w1', 0), (w3b, 'w3', 1)):
                        for dc in range(8):
                            P.add('tensor', lambda e, wb=wb, b=b, dc=dc, f=f, pb=pb, col=col, hs=hs: e.matmul(
                                ph[pb][:, col, :], lhsT=wb[b][:, dc, f * 128:(f + 1) * 128], rhs=hx[hs][:, dc, :], start=(dc == 0), stop=(dc == 7)),
                                r=[(wk, b, dc), ('hx', hs)], w=[('ph', pb)])
                    P.add('scalar', lambda e, pb=pb: e.activation(out=sil[pb][:], in_=ph[pb][:, 0, :], func=AF.Silu), r=[('ph', pb)], w=[('sil', pb)])
                    P.add('vector', lambda e, pb=pb, f=f: e.tensor_tensor(out=gT[:, f, :], in0=sil[pb][:], in1=ph[pb][:, 1, :], op=ALU.mult),
                          r=[('sil', pb), ('ph', pb)], w=[('gT', f)])
                for j in range(2):
                    for half in range(2):
                        db = nd % 2; ts = nd % 4; nd += 1
                        for f in range(HF):
                            P.add('tensor', lambda e, db=db, f=f, j=j, half=half, b=b: e.matmul(
                                pD[db][:], lhsT=gT[:, f, j * 128:(j + 1) * 128], rhs=w2b[b][:, f, half * 512:(half + 1) * 512], start=(f == 0), stop=(f == HF - 1)),
                                r=[('gT', f), ('w2', b, f)], w=[('pD', db)])
                        if gates is None:
                            P.add('vector', lambda e, db=db, ts=ts, half=half, v=v: e.tensor_tensor(out=tmp[ts][:], in0=pD[db][:],
                                                                                           in1=g2bc[v][:, half * 512:(half + 1) * 512], op=ALU.mult),
                                  r=[('pD', db), ('g2bc', v)], w=[('tmp', ts)])
                        else:
                            P.add('vector', lambda e, db=db, ts=ts, half=half, v=v, c=ch0 + j, ex=ex: e.scalar_tensor_tensor(
                                out=tmp[ts][:], in0=pD[db][:], scalar=gates[:, c, ex:ex + 1], in1=g2bc[v][:, half * 512:(half + 1) * 512],
                                op0=ALU.mult, op1=ALU.mult), r=[('pD', db), ('g2bc', v), 'gates'], w=[('tmp', ts)])
                        P.dma('gpsimd', odst[j * 128:(j + 1) * 128, half * 512:(half + 1) * 512], tmp[ts][:], r=[('tmp', ts)],
                              w=[('out', odst.name, ch0 + j, half)], accum_op=ALU.add)
        P.emit(ctx)


def stage1b(k, NSEG=1):
    dr = k.dr
    units = [(dr['ffn_w1'][:, h * 1408:(h + 1) * 1408], dr['ffn_w3'][:, h * 1408:(h + 1) * 1408], dr['ffn_w2'][h * 1408:(h + 1) * 1408, :], 0)
             for h in range(2)]
    tiles = [(dr['hxD'][:, :, 256 * t:256 * (t + 1)], dr['xaD'][256 * t:256 * (t + 1), :], 0, 2 * t) for t in range(NT * NSEG)]
    tiles.append((dr['hcD'][:, :, :], dr['caD'][:, :], 1, 2 * NT * NSEG))
    ffn_pass(k, "b_", units, tiles, {0: dr['modD'][0, 0, 5120:6144], 1: dr['modD'][0, 1, 5120:6144]})


def stage3(k, NQB=8, NKC=130, A_LIST=(0, 1)):
    nc = k.nc; dr = k.dr; NK = NKC * 128
    with ExitStack() as ctx:
        T = lambda n, sh, dt=F32: ctx.enter_context(nc.sbuf_tensor("c_" + n, sh, dt))
        P = Prog(nc)
        KT = T("KT", [128, NK], BF16); V = T("V", [128, NKC, 2, 65], BF16)
        wo = T("wo", [64, 16, 1024], BF16); wof = T("wof", [64, 2, 1024]); g1bc = T("g1bc", [64, 1024])
        QT = [T(f"QT{i}", [128, 4, 512], BF16) for i in range(2)]
        PT = [T(f"PT{i}", [128, 1024], BF16) for i in range(3)]
        oS = T("oS", [65, 2, 512]); attnT = T("attnT", [64, 8, 512], BF16)
        ones = T("ones", [65, 64]); tmp = [T(f"tmp{i}", [128, 512]) for i in range(2)]
        ps = [ctx.enter_context(nc.psum_tensor(f"c_ps{i}", [128, 1024], F32)) for i in range(2)]
        po = [ctx.enter_context(nc.psum_tensor(f"c_po{i}", [128, 512], F32)) for i in range(2)]
        pbc = ctx.enter_context(nc.psum_tensor("c_pbc", [128, 512], F32))
        pw = ctx.enter_context(nc.psum_tensor("c_pw", [128, 512], F32))
        P.add('gpsimd', lambda e: e.memset(ones[:], 1.0), w=['ones'])
        P.dma('sync', g1bc[:], dr['modD'][1, 0, 2048:3072].partition_broadcast(64), w=['g1bc'])
        for h in range(16):
            sl = h % 2
            P.dma('sync', wof[:, sl, :], dr['w_o'][h * 64:(h + 1) * 64, :], w=[('wof', sl)])
            P.add('vector', lambda e, h=h, sl=sl: e.tensor_tensor(out=wo[:, h, :], in0=wof[:, sl, :], in1=g1bc[:], op=ALU.mult),
                  r=[('wof', sl), 'g1bc'], w=['wo'])
        nq = 0; nps = 0; npt = 0; ntmp = 0
        for a in A_LIST:
            P.dma('sync', KT[:, :], dr['KTD'][2 * a:2 * a + 2, :, :].rearrange("b d t -> (b d) t"), w=['KT'])
            for c0 in range(0, NKC, 26):
                c1 = min(NKC, c0 + 26)
                P.dma('sync', V[:, c0:c1, :, :], dr['VD'][c0 * 128:c1 * 128, 2 * a:2 * a + 2, :].rearrange("(c p) g e -> p c g e", p=128), w=['V'])
            for qb in range(NQB):
                qs = nq % 2; nq += 1
                P.dma('sync', QT[qs][:], dr['QTD'][a * 8:(a + 1) * 8, :, qb * 512:(qb + 1) * 512].rearrange("(i b) d t -> (b d) i t", b=2), w=[('QT', qs)])
                for i in range(4):
                    def qk(kc, s):
                        for b in range(2):
                            P.add('tensor', lambda e, b=b, kc=kc, s=s, qs=qs, i=i: e.matmul(
                                ps[s][:, b * 512:(b + 1) * 512], lhsT=KT[b * 64:(b + 1) * 64, kc * 128:(kc + 1) * 128],
                                rhs=QT[qs][b * 64:(b + 1) * 64, i, :], start=True, stop=True),
                                r=['KT', ('QT', qs)], w=[('ps', s)])
                    s0 = nps % 2; nps += 1
                    qk(0, s0); cur = s0
                    for kc in range(NKC):
                        t = npt % 3; npt += 1
                        if kc + 1 < NKC:
                            s1 = nps % 2; nps += 1
                            qk(kc + 1, s1)
                        P.add('scalar', lambda e, cur=cur, t=t: e.activation(out=PT[t][:], in_=ps[cur][:], func=AF.Exp, scale=0.125),
                              r=[('ps', cur)], w=[('PT', t)])
                        for b in range(2):
                            P.add('tensor', lambda e, b=b, kc=kc, t=t: e.matmul(
                                po[b][0:65, :], lhsT=V[:, kc, b, :], rhs=PT[t][:, b * 512:(b + 1) * 512], start=(kc == 0), stop=(kc == NKC - 1)),
                                r=['V', ('PT', t)], w=[('po', b)])
                        if kc + 1 < NKC: cur = s1
                    for b in range(2):
                        P.add('vector', lambda e, b=b: e.tensor_copy(out=oS[:, b, :], in_=po[b][0:65, :]), r=[('po', b)], w=[('oS', b)])
                        P.add('vector', lambda e, b=b: e.reciprocal(out=oS[64:65, b, :], in_=oS[64:65, b, :]), r=[('oS', b)], w=[('oS', b)])
                        P.add('tensor', lambda e, b=b: e.matmul(pbc[0:64, :], lhsT=ones[64:65, :], rhs=oS[64:65, b, :], start=True, stop=True),
                              r=[('oS', b), 'ones'], w=['pbc'])
                        P.add('vector', lambda e, b=b, i=i: e.tensor_tensor(out=attnT[:, i * 2 + b, :], in0=oS[0:64, b, :], in1=pbc[0:64, :], op=ALU.mult),
                              r=[('oS', b), 'pbc'], w=[('attnT', i * 2 + b)])
                for j in range(4):
                    for half in range(2):
                        for hh in range(8):
                            i_, b_ = hh // 2, hh % 2; h = a * 8 + b_ * 4 + i_
                            P.add('tensor', lambda e, hh=hh, h=h, j=j, half=half: e.matmul(
                                pw[:, :], lhsT=attnT[:, hh, j * 128:(j + 1) * 128], rhs=wo[:, h, half * 512:(half + 1) * 512], start=(hh == 0), stop=(hh == 7)),
                                r=[('attnT', hh), 'wo'], w=['pw'])
                        ts = ntmp % 2; ntmp += 1
                        P.add('vector', lambda e, ts=ts: e.tensor_copy(out=tmp[ts][:], in_=pw[:, :]), r=['pw'], w=[('tmp', ts)])
                        r0 = qb * 512 + j * 128
                        P.dma('gpsimd', dr['xaD'][r0:r0 + 128, half * 512:(half + 1) * 512], tmp[ts][:], r=[('tmp', ts)],
                              w=[('xo', qb, j, half)], accum_op=ALU.add)
        P.emit(ctx)


def vw(t, off, dims):
    b = t[:]
    return bass.AP(tensor=b.tensor, offset=b.offset + off, ap=[list(b.ap[0])] + [list(d) for d in dims])


def stage2(k, ntiles=None, NSEG=1, W=2):
    nc = k.nc; dr = k.dr
    with ExitStack() as ctx:
        T = lambda n, sh, dt=F32: ctx.enter_context(nc.sbuf_tensor("q_" + n, sh, dt))
        P = Prog(nc)
        ident, eps_t = consts(k, ctx, P, "q_")
        AB = load_AB(k, ctx, P, 1, "q_")
        wq = T("wq", [128, 8, 1536], BF16)
        for dc in range(8):
            for a in range(2):
                for b in range(2):
                    P.dma('gpsimd', vw(wq, dc * 1536 + a * 512 + b * 64, [[128, 4], [1, 64]]),
                          dr['w_qkv'][dc * 128:(dc + 1) * 128, a * 512 + b * 256:a * 512 + (b + 1) * 256].rearrange("p (i d) -> p i d", d=64), w=[('wq', dc)])
            P.dma('gpsimd', wq[:, dc, 1024:1536], dr['w_qkv'][dc * 128:(dc + 1) * 128, 1024:1536], w=[('wq', dc)])
        gbc = T("gbc", [128, 1280])
        P.dma('sync', gbc[:], dr['qkg'][0, :].partition_broadcast(128), w=['gbc'])
        xt = [T(f"xt{s}", [128, 2, 1024]) for s in range(W)]
        ssq = [T(f"ssq{s}", [128, 2]) for s in range(W)]; rstd = [T(f"rstd{s}", [128, 2]) for s in range(W)]
        xn = [T(f"xn{s}", [128, 2, 1024], BF16) for s in range(W)]; hx = [T(f"hx{s}", [128, 8, 256], BF16) for s in range(W)]
        cs = [T(f"cs{s}", [128, 2, 2, 64]) for s in range(W)]
        qk_ = [[T(f"qk{s}{j}", [128, 1536]) for j in range(2)] for s in range(W)]; sq_ = [[T(f"sq{s}{j}", [128, 1280]) for j in range(2)] for s in range(W)]
        rh_ = [[T(f"rh{s}{j}", [128, 20]) for j in range(2)] for s in range(W)]; qn_ = [[T(f"qn{s}{j}", [128, 1280]) for j in range(2)] for s in range(W)]
        t2_ = [[T(f"t2{s}{j}", [128, 1280]) for j in range(2)] for s in range(W)]; qr_ = [[T(f"qr{s}{j}", [128, 1280], BF16) for j in range(2)] for s in range(W)]
        vs = [T(f"vs{s}", [128, 2, 4, 65], BF16) for s in range(W)]
        qT = [T(f"qT{s}", [128, 10, 2, 128], BF16) for s in range(W)]
        pT = ctx.enter_context(nc.psum_tensor("q_pT", [128, 4, 512], BF16))
        pq = [ctx.enter_context(nc.psum_tensor(f"q_pq{i}", [128, 512], F32)) for i in range(3)]
        for s in range(W):
            P.add('gpsimd', lambda e, s=s: e.memset(vs[s][:], 1.0), w=[('vs', s)])
        tiles = [(0, t) for t in range(NT * NSEG)] + [(1, 0)]
        if ntiles: tiles = tiles[:ntiles - 1] + [(1, 0)]
        def tile_body(P, ti, v, t):
            s = ti % W
            qk, sq, rh, qn, t2, qr = qk_[s], sq_[s], rh_[s], qn_[s], t2_[s], qr_[s]
            src = dr['xaD'] if v == 0 else dr['caD']
            r0 = 256 * t; k0 = r0 if v == 0 else 4096 * NSEG
            P.dma('sync', xt[s][:], src[r0:r0 + 256, :].rearrange("(j p) d -> p j d", p=128), w=[('xt', s)])
            P.dma('sync', cs[s][:, :, 0, :], dr['ropeC'][k0:k0 + 256, :].rearrange("(j p) e -> p j e", p=128), w=[('cs', s)])
            P.dma('sync', cs[s][:, :, 1, :], dr['ropeS'][k0:k0 + 256, :].rearrange("(j p) e -> p j e", p=128), w=[('cs', s)])
            emit_norm_T(P, [xt[s][:, 0, :], xt[s][:, 1, :]], ('xt', s), ssq[s], rstd[s], [xn[s][:, 0, :], xn[s][:, 1, :]], eps_t, ident, pT,
                        AB[:, v, 0, 0, :], AB[:, v, 0, 1, :], hx[s], ('n', s))
            full = (v == 0 and t < NT)
            c0 = 0 if full else 1024; h0 = c0 // 64; nh = 20 - h0; W_ = 1280 - c0
            for j in range(2):
                for cb in (range(3) if full else (2,)):
                  with P.atom():
                    for dc in range(8):
                        P.add('tensor', lambda e, s=s, j=j, cb=cb, dc=dc: e.matmul(pq[cb][:], lhsT=hx[s][:, dc, j * 128:(j + 1) * 128],
                                                                                 rhs=wq[:, dc, cb * 512:(cb + 1) * 512], start=(dc == 0), stop=(dc == 7)),
                              r=[('n', s, 'hx'), ('wq', dc)], w=[('pq', cb)])
                    P.add('scalar', lambda e, j=j, cb=cb: e.activation(out=qk[j][:, cb * 512:(cb + 1) * 512], in_=pq[cb][:], func=AF.Identity),
                          r=[('pq', cb)], w=[('qk', s, j)])
                Q = ('qk', s, j)
                hd = lambda ap: ap.rearrange("p (h d) -> p h d", d=64)
                P.add('scalar', lambda e, j=j, c0=c0: e.activation(out=sq[j][:, c0:1280], in_=qk[j][:, c0:1280], func=AF.Square), r=[Q], w=[('sq', s, j)])
                P.add('vector', lambda e, j=j, c0=c0, h0=h0: e.tensor_reduce(out=rh[j][:, h0:20], in_=hd(sq[j][:, c0:1280]), axis=AX.X, op=ALU.add),
                      r=[('sq', s, j)], w=[('rh', s, j)])
                P.add('scalar', lambda e, j=j, h0=h0: e.activation(out=rh[j][:, h0:20], in_=rh[j][:, h0:20], func=AF.Sqrt, scale=1.0 / 64, bias=eps_t[:, 0:1]),
                      r=[('rh', s, j), 'eps'], w=[('rh', s, j)])
                P.add('vector', lambda e, j=j, h0=h0: e.reciprocal(out=rh[j][:, h0:20], in_=rh[j][:, h0:20]), r=[('rh', s, j)], w=[('rh', s, j)])
                P.add('vector', lambda e, j=j, c0=c0, h0=h0, nh=nh: e.tensor_tensor(out=hd(qn[j][:, c0:1280]), in0=hd(qk[j][:, c0:1280]),
                                                                               in1=vw(rh[j], h0, [[1, nh], [0, 64]]), op=ALU.mult), r=[Q, ('rh', s, j)], w=[('qn', s, j)])
                P.add('vector', lambda e, j=j, c0=c0: e.tensor_tensor(out=qn[j][:, c0:1280], in0=qn[j][:, c0:1280], in1=gbc[:, c0:1280], op=ALU.mult),
                      r=['gbc'], w=[('qn', s, j)])
                cofs = j * 128; sofs = j * 128 + 64
                P.add('vector', lambda e, j=j, s=s, sofs=sofs, c0=c0, nh=nh: e.tensor_tensor(out=vw(t2[j], c0, [[64, nh], [32, 2], [1, 16]]),
                                                                                       in0=vw(qn[j], c0 + 16, [[64, nh], [32, 2], [1, 16]]),
                                                                                       in1=vw(cs[s], sofs, [[0, nh], [32, 2], [1, 16]]), op=ALU.mult),
                      r=[('qn', s, j), ('cs', s)], w=[('t2', s, j)])
                P.add('vector', lambda e, j=j, s=s, sofs=sofs, c0=c0, nh=nh: e.tensor_tensor(out=vw(t2[j], c0 + 16, [[64, nh], [32, 2], [1, 16]]),
                                                                                       in0=vw(qn[j], c0, [[64, nh], [32, 2], [1, 16]]),
                                                                                       in1=vw(cs[s], sofs + 16, [[0, nh], [32, 2], [1, 16]]), op=ALU.mult),
                      r=[('qn', s, j), ('cs', s)], w=[('t2', s, j)])
                P.add('vector', lambda e, j=j, s=s, cofs=cofs, c0=c0, nh=nh: e.tensor_tensor(out=hd(qn[j][:, c0:1280]), in0=hd(qn[j][:, c0:1280]),
                                                                                       in1=vw(cs[s], cofs, [[0, nh], [1, 64]]), op=ALU.mult),
                      r=[('cs', s), ('t2', s, j)], w=[('qn', s, j)])
                P.add('vector', lambda e, j=j, c0=c0: e.tensor_tensor(out=qr[j][:, c0:1280], in0=qn[j][:, c0:1280], in1=t2[j][:, c0:1280], op=ALU.add),
                      r=[('t2', s, j)], w=[('qn', s, j), ('qr', s, j)])
                P.add('scalar', lambda e, j=j, s=s: e.activation(out=vs[s][:, j, :, 0:64], in_=qk[j][:, 1280:1536].rearrange("p (g d) -> p g d", d=64), func=AF.Identity),
                      r=[Q], w=[('vs', s)])
                for grp in (range(3) if full else (2,)):
                  with P.atom():
                    rg = (0, 2, 1)[grp]; bk = rg // 2; n = 4 if grp < 2 else 2
                    for u in range(n):
                        sl = grp * 4 + u
                        P.add('tensor', lambda e, j=j, sl=sl, rg=rg, u=u: e.transpose(out=pT[:, rg, u * 128:(u + 1) * 128], in_=qr[j][:, sl * 128:(sl + 1) * 128],
                                                                                identity=ident[:]), r=[('qr', s, j), 'ident'], w=[('pT', bk)])
                    P.add('vector', lambda e, j=j, s=s, grp=grp, rg=rg, n=n: e.tensor_copy(out=qT[s][:, grp * 4:grp * 4 + n, j, :],
                                                                               in_=pT[:, rg, 0:n * 128].rearrange("p (u t) -> p u t", t=128)),
                          r=[('pT', bk)], w=[('qT', s)])
            for j in range(2):
                if v == 0 and t < NT:
                    P.dma('sync', dr['QTD'][:, :, r0 + j * 128:r0 + (j + 1) * 128].rearrange("(hp two) d t -> (two d) hp t", two=2), qT[s][:, 0:8, j, :],
                          r=[('qT', s)], w=['QTD'])
                P.dma('sync', dr['KTD'][:, :, k0 + j * 128:k0 + (j + 1) * 128].rearrange("(a b) d t -> (b d) a t", b=2), qT[s][:, 8:10, j, :],
                      r=[('qT', s)], w=['KTD'])
            P.dma('sync', dr['VD'][k0:k0 + 256, :, :].rearrange("(j p) g e -> p j g e", p=128), vs[s][:], r=[('vs', s)], w=['VD'])
        recs = []
        for ti, (v, t) in enumerate(tiles):
            R = Rec(); recs.append(R); tile_body(R, ti, v, t)
        interleave(P, recs, W)
        P.emit(ctx)


def stage3b(k, ntiles=NT, W=2):
    nc = k.nc; dr = k.dr
    with ExitStack() as ctx:
        T = lambda n, sh, dt=F32: ctx.enter_context(nc.sbuf_tensor("r_" + n, sh, dt))
        P = Prog(nc)
        ident, eps_t = consts(k, ctx, P, "r_")
        AB = load_AB(k, ctx, P, 1, "r_")
        RW = T("RW", [128, 8, 1024]); Abc = T("Abc", [128, 1024]); Bbc = T("Bbc", [128, 1024]); gbc = T("gbc", [128, 1024])
        cbc = T("cbc", [128, 8]); junk = T("junk", [128, 1024])
        P.dma('sync', Abc[:], dr['modD'][1, 0, 4096:5120].partition_broadcast(128), w=['Abc'])
        P.dma('sync', Bbc[:], dr['modD'][1, 0, 3072:4096].partition_broadcast(128), w=['Bbc'])
        P.dma('sync', gbc[:], dr['norm_g'][1, 1, :].partition_broadcast(128), w=['gbc'])
        P.add('vector', lambda e: e.scalar_tensor_tensor(out=Abc[:], in0=Abc[:], scalar=1.0, in1=gbc[:], op0=ALU.add, op1=ALU.mult), r=['gbc'], w=['Abc'])
        P.add('gpsimd', lambda e: e.memset(cbc[:], 0.0), w=['cbc'])
        for ex in range(8):
            P.dma('sync', RW[:, ex, :], dr['rwT'][ex, :].partition_broadcast(128), w=[('RW', ex)])
            P.add('vector', lambda e, ex=ex: e.scalar_tensor_tensor(out=junk[:], in0=Bbc[:], scalar=1.0, in1=RW[:, ex, :], op0=ALU.mult, op1=ALU.mult,
                                                                    accum_out=cbc[:, ex:ex + 1]), r=['Bbc', ('RW', ex), 'cbc'], w=['junk', 'cbc'])
            P.add('vector', lambda e, ex=ex: e.tensor_tensor(out=RW[:, ex, :], in0=RW[:, ex, :], in1=Abc[:], op=ALU.mult), r=['Abc'], w=[('RW', ex)])
        xt = [T(f"xt{s}", [128, 2, 1024]) for s in range(W)]
        ssq = [T(f"ssq{s}", [128, 2]) for s in range(W)]; rstd = [T(f"rstd{s}", [128, 2]) for s in range(W)]
        xn = [T(f"xn{s}", [128, 2, 1024], BF16) for s in range(W)]; hx = [T(f"hx{s}", [128, 8, 256], BF16) for s in range(W)]
        raw = [T(f"raw{s}", [128, 2, 8]) for s in range(W)]
        sm = [{n: T(f"{n}{s}", [128, 2, 8]) for n in ("lg", "lg2", "mk1", "mk2")} for s in range(W)]
        for s in range(W):
            sm[s].update({n: T(f"{n}{s}", [128, 2]) for n in ("m1", "m2", "e2", "g1v", "g2v")}); sm[s]["junk"] = T(f"junkt{s}", [128, 1024])
        gates = T("gates", [128, 32, 8])
        pT = ctx.enter_context(nc.psum_tensor("r_pT", [128, 4, 512], BF16))
        for s in range(W):
            P.add('gpsimd', lambda e, s=s: e.memset(raw[s][:], 0.0), w=[('raw', s)])
        P0 = P

        def tile_body(P, t):
            s = t % W; r0 = 256 * t
            lg, lg2, mk1, mk2, m1, m2, e2, g1v, g2v, junk = [sm[s][n] for n in ('lg', 'lg2', 'mk1', 'mk2', 'm1', 'm2', 'e2', 'g1v', 'g2v', 'junk')]
            K_ = lambda n: (n, s)
            P.dma('sync', xt[s][:], dr['xaD'][r0:r0 + 256, :].rearrange("(j p) d -> p j d", p=128), w=[('xt', s)])
            emit_norm_T(P, [xt[s][:, 0, :], xt[s][:, 1, :]], ('xt', s), ssq[s], rstd[s], [xn[s][:, 0, :], xn[s][:, 1, :]], eps_t, ident, pT,
                        AB[:, 0, 1, 0, :], AB[:, 0, 1, 1, :], hx[s], ('n', s))
            P.dma('sync', dr['hxD'][:, :, r0:r0 + 256].rearrange("c p t -> p c t"), hx[s][:], r=[('n', s, 'hx')], w=['hxD'])
            for j in range(2):
                for ex in range(8):
                    P.add('vector', lambda e, s=s, j=j, ex=ex: e.scalar_tensor_tensor(out=junk[:], in0=xt[s][:, j, :], scalar=1.0, in1=RW[:, ex, :],
                                                                                    op0=ALU.mult, op1=ALU.mult, accum_out=raw[s][:, j, ex:ex + 1]),
                          r=[('xt', s), ('RW', ex), ('raw', s)], w=[K_('junk'), ('raw', s)])
            V = 'vector'
            P.add(V, lambda e, s=s: e.tensor_tensor(out=lg[:], in0=raw[s][:], in1=vw(rstd[s], 0, [[1, 2], [0, 8]]), op=ALU.mult), r=[('raw', s), ('n', s, 'rstd')], w=[K_('lg')])
            P.add('gpsimd', lambda e, s=s: e.memset(raw[s][:], 0.0), r=[K_('lg')], w=[('raw', s)])
            P.add(V, lambda e: e.tensor_tensor(out=lg[:], in0=lg[:], in1=vw(cbc, 0, [[0, 2], [1, 8]]), op=ALU.add), r=['cbc'], w=[K_('lg')])
            P.add(V, lambda e: e.tensor_reduce(out=m1[:], in_=lg[:], axis=AX.X, op=ALU.max), r=[K_('lg')], w=[K_('m1')])
            P.add(V, lambda e: e.tensor_tensor(out=mk1[:], in0=lg[:], in1=vw(m1, 0, [[1, 2], [0, 8]]), op=ALU.is_equal), r=[K_('lg'), K_('m1')], w=[K_('mk1')])
            P.add(V, lambda e: e.scalar_tensor_tensor(out=lg2[:], in0=mk1[:], scalar=-1e30, in1=lg[:], op0=ALU.mult, op1=ALU.add), r=[K_('mk1'), K_('lg')], w=[K_('lg2')])
            P.add(V, lambda e: e.tensor_reduce(out=m2[:], in_=lg2[:], axis=AX.X, op=ALU.max), r=[K_('lg2')], w=[K_('m2')])
            P.add(V, lambda e: e.tensor_tensor(out=mk2[:], in0=lg2[:], in1=vw(m2, 0, [[1, 2], [0, 8]]), op=ALU.is_equal), r=[K_('lg2'), K_('m2')], w=[K_('mk2')])
            P.add(V, lambda e: e.tensor_tensor(out=e2[:], in0=m2[:], in1=m1[:], op=ALU.subtract), r=[K_('m1'), K_('m2')], w=[K_('e2')])
            P.add('scalar', lambda e: e.activation(out=e2[:], in_=e2[:], func=AF.Exp), r=[K_('e2')], w=[K_('e2')])
            P.add(V, lambda e: e.tensor_scalar(out=g1v[:], in0=e2[:], scalar1=1.0, scalar2=None, op0=ALU.add), r=[K_('e2')], w=[K_('g1v')])
            P.add(V, lambda e: e.reciprocal(out=g1v[:], in_=g1v[:]), r=[K_('g1v')], w=[K_('g1v')])
            P.add(V, lambda e: e.tensor_tensor(out=g2v[:], in0=e2[:], in1=g1v[:], op=ALU.mult), r=[K_('e2'), K_('g1v')], w=[K_('g2v')])
            P.add(V, lambda e: e.tensor_tensor(out=mk1[:], in0=mk1[:], in1=vw(g1v, 0, [[1, 2], [0, 8]]), op=ALU.mult), r=[K_('g1v')], w=[K_('mk1')])
            P.add(V, lambda e: e.tensor_tensor(out=mk2[:], in0=mk2[:], in1=vw(g2v, 0, [[1, 2], [0, 8]]), op=ALU.mult), r=[K_('g2v')], w=[K_('mk2')])
            P.add(V, lambda e, t=t: e.tensor_tensor(out=gates[:, 2 * t:2 * t + 2, :], in0=mk1[:], in1=mk2[:], op=ALU.add), r=[K_('mk1'), K_('mk2')], w=['gates'])
        recs = []
        for t in range(ntiles):
            R = Rec(); recs.append(R); tile_body(R, t)
        interleave(P0, recs, W)
        P = P0
        P.dma('sync', dr['gatesD'][0:ntiles * 256, :].rearrange("(c p) e -> p c e", p=128), gates[:, 0:2 * ntiles, :], r=['gates'], w=['gatesD'])
        P.emit(ctx)


def stage4(k, nexp=8):
    dr = k.dr
    units = [(dr['moe_w1'][ex][:, h * 1408:(h + 1) * 1408], dr['moe_w3'][ex][:, h * 1408:(h + 1) * 1408], dr['moe_w2'][ex][h * 1408:(h + 1) * 1408, :], ex)
             for ex in range(nexp) for h in range(2)]
    tiles = [(dr['hxD'][:, :, 256 * t:256 * (t + 1)], dr['xaD'][256 * t:256 * (t + 1), :], 0, 2 * t) for t in range(NT)]
    ffn_pass(k, "m_", units, tiles, {0: dr['modD'][1, 0, 5120:6144]}, gatesD=dr['gatesD'])


def stage5(k, ntiles=NT):
    nc = k.nc; dr = k.dr
    with ExitStack() as ctx:
        T = lambda n, sh, dt=F32: ctx.enter_context(nc.sbuf_tensor("f_" + n, sh, dt))
        P = Prog(nc)
        eps_t = T("eps", [128, 1]); fg = T("fg", [128, 1024])
        P.add('gpsimd', lambda e: e.memset(eps_t[:], 1e-6), w=['eps'])
        P.dma('sync', fg[:], dr['final_g'][0, :].partition_broadcast(128), w=['fg'])
        xt = [T(f"xt{s}", [128, 2, 1024]) for s in range(2)]; ot = [T(f"ot{s}", [128, 2, 1024]) for s in range(2)]
        ssq = [T(f"ssq{s}", [128, 2]) for s in range(2)]; rstd = [T(f"rstd{s}", [128, 2]) for s in range(2)]
        for t in range(ntiles):
            s = t % 2; r0 = 256 * t
            P.dma('sync', xt[s][:], dr['xaD'][r0:r0 + 256, :].rearrange("(j p) d -> p j d", p=128), w=[('xt', s)])
            for j in range(2):
                P.add('scalar', lambda e, s=s, j=j: e.activation(out=ot[s][:, j, :], in_=xt[s][:, j, :], func=AF.Square, accum_out=ssq[s][:, j:j + 1]),
                      r=[('xt', s)], w=[('ot', s), ('ssq', s)])
            P.add('scalar', lambda e, s=s: e.activation(out=rstd[s][:], in_=ssq[s][:], func=AF.Sqrt, scale=1.0 / D, bias=eps_t[:, 0:1]),
                  r=[('ssq', s), 'eps'], w=[('rstd', s)])
            P.add('vector', lambda e, s=s: e.reciprocal(out=rstd[s][:], in_=rstd[s][:]), r=[('rstd', s)], w=[('rstd', s)])
            for j in range(2):
                P.add('vector', lambda e, s=s, j=j: e.scalar_tensor_tensor(out=ot[s][:, j, :], in0=xt[s][:, j, :], scalar=rstd[s][:, j:j + 1], in1=fg[:],
                                                                                              op0=ALU.mult, op1=ALU.mult),
                      r=[('xt', s), ('rstd', s), 'fg'], w=[('ot', s)])
            P.dma('sync', dr['out'][r0:r0 + 256, :].rearrange("(j p) d -> p j d", p=128), ot[s][:], r=[('ot', s)], w=['out'])
        P.emit(ctx)


POOLW=(2,4,8,16)
def edge_row(L, realfirst, reallast):
    inv=np.zeros((2,4,2,8),np.float32)
    for g,w in enumerate(POOLW):
        half=w//2
        for i in range(8):
            t=i; cnt=(min(t+half,L)-max(t-half,0)) if realfirst else w
            inv[0,g,:,i]=1.0/cnt
            t=L-8+i; cnt=(min(t+half,L)-max(t-half,0)) if reallast else w
            inv[1,g,:,i]=1.0/cnt
    return inv.reshape(128), [0.0 if realfirst else 1.0, 0.0 if reallast else 1.0]
def seg_quarters(r, NSEG):
    q=r%4
    if NSEG==1: return [q]
    if NSEG==4: return [q]+[o for o in range(4) if o!=q]
    m=r%2; p=(r%4)//2
    return [2*m+p, 2*m+1-p]
def core_inputs(r, inp, NSEG=1):
    b=r//4; qs=seg_quarters(r,NSEG); x=inp['x'][b]
    xin=np.zeros((NSEG,4112,1024),np.float32); inv=np.zeros((NSEG+1,128),np.float32); msk=np.zeros((1,2*(NSEG+1)),np.float32)
    for sg,q in enumerate(qs):
        lo=q*4096-8; hi=q*4096+4096+8; a=max(lo,0); z=min(hi,16384)
        xin[sg,a-lo:a-lo+(z-a)]=x[a:z]
        inv[sg],msk[0,2*sg:2*sg+2]=edge_row(16384,q==0,q==3)
    inv[NSEG],msk[0,2*NSEG:]=edge_row(256,True,True)
    ctxin=np.zeros((272,1024),np.float32); ctxin[8:264]=inp['ctx'][b]
    cvec=np.stack([inp['c'][b], inp['c_ctx']]).astype(np.float32)
    return dict(xin=xin, ctxin=ctxin, cvec=cvec, inv=inv, msk=msk)
def rope_gain(inp):
    return np.concatenate([np.tile(inp['q_norm_g'][0],16), np.tile(inp['k_norm_g'][0],4)]).astype(np.float32)[None,:]
def rope_tables(r, NSEG=1):
    n=NSEG*4096
    C=np.ones((n+256,64),np.float32); S=np.zeros((n+256,64),np.float32)
    fr=(10000.0**(-np.arange(16,dtype=np.float32)/16)).astype(np.float32)
    for sg,q in enumerate(seg_quarters(r,NSEG)):
        t=np.arange(q*4096,(q+1)*4096); row=(t//64).astype(np.float32); col=(t%64).astype(np.float32)
        ar=row[:,None]*fr; ac=col[:,None]*fr
        C[sg*4096:(sg+1)*4096]=np.concatenate([np.cos(ar),np.cos(ar),np.cos(ac),np.cos(ac)],1)
        S[sg*4096:(sg+1)*4096]=np.concatenate([-np.sin(ar),np.sin(ar),-np.sin(ac),np.sin(ac)],1)
    return {'ropeC':C,'ropeS':S}


NSEG = 4


def _build():
    k = K()
    k.din('xin', [NSEG, 4112, 1024]); k.din('ctxin', [272, 1024]); k.din('cvec', [2, 1024]); k.din('inv', [NSEG + 1, 128]); k.din('msk', [1, 2 * (NSEG + 1)])
    k.din('ada_w', [2, 1024, 6144]); k.din('ada_b', [2, 6144]); k.din('norm_g', [2, 2, 1024]); k.din('pool_w', [4, 256, 256]); k.din('pool_scale', [1, 1024])
    k.din('ffn_w1', [1024, 2816]); k.din('ffn_w3', [1024, 2816]); k.din('ffn_w2', [2816, 1024])
    k.din('w_qkv', [1024, 1536]); k.din('qkg', [1, 1280]); k.din('ropeC', [NSEG * 4096 + 256, 64]); k.din('ropeS', [NSEG * 4096 + 256, 64])
    k.din('w_o', [1024, 1024]); k.din('rwT', [8, 1024]); k.din('final_g', [1, 1024])
    k.din('moe_w1', [8, 1024, 2816]); k.din('moe_w3', [8, 1024, 2816]); k.din('moe_w2', [8, 2816, 1024])
    k.dint('modD', [2, 2, 6144]); k.dint('xaD', [NSEG * 4096, 1024]); k.dint('caD', [256, 1024])
    k.dint('hxD', [8, 128, NSEG * 4096], BF16); k.dint('hcD', [8, 128, 256], BF16)
    k.dint('QTD', [16, 64, 4096], BF16); k.dint('KTD', [4, 64, NSEG * 4096 + 256], BF16); k.dint('VD', [NSEG * 4096 + 256, 4, 65], BF16)
    k.dint('gatesD', [4096, 8]); k.dout('out', [4096, 1024])
    stage0(k); stage1a(k, NSEG=NSEG); stage1b(k, NSEG=NSEG); stage2(k, NSEG=NSEG)
    stage3(k, NKC=(NSEG * 4096 + 256) // 128); stage3b(k); stage4(k); stage5(k)
    return k.nc


def kernel(x, c, ctx, c_ctx, ada_w, ada_b, norm_g, pool_w, pool_scale, ffn_w1, ffn_w3, ffn_w2,
           w_qkv, w_o, q_norm_g, k_norm_g, router_w, moe_w1, moe_w3, moe_w2, final_g):
    f = lambda a: np.ascontiguousarray(np.asarray(a, dtype=np.float32))
    inp = dict(x=f(x), c=f(c), ctx=f(ctx), c_ctx=f(c_ctx), q_norm_g=f(q_norm_g), k_norm_g=f(k_norm_g))
    shared = dict(ada_w=f(ada_w), ada_b=f(ada_b), norm_g=f(norm_g), pool_w=f(pool_w)[0], pool_scale=f(pool_scale),
                  ffn_w1=f(ffn_w1)[0], ffn_w3=f(ffn_w3)[0], ffn_w2=f(ffn_w2)[0], w_qkv=f(w_qkv)[0], qkg=rope_gain(inp),
                  w_o=f(w_o)[0], rwT=np.ascontiguousarray(f(router_w)[0].T), final_g=f(final_g)[None, :],
                  moe_w1=f(moe_w1)[0], moe_w3=f(moe_w3)[0], moe_w2=f(moe_w2)[0])
    maps = [{**core_inputs(r, inp, NSEG), **rope_tables(r, NSEG), **shared} for r in range(8)]
    res = run_bass_kernel_spmd(_build(), maps, core_ids=list(range(8))).results
    out = np.stack([np.concatenate([np.asarray(res[4 * b + q]['out']) for q in range(4)], axis=0) for b in range(2)])
    return out.astype(np.float32)
```

```python
import numpy as np, os
DBG=os.environ.get('DBG','')
from contextlib import ExitStack
import concourse.bass as bass
import concourse.mybir as mybir
from concourse.bass_utils import run_bass_kernel_spmd

F32 = mybir.dt.float32; BF16 = mybir.dt.bfloat16
AF = mybir.ActivationFunctionType; ALU = mybir.AluOpType; AX = mybir.AxisListType

D = 1024; DFF = 2816; NF = 22; SEQ = 16384; CTX = 256; TPC = 4096; TT = 256; NT = TPC // TT
ENGS = ['sync', 'scalar', 'vector', 'gpsimd', 'tensor']
NDSEM = 16
SAME_SYNC = {'gpsimd', 'scalar', 'vector'}
_uid = [0]


class _Op:
    __slots__ = ('eng', 'fn', 'deps', 'dma', 'signal', 'sigval', 'dsem', 'dval', 'prev_dma')

    def __init__(s, eng, fn, dma):
        s.eng = eng; s.fn = fn; s.dma = dma; s.deps = []; s.signal = False; s.sigval = 0
        s.dsem = None; s.dval = 0; s.prev_dma = None


class Prog:
    def __init__(s, nc):
        s.nc = nc; s.ops = {e: [] for e in ENGS}; s.lastw = {}; s.readers = {}
        s.dma_count = {e: 0 for e in ENGS}; s.dma_hist = {e: [] for e in ENGS}

    def add(s, eng, fn, r=(), w=(), dma=False):
        op = _Op(eng, fn, dma); deps = []
        for k in r:
            d = s.lastw.get(k)
            if d is not None: deps.append(d)
        for k in w:
            d = s.lastw.get(k)
            if d is not None: deps.append(d)
            deps.extend(s.readers.get(k, ()))
        seen = set()
        for d in deps:
            if id(d) in seen: continue
            seen.add(id(d))
            if d.dma or d.eng != eng or eng in SAME_SYNC:
                op.deps.append(d)
                if not d.dma: d.signal = True
        for k in w:
            s.lastw[k] = op; s.readers[k] = []
        for k in r:
            s.readers.setdefault(k, []).append(op)
        if dma:
            n = s.dma_count[eng]; s.dma_count[eng] = n + 1
            op.dsem = n % NDSEM; op.dval = 16 * (n // NDSEM + 1)
            h = s.dma_hist[eng]
            if n >= NDSEM: op.prev_dma = h[n - NDSEM]
            h.append(op)
        s.ops[eng].append(op)
        return op

    def dma(s, eng, out, in_, r=(), w=(), **kw):
        return s.add(eng, lambda e: e.dma_start(out=out, in_=in_, **kw), r=r, w=w, dma=True)

    def emit(s, ctx):
        nc = s.nc; _uid[0] += 1; u = _uid[0]
        esem = {e: nc.alloc_semaphore(name=f"es{u}_{e}") for e in ENGS}
        dsem = {e: [nc.alloc_semaphore(name=f"ds{u}_{e}{i}") for i in range(NDSEM)]
                for e in ENGS if s.dma_count[e] > 0}
        allsems = list(esem.values()) + [h for l in dsem.values() for h in l]

        def _cleanup():
            nc.clear_and_free_semaphores(allsems)
            nc.all_engine_barrier()
        ctx.callback(_cleanup)
        for e in ENGS:
            c = 0
            for op in s.ops[e]:
                if op.signal and not op.dma:
                    c += 1; op.sigval = c
        block = ctx.enter_context(nc.Block())

        def mk(ename):
            def body(e):
                waited = {}

                def wait(sem, val, key):
                    if waited.get(key, 0) < val:
                        e.wait_ge(sem, val); waited[key] = val
                for op in s.ops[ename]:
                    for d in op.deps:
                        if d.dma: wait(dsem[d.eng][d.dsem], d.dval, ('d', d.eng, d.dsem))
                        else: wait(esem[d.eng], d.sigval, ('e', d.eng))
                    if op.dma and op.prev_dma is not None:
                        d = op.prev_dma; wait(dsem[d.eng][d.dsem], d.dval, ('d', d.eng, d.dsem))
                    ins = op.fn(e)
                    if op.dma: ins.then_inc(dsem[ename][op.dsem], 16)
                    elif op.signal: ins.then_inc(esem[ename], 1)
                if ename == 'sync':
                    for q in dsem:
                        for d in s.dma_hist[q][-NDSEM:]:
                            wait(dsem[q][d.dsem], d.dval, ('d', q, d.dsem))
            return body
        for ename in ENGS:
            if s.ops[ename] or ename == 'sync':
                getattr(block, ename)(mk(ename))


class Rec:
    def __init__(s): s.l = []; s.cur = None

    def _put(s, op):
        if s.cur is not None: s.cur.append(op)
        else: s.l.append([op])

    def add(s, *a, **k): s._put(('add', a, k))

    def dma(s, *a, **k): s._put(('dma', a, k))

    def atom(s):
        rec = s

        class _A:
            def __enter__(self_):
                assert rec.cur is None; rec.cur = []

            def __exit__(self_, *a):
                rec.l.append(rec.cur); rec.cur = None
        return _A()


def interleave(P, recs, W):
    active = {}; nxt = 0
    while nxt < len(recs) or active:
        while nxt < len(recs) and (nxt % W) not in active:
            active[nxt % W] = iter(recs[nxt].l); nxt += 1
        for sl in sorted(active, key=lambda q: q):
            unit = next(active[sl], None)
            if unit is None:
                del active[sl]; continue
            for op in unit:
                getattr(P, op[0])(*op[1], **op[2])


class _NoAtom:
    def __enter__(s): pass

    def __exit__(s, *a): pass


def atom_of(R):
    return R.atom() if isinstance(R, Rec) else _NoAtom()


class K:
    def __init__(s):
        s.nc = bass.Bass("TRN2", target_bir_lowering=False)
        s.dr = {}

    def din(s, name, shape, dt=F32):
        s.dr[name] = s.nc.dram_tensor(name, list(shape), dt, kind="ExternalInput").ap(); return s.dr[name]

    def dout(s, name, shape, dt=F32):
        s.dr[name] = s.nc.dram_tensor(name, list(shape), dt, kind="ExternalOutput").ap(); return s.dr[name]

    def dint(s, name, shape, dt=F32):
        s.dr[name] = s.nc.dram_tensor(name, list(shape), dt, kind="Internal").ap(); return s.dr[name]


def stage0(k):
    nc = k.nc; dr = k.dr
    with ExitStack() as ctx:
        T = lambda n, sh, dt=F32: ctx.enter_context(nc.sbuf_tensor("s0_" + n, sh, dt))
        cT = T("cT", [128, 8, 2]); scT = T("scT", [128, 8, 2])
        awt = [T(f"aw{i}", [128, 8, 512]) for i in range(2)]
        adab = T("adab", [2, 2, 6144]); modsb = T("mod", [2, 2, 6144])
        pm = [ctx.enter_context(nc.psum_tensor(f"s0_pm{i}", [128, 512], F32)) for i in range(2)]
        P = Prog(nc)
        for v in range(2):
            P.dma('sync', cT[:, :, v], dr['cvec'][v, :].rearrange("(c p) -> p c", p=128), w=['cT'], allow_slow_non_contiguous=True)
        for i in range(2):
            P.dma('gpsimd', adab[:, i, :], dr['ada_b'][i, :].partition_broadcast(2), w=[('adab', i)])
        P.add('scalar', lambda e: e.activation(out=scT[:], in_=cT[:], func=AF.Silu), r=['cT'], w=['scT'])
        n = 0
        for i in range(2):
            for cb in range(12):
                sl = n % 2; n += 1
                P.dma('sync', awt[sl][:], dr['ada_w'][i, :, cb * 512:(cb + 1) * 512].rearrange("(c p) n -> p c n", p=128),
                      w=[('aw', sl)])
                for kc in range(8):
                    P.add('tensor', lambda e, sl=sl, kc=kc: e.matmul(pm[sl][0:2, :], lhsT=scT[:, kc, :], rhs=awt[sl][:, kc, :],
                                                                     start=(kc == 0), stop=(kc == 7)),
                          r=['scT', ('aw', sl)], w=[('pm', sl)])
                P.add('vector', lambda e, sl=sl, i=i, cb=cb: e.tensor_tensor(out=modsb[:, i, cb * 512:(cb + 1) * 512], in0=pm[sl][0:2, :],
                                                                            in1=adab[:, i, cb * 512:(cb + 1) * 512], op=ALU.add),
                      r=[('pm', sl), ('adab', i)], w=['modsb'])
        P.dma('sync', dr['modD'].rearrange("i v n -> v i n"), modsb[:], r=['modsb'], w=['modD'])
        P.emit(ctx)


def load_AB(k, ctx, P, layer, pref):
    nc = k.nc; dr = k.dr
    T = lambda n, sh, dt=F32: ctx.enter_context(nc.sbuf_tensor(pref + n, sh, dt))
    modF = T("modF", [128, 2, 48]); gF = T("gF", [128, 2, 8]); AB = T("AB", [128, 2, 2, 2, 8])
    for v in range(2):
        P.dma('sync', modF[:, v, :], dr['modD'][layer, v, :].rearrange("(kc p) -> p kc", p=128), w=[('modF', v)],
              allow_slow_non_contiguous=True)
    for sub in range(2):
        P.dma('sync', gF[:, sub, :], dr['norm_g'][layer, sub, :].rearrange("(c p) -> p c", p=128), w=['gF'], allow_slow_non_contiguous=True)
    for v in range(2):
        for sub in range(2):
            sh = modF[:, v, (3 * sub) * 8:(3 * sub) * 8 + 8]; sc = modF[:, v, (3 * sub + 1) * 8:(3 * sub + 1) * 8 + 8]
            P.add('vector', lambda e, v=v, sub=sub, sc=sc: e.scalar_tensor_tensor(out=AB[:, v, sub, 0, :], in0=sc, scalar=1.0, in1=gF[:, sub, :],
                                                                              op0=ALU.add, op1=ALU.mult),
                  r=[('modF', v), 'gF'], w=['AB'])
            P.add('vector', lambda e, v=v, sub=sub, sh=sh: e.tensor_copy(out=AB[:, v, sub, 1, :], in_=sh), r=[('modF', v)], w=['AB'])
    return AB


def consts(k, ctx, P, pref):
    nc = k.nc
    ident = ctx.enter_context(nc.sbuf_tensor(pref + "ident", [128, 128], BF16))
    eps_t = ctx.enter_context(nc.sbuf_tensor(pref + "eps", [128, 1], F32))
    P.add('gpsimd', lambda e: e.memset(ident[:], 0.0), w=['ident'])
    P.add('gpsimd', lambda e: e.affine_select(out=ident[:], in_=ident[:], pattern=[[-1, 128]], compare_op=ALU.not_equal, fill=1.0,
                                              base=0, channel_multiplier=1), w=['ident'])
    P.add('gpsimd', lambda e: e.memset(eps_t[:], 1e-6), w=['eps'])
    return ident, eps_t


def norm_ops(P, xt_chunks, xkey, ssq, rstd, xn_chunks, eps_t, s):
    n = len(xt_chunks)
    for j, (xc, xo) in enumerate(zip(xt_chunks, xn_chunks)):
        np_ = xc.shape[0]
        P.add('scalar', lambda e, xc=xc, xo=xo, j=j, np_=np_: e.activation(out=xo, in_=xc, func=AF.Square, accum_out=ssq[0:np_, j:j + 1]),
              r=[xkey], w=[('xn', s), ('ssq', s)])
    P.add('scalar', lambda e: e.activation(out=rstd[:, 0:n], in_=ssq[:, 0:n], func=AF.Sqrt, scale=1.0 / D, bias=eps_t[:, 0:1]),
          r=[('ssq', s), 'eps'], w=[('rstd', s)])
    P.add('vector', lambda e: e.reciprocal(out=rstd[:, 0:n], in_=rstd[:, 0:n]), r=[('rstd', s)], w=[('rstd', s)])
    for j, (xc, xo) in enumerate(zip(xt_chunks, xn_chunks)):
        np_ = xc.shape[0]
        P.add('gpsimd', lambda e, xc=xc, xo=xo, j=j, np_=np_: e.tensor_scalar(out=xo, in0=xc, scalar1=rstd[0:np_, j:j + 1], scalar2=None, op0=ALU.mult),
              r=[xkey, ('rstd', s)], w=[('xn', s)])


def emit_norm_T(P, xcs, xkey, ssq, rstd, xos, eps_t, ident, pT, A, B, hx, nk):
    n = len(xcs)
    for j in range(n):
        P.add('scalar', lambda e, j=j: e.activation(out=xos[j], in_=xcs[j], func=AF.Square, accum_out=ssq[:, j:j + 1]),
              r=[xkey], w=[nk + ('xn',), nk + ('ssq',)])
    P.add('scalar', lambda e: e.activation(out=rstd[:, 0:n], in_=ssq[:, 0:n], func=AF.Sqrt, scale=1.0 / D, bias=eps_t[:, 0:1]),
          r=[nk + ('ssq',), 'eps'], w=[nk + ('rstd',)])
    P.add('vector', lambda e: e.reciprocal(out=rstd[:, 0:n], in_=rstd[:, 0:n]), r=[nk + ('rstd',)], w=[nk + ('rstd',)])
    for j in range(n):
        P.add('scalar', lambda e, j=j: e.activation(out=xos[j], in_=xcs[j], func=AF.Identity, scale=rstd[:, j:j + 1]),
              r=[xkey, nk + ('rstd',)], w=[nk + ('xn',)])
    for pr in range(4):
        bk = pr % 2
        with atom_of(P):
            for fc in (2 * pr, 2 * pr + 1):
                q = fc % 4
                for j in range(n):
                    P.add('tensor', lambda e, q=q, j=j, fc=fc: e.transpose(out=pT[:, q, j * 128:(j + 1) * 128], in_=xos[j][:, fc * 128:(fc + 1) * 128],
                                                                           identity=ident[:]),
                          r=[nk + ('xn',), 'ident'], w=[('pT', bk)])
            for fc in (2 * pr, 2 * pr + 1):
                q = fc % 4
                P.add('scalar', lambda e, fc=fc, q=q: e.activation(out=hx[:, fc, 0:128 * n], in_=pT[:, q, 0:128 * n], func=AF.Identity,
                                                                    scale=A[:, fc:fc + 1], bias=B[:, fc:fc + 1]),
                      r=[('pT', bk), 'AB'], w=[nk + ('hx',)])

def stage1a(k, upto=99, ntiles=None, NSEG=1, W=int(os.environ.get('W1A', '4'))):
    nc = k.nc; dr = k.dr
    with ExitStack() as ctx:
        T = lambda n, sh, dt=F32: ctx.enter_context(nc.sbuf_tensor("a_" + n, sh, dt))
        P = Prog(nc)
        ident, eps_t = consts(k, ctx, P, "a_")
        AB = load_AB(k, ctx, P, 0, "a_")
        gp_bc = [T(f"gpbc{v}", [128, 1024]) for v in range(2)]; ps_bc = T("psbc", [128, 1024])
        P.dma('sync', ps_bc[:], dr['pool_scale'][0, :].partition_broadcast(128), w=['psbc'])
        for v in range(2):
            P.dma('sync', gp_bc[v][:], dr['modD'][0, v, 2048:3072].partition_broadcast(128), w=[('gpbc', v)])
            P.add('gpsimd', lambda e, v=v: e.tensor_tensor(out=gp_bc[v][:], in0=gp_bc[v][:], in1=ps_bc[:], op=ALU.mult),
                  r=['psbc'], w=[('gpbc', v)])
        inv = T("inv", [128, NSEG + 1, 2, 4, 2, 8]); msk = T("msk", [128, 2 * (NSEG + 1)])
        for sg in range(NSEG + 1):
            P.dma('sync', inv[:, sg].rearrange("p b c d e -> p (b c d e)"), dr['inv'][sg, :].partition_broadcast(128), w=['inv'])
        P.dma('sync', msk[:], dr['msk'][0, :].partition_broadcast(128), w=['msk'])
        wg = T("wg", [128, 4, 2, 256], BF16)
        P.dma('gpsimd', wg[:], dr['pool_w'].rearrange("g (c p) e -> p g c e", p=128), w=['wg'])
        xt = [T(f"xt{s}", [128, 2, 1024]) for s in range(W)]; xh = [T(f"xh{s}", [128, 1024]) for s in range(W)]
        ssq = [T(f"ssq{s}", [128, 3]) for s in range(W)]; rstd = [T(f"rstd{s}", [128, 3]) for s in range(W)]
        xn = [T(f"xn{s}", [128, 3, 1024], BF16) for s in range(W)]
        hxT = [T(f"hxT{s}", [128, 8, 272], BF16) for s in range(W)]
        SA = [T(f"SA{s}", [128, 2, 272]) for s in range(W)]; SB = [T(f"SB{s}", [128, 2, 272]) for s in range(W)]
        te = [T(f"te{s}", [128, 2, 8]) for s in range(W)]
        dT = [T(f"dT{s}", [128, 8, 256], BF16) for s in range(W)]
        tmp = [T(f"tmp{s}", [128, 1024]) for s in range(2)]
        ssq2 = [T(f"ssq2{s}", [128, 2]) for s in range(W)]; rstd2 = [T(f"rstd2{s}", [128, 2]) for s in range(W)]
        xn2 = [T(f"xn2{s}", [128, 2, 1024], BF16) for s in range(W)]; hx2 = [T(f"hx2{s}", [128, 8, 256], BF16) for s in range(W)]
        pT = ctx.enter_context(nc.psum_tensor("a_pT", [128, 4, 512], BF16))
        pD = [ctx.enter_context(nc.psum_tensor(f"a_pD{s}", [128, 1024], F32)) for s in range(2)]
        for s in range(W):
            P.add('gpsimd', lambda e, s=s: e.memset(ssq[s][:], 1.0), w=[('ssq', s)])
            P.add('gpsimd', lambda e, s=s: e.memset(xh[s][:], 0.0), w=[('xh', s)])
            P.add('gpsimd', lambda e, s=s: e.memset(xn[s][:, 2, :], 0.0), w=[('xn', s)])
        tiles = [(0, sg, t) for sg in range(NSEG) for t in range(NT)] + [(1, NSEG, 0)]
        if ntiles: tiles = tiles[:ntiles]
        recs = []
        for ti, (v, sg, t) in enumerate(tiles):
            s = ti % W
            R = Rec(); recs.append(R)
            src = dr['xin'][sg] if v == 0 else dr['ctxin']; dst = dr['xaD'][sg * 4096:(sg + 1) * 4096, :] if v == 0 else dr['caD']
            r0 = 256 * t
            R.dma('sync', xt[s][:], src[r0 + 8:r0 + 264, :].rearrange("(j p) d -> p j d", p=128), w=[('xt', s)])
            R.dma('sync', xh[s][0:8, :], src[r0:r0 + 8, :], w=[('xh', s)])
            R.dma('sync', xh[s][32:40, :], src[r0 + 264:r0 + 272, :], w=[('xh', s)])
            xcs = [xt[s][:, 0, :], xt[s][:, 1, :], xh[s][:, :]]; xos = [xn[s][:, 0, :], xn[s][:, 1, :], xn[s][:, 2, :]]
            for j in range(3):
                np_ = xcs[j].shape[0]
                R.add('scalar', lambda e, j=j, np_=np_, s=s, xcs=xcs, xos=xos: e.activation(out=xos[j], in_=xcs[j], func=AF.Square,
                                                                                      accum_out=ssq[s][0:np_, j:j + 1]),
                      r=[('xt', s), ('xh', s)], w=[('xn', s), ('ssq', s)])
            R.add('scalar', lambda e, s=s: e.activation(out=rstd[s][:], in_=ssq[s][:], func=AF.Sqrt, scale=1.0 / D, bias=eps_t[:, 0:1]),
                  r=[('ssq', s), 'eps'], w=[('rstd', s)])
            R.add('vector', lambda e, s=s: e.reciprocal(out=rstd[s][:], in_=rstd[s][:]), r=[('rstd', s)], w=[('rstd', s)])
            for j in range(3):
                np_ = xcs[j].shape[0]
                R.add('scalar', lambda e, j=j, np_=np_, s=s, xcs=xcs, xos=xos: e.activation(out=xos[j], in_=xcs[j], func=AF.Identity,
                                                                                      scale=rstd[s][0:np_, j:j + 1]),
                      r=[('xt', s), ('xh', s), ('rstd', s)], w=[('xn', s)])
            if upto < 1: continue
            for pr in range(4):
              bk = pr % 2
              with R.atom():
                for fc in (2 * pr, 2 * pr + 1):
                    q = fc % 4
                    for jj in range(3):
                        R.add('tensor', lambda e, s=s, q=q, jj=jj, fc=fc: e.transpose(out=pT[:, q, jj * 128:(jj + 1) * 128],
                                                                                 in_=xn[s][:, jj, fc * 128:(fc + 1) * 128], identity=ident[:]),
                              r=[('xn', s), 'ident'], w=[('pT', bk)])
                for fc in (2 * pr, 2 * pr + 1):
                    q = fc % 4
                    A_ = AB[:, v, 0, 0, fc:fc + 1]; B_ = AB[:, v, 0, 1, fc:fc + 1]
                    R.add('scalar', lambda e, s=s, fc=fc, q=q, A_=A_, B_=B_: e.activation(out=hxT[s][:, fc, 8:264], in_=pT[:, q, 0:256], func=AF.Identity,
                                                                                      scale=A_, bias=B_),
                          r=[('pT', bk), 'AB'], w=[('hxT', s)])
                    for (o0, i0) in ((0, 256), (264, 288)):
                        R.add('scalar', lambda e, s=s, fc=fc, q=q, A_=A_, B_=B_, o0=o0, i0=i0: e.activation(out=hxT[s][:, fc, o0:o0 + 8], in_=pT[:, q, i0:i0 + 8],
                                                                                                  func=AF.Identity, scale=A_, bias=B_),
                              r=[('pT', bk), 'AB'], w=[('hxT', s)])
            if upto < 2: continue
            first = (t == 0); last = (v == 1) or (t == NT - 1)
            if first:
                R.add('vector', lambda e, s=s, sg=sg: e.tensor_scalar(out=hxT[s][:, :, 0:8], in0=hxT[s][:, :, 0:8], scalar1=msk[:, 2 * sg:2 * sg + 1],
                                                                     scalar2=None, op0=ALU.mult), r=['msk'], w=[('hxT', s)])
            if last:
                R.add('vector', lambda e, s=s, sg=sg: e.tensor_scalar(out=hxT[s][:, :, 264:272], in0=hxT[s][:, :, 264:272], scalar1=msk[:, 2 * sg + 1:2 * sg + 2],
                                                                     scalar2=None, op0=ALU.mult), r=['msk'], w=[('hxT', s)])
            for g in range(4):
                h = hxT[s][:, 2 * g:2 * g + 2, :]
                bufs = [SA[s], SB[s]]
                R.add('vector', lambda e, s=s, h=h: e.tensor_tensor(out=SA[s][:, :, 1:272], in0=h[:, :, 0:271], in1=h[:, :, 1:272], op=ALU.add),
                      r=[('hxT', s)], w=[('S', s)])
                cur = 0
                steps = [(1, 2, 271), (2, 4, 269), (4, 8, 265)]
                for (sh_, lo, hi) in steps[:g]:
                    a = bufs[cur]; b = bufs[1 - cur]
                    R.add('vector', lambda e, a=a, b=b, sh_=sh_, lo=lo, hi=hi: e.tensor_tensor(out=b[:, :, lo:hi], in0=a[:, :, lo - sh_:hi - sh_],
                                                                                         in1=a[:, :, lo + sh_:hi + sh_], op=ALU.add),
                          r=[('S', s)], w=[('S', s)])
                    cur = 1 - cur
                Sf = bufs[cur]; wv = 1.0 / (2 << g)
                R.add('vector', lambda e, s=s, g=g, Sf=Sf, h=h, wv=wv: e.scalar_tensor_tensor(out=dT[s][:, 2 * g:2 * g + 2, :], in0=Sf[:, :, 8:264], scalar=wv,
                                                                                         in1=h[:, :, 8:264], op0=ALU.mult, op1=ALU.subtract),
                      r=[('S', s), ('hxT', s)], w=[('dT', s)])
                for (flag, eidx, c0) in ((first, 0, 0), (last, 1, 248)):
                    if not flag: continue
                    R.add('vector', lambda e, s=s, g=g, Sf=Sf, sg=sg, eidx=eidx, c0=c0: e.tensor_tensor(out=te[s][:], in0=Sf[:, :, 8 + c0:16 + c0],
                                                                                                in1=inv[:, sg, eidx, g, :, :], op=ALU.mult),
                          r=[('S', s), 'inv'], w=[('te', s)])
                    R.add('vector', lambda e, s=s, g=g, h=h, c0=c0: e.tensor_tensor(out=dT[s][:, 2 * g:2 * g + 2, c0:c0 + 8], in0=te[s][:],
                                                                                in1=h[:, :, 8 + c0:16 + c0], op=ALU.subtract),
                          r=[('te', s), ('hxT', s)], w=[('dT', s)])
            if upto < 3: continue
            for j in range(2):
              with R.atom():
                for g in range(4):
                    for c in range(2):
                        R.add('tensor', lambda e, s=s, j=j, g=g, c=c: e.matmul(pD[j][:, g * 256:(g + 1) * 256], lhsT=dT[s][:, 2 * g + c, j * 128:(j + 1) * 128],
                                                                              rhs=wg[:, g, c, :], start=(c == 0), stop=(c == 1)),
                              r=[('dT', s), 'wg'], w=[('pD', j)])
                R.add('vector', lambda e, s=s, j=j, v=v: e.tensor_tensor(out=tmp[j][:], in0=pD[j][:], in1=gp_bc[v][:], op=ALU.mult),
                      r=[('pD', j), ('gpbc', v)], w=[('tmp', j)])
                R.add('vector', lambda e, s=s, j=j: e.tensor_tensor(out=xt[s][:, j, :], in0=xt[s][:, j, :], in1=tmp[j][:], op=ALU.add),
                      r=[('tmp', j)], w=[('xt', s)])
            R.dma('sync', dst[r0:r0 + 256, :].rearrange("(j p) d -> p j d", p=128), xt[s][:], r=[('xt', s)], w=['xaD'])
            hdst = dr['hxD'][sg * NT + t] if v == 0 else dr['hcD'][0]
            emit_norm_T(R, [xt[s][:, 0, :], xt[s][:, 1, :]], ('xt', s), ssq2[s], rstd2[s], [xn2[s][:, 0, :], xn2[s][:, 1, :]], eps_t, ident, pT,
                        AB[:, v, 1, 0, :], AB[:, v, 1, 1, :], hx2[s], ('n2', s))
            R.dma('sync', hdst, hx2[s][:].rearrange("p c t -> p (c t)"), r=[('n2', s, 'hx')], w=['hxD'])
        interleave(P, recs, W)
        P.emit(ctx)


def ffn_pass(k, pref, units, tiles, g2_specs, gatesD=None):
    nc = k.nc; dr = k.dr; HF = 11; HW = 1408
    with ExitStack() as ctx:
        T = lambda n, sh, dt=F32: ctx.enter_context(nc.sbuf_tensor(pref + n, sh, dt))
        P = Prog(nc)
        w1b = [T(f"w1b{i}", [128, 8, HW], BF16) for i in range(2)]; w3b = [T(f"w3b{i}", [128, 8, HW], BF16) for i in range(2)]
        w2b = [T(f"w2b{i}", [128, HF, 1024], BF16) for i in range(2)]
        g2bc = {v: T(f"g2bc{v}", [128, 1024]) for v in g2_specs}
        for v, src in g2_specs.items():
            P.dma('sync', g2bc[v][:], src.partition_broadcast(128), w=[('g2bc', v)])
        gates = None
        if gatesD is not None:
            gates = T("gates", [128, 32, 8])
            P.dma('sync', gates[:], gatesD.rearrange("(c p) e -> p c e", p=128), w=['gates'])
        hx = [T(f"hx{i}", [128, 8, 256], BF16) for i in range(3)]
        gT = T("gT", [128, HF, 256], BF16); sil = [T(f"sil{i}", [128, 256], BF16) for i in range(2)]
        tmp = [T(f"tmp{i}", [128, 512]) for i in range(4)]
        ph = [ctx.enter_context(nc.psum_tensor(f"{pref}ph{i}", [128, 2, 256], F32)) for i in range(2)]
        pD = [ctx.enter_context(nc.psum_tensor(f"{pref}pD{i}", [128, 512], F32)) for i in range(2)]
        nh = 0; nd = 0
        def loadw(u):
            (w1, w3, w2, ex) = units[u]; b = u % 2
            for dc in range(8):
                P.dma('gpsimd', w1b[b][:, dc, :], w1[dc * 128:(dc + 1) * 128, :], w=[('w1', b, dc)])
                P.dma('gpsimd', w3b[b][:, dc, :], w3[dc * 128:(dc + 1) * 128, :], w=[('w3', b, dc)])
            for f in range(HF):
                P.dma('gpsimd', w2b[b][:, f, :], w2[f * 128:(f + 1) * 128, :], w=[('w2', b, f)])
        loadw(0)
        for u, (w1, w3, w2, ex) in enumerate(units):
            b = u % 2
            if u + 1 < len(units): loadw(u + 1)
            for (hsrc, odst, v, ch0) in tiles:
                hs = nh % 3; nh += 1
                P.dma('sync', hx[hs][:].rearrange("p c t -> p (c t)"), hsrc, w=[('hx', hs)])
                for f in range(HF):
                    pb = f % 2
                    for (wb, wk, col) in ((w1b, 'w1', 0), (w3b, 'w3', 1)):
                        for dc in range(8):
                            P.add('tensor', lambda e, wb=wb, b=b, dc=dc, f=f, pb=pb, col=col, hs=hs: e.matmul(
                                ph[pb][:, col, :], lhsT=wb[b][:, dc, f * 128:(f + 1) * 128], rhs=hx[hs][:, dc, :], start=(dc == 0), stop=(dc == 7)),
                                r=[(wk, b, dc), ('hx', hs)], w=[('ph', pb)])
                    P.add('scalar', lambda e, pb=pb: e.activation(out=sil[pb][:], in_=ph[pb][:, 0, :], func=AF.Silu), r=[('ph', pb)], w=[('sil', pb)])
                    P.add('vector', lambda e, pb=pb, f=f: e.tensor_tensor(out=gT[:, f, :], in0=sil[pb][:], in1=ph[pb][:, 1, :], op=ALU.mult),
                          r=[('sil', pb), ('ph', pb)], w=[('gT', f)])
                for j in range(2):
                    for half in range(2):
                        db = nd % 2; ts = nd % 4; nd += 1
                        for f in range(HF):
                            P.add('tensor', lambda e, db=db, f=f, j=j, half=half, b=b: e.matmul(
                                pD[db][:], lhsT=gT[:, f, j * 128:(j + 1) * 128], rhs=w2b[b][:, f, half * 512:(half + 1) * 512], start=(f == 0), stop=(f == HF - 1)),
                                r=[('gT', f), ('w2', b, f)], w=[('pD', db)])
                        if gates is None:
                            P.add('vector', lambda e, db=db, ts=ts, half=half, v=v: e.tensor_tensor(out=tmp[ts][:], in0=pD[db][:],
                                                                                           in1=g2bc[v][:, half * 512:(half + 1) * 512], op=ALU.mult),
                                  r=[('pD', db), ('g2bc', v)], w=[('tmp', ts)])
                        else:
                            P.add('vector', lambda e, db=db, ts=ts, half=half, v=v, c=ch0 + j, ex=ex: e.scalar_tensor_tensor(
                                out=tmp[ts][:], in0=pD[db][:], scalar=gates[:, c, ex:ex + 1], in1=g2bc[v][:, half * 512:(half + 1) * 512],
                                op0=ALU.mult, op1=ALU.mult), r=[('pD', db), ('g2bc', v), 'gates'], w=[('tmp', ts)])
                        P.dma('gpsimd', odst[j * 128:(j + 1) * 128, half * 512:(half + 1) * 512], tmp[ts][:], r=[('tmp', ts)],
                              w=[('out', odst.name, ch0 + j, half)], accum_op=ALU.add)
        P.emit(ctx)


def stage1b(k, NSEG=1):
    dr = k.dr
    units = [(dr['ffn_w1'][:, h * 1408:(h + 1) * 1408], dr['ffn_w3'][:, h * 1408:(h + 1) * 1408], dr['ffn_w2'][h * 1408:(h + 1) * 1408, :], 0)
             for h in range(2)]
    tiles = [(dr['hxD'][t], dr['xaD'][256 * t:256 * (t + 1), :], 0, 2 * t) for t in range(NT * NSEG)]
    tiles.append((dr['hcD'][0], dr['caD'][:, :], 1, 2 * NT * NSEG))
    ffn_pass(k, "b_", units, tiles, {0: dr['modD'][0, 0, 5120:6144], 1: dr['modD'][0, 1, 5120:6144]})


def stage3(k, NQB=8, NKC=130, A_LIST=(0, 1)):
    nc = k.nc; dr = k.dr; NK = NKC * 128
    with ExitStack() as ctx:
        T = lambda n, sh, dt=F32: ctx.enter_context(nc.sbuf_tensor("c_" + n, sh, dt))
        P = Prog(nc)
        KT = T("KT", [128, NK], BF16); V = T("V", [128, NKC, 2, 65], BF16)
        wo = T("wo", [64, 16, 1024], BF16); wof = T("wof", [64, 2, 1024]); g1bc = T("g1bc", [64, 1024])
        QT = [T(f"QT{i}", [128, 4, 512], BF16) for i in range(2)]
        PT = [T(f"PT{i}", [128, 1024], BF16) for i in range(3)]
        oS = T("oS", [65, 2, 512]); attnT = T("attnT", [64, 8, 512], BF16)
        ones = T("ones", [65, 64]); tmp = [T(f"tmp{i}", [128, 512]) for i in range(2)]
        ps = [ctx.enter_context(nc.psum_tensor(f"c_ps{i}", [128, 1024], F32)) for i in range(2)]
        po = [ctx.enter_context(nc.psum_tensor(f"c_po{i}", [128, 512], F32)) for i in range(2)]
        pbc = ctx.enter_context(nc.psum_tensor("c_pbc", [128, 512], F32))
        pw = ctx.enter_context(nc.psum_tensor("c_pw", [128, 512], F32))
        P.add('gpsimd', lambda e: e.memset(ones[:], 1.0), w=['ones'])
        P.dma('sync', g1bc[:], dr['modD'][1, 0, 2048:3072].partition_broadcast(64), w=['g1bc'])
        for h in range(16):
            sl = h % 2
            P.dma('sync', wof[:, sl, :], dr['w_o'][h * 64:(h + 1) * 64, :], w=[('wof', sl)])
            P.add('vector', lambda e, h=h, sl=sl: e.tensor_tensor(out=wo[:, h, :], in0=wof[:, sl, :], in1=g1bc[:], op=ALU.mult),
                  r=[('wof', sl), 'g1bc'], w=['wo'])
        nq = 0; nps = 0; npt = 0; ntmp = 0
        for a in A_LIST:
            P.dma('sync', KT[:, :], dr['KTD'][2 * a:2 * a + 2, :, :].rearrange("b d t -> (b d) t"), w=['KT'])
            for c0 in range(0, NKC, 26):
                c1 = min(NKC, c0 + 26)
                P.dma('sync', V[:, c0:c1, :, :], dr['VD'][c0 * 128:c1 * 128, 2 * a:2 * a + 2, :].rearrange("(c p) g e -> p c g e", p=128), w=['V'])
            for qb in range(NQB):
                qs = nq % 2; nq += 1
                P.dma('sync', QT[qs][:], dr['QTD'][a * 8:(a + 1) * 8, :, qb * 512:(qb + 1) * 512].rearrange("(i b) d t -> (b d) i t", b=2), w=[('QT', qs)])
                for i in range(4):
                    def qk(kc, s):
                        for b in range(2):
                            P.add('tensor', lambda e, b=b, kc=kc, s=s, qs=qs, i=i: e.matmul(
                                ps[s][:, b * 512:(b + 1) * 512], lhsT=KT[b * 64:(b + 1) * 64, kc * 128:(kc + 1) * 128],
                                rhs=QT[qs][b * 64:(b + 1) * 64, i, :], start=True, stop=True),
                                r=['KT', ('QT', qs)], w=[('ps', s)])
                    s0 = nps % 2; nps += 1
                    qk(0, s0); cur = s0
                    for kc in range(NKC):
                        t = npt % 3; npt += 1
                        if kc + 1 < NKC:
                            s1 = nps % 2; nps += 1
                            qk(kc + 1, s1)
                        P.add('scalar', lambda e, cur=cur, t=t: e.activation(out=PT[t][:], in_=ps[cur][:], func=AF.Exp, scale=0.125),
                              r=[('ps', cur)], w=[('PT', t)])
                        for b in range(2):
                            P.add('tensor', lambda e, b=b, kc=kc, t=t: e.matmul(
                                po[b][0:65, :], lhsT=V[:, kc, b, :], rhs=PT[t][:, b * 512:(b + 1) * 512], start=(kc == 0), stop=(kc == NKC - 1)),
                                r=['V', ('PT', t)], w=[('po', b)])
                        if kc + 1 < NKC: cur = s1
                    for b in range(2):
                        P.add('vector', lambda e, b=b: e.tensor_copy(out=oS[:, b, :], in_=po[b][0:65, :]), r=[('po', b)], w=[('oS', b)])
                        P.add('vector', lambda e, b=b: e.reciprocal(out=oS[64:65, b, :], in_=oS[64:65, b, :]), r=[('oS', b)], w=[('oS', b)])
                        P.add('tensor', lambda e, b=b: e.matmul(pbc[0:64, :], lhsT=ones[64:65, :], rhs=oS[64:65, b, :], start=True, stop=True),
                              r=[('oS', b), 'ones'], w=['pbc'])
                        P.add('vector', lambda e, b=b, i=i: e.tensor_tensor(out=attnT[:, i * 2 + b, :], in0=oS[0:64, b, :], in1=pbc[0:64, :], op=ALU.mult),
                              r=[('oS', b), 'pbc'], w=[('attnT', i * 2 + b)])
                for j in range(4):
                    for half in range(2):
                        for hh in range(8):
                            i_, b_ = hh // 2, hh % 2; h = a * 8 + b_ * 4 + i_
                            P.add('tensor', lambda e, hh=hh, h=h, j=j, half=half: e.matmul(
                                pw[:, :], lhsT=attnT[:, hh, j * 128:(j + 1) * 128], rhs=wo[:, h, half * 512:(half + 1) * 512], start=(hh == 0), stop=(hh == 7)),
                                r=[('attnT', hh), 'wo'], w=['pw'])
                        ts = ntmp % 2; ntmp += 1
                        P.add('vector', lambda e, ts=ts: e.tensor_copy(out=tmp[ts][:], in_=pw[:, :]), r=['pw'], w=[('tmp', ts)])
                        r0 = qb * 512 + j * 128
                        P.dma('gpsimd', dr['xaD'][r0:r0 + 128, half * 512:(half + 1) * 512], tmp[ts][:], r=[('tmp', ts)],
                              w=[('xo', qb, j, half)], accum_op=ALU.add)
        P.emit(ctx)


def vw(t, off, dims):
    b = t[:]
    return bass.AP(tensor=b.tensor, offset=b.offset + off, ap=[list(b.ap[0])] + [list(d) for d in dims])


def stage2(k, ntiles=None, NSEG=1, W=2):
    nc = k.nc; dr = k.dr
    with ExitStack() as ctx:
        T = lambda n, sh, dt=F32: ctx.enter_context(nc.sbuf_tensor("q_" + n, sh, dt))
        P = Prog(nc)
        ident, eps_t = consts(k, ctx, P, "q_")
        AB = load_AB(k, ctx, P, 1, "q_")
        wq = T("wq", [128, 8, 1536], BF16)
        for dc in range(8):
            for a in range(2):
                for b in range(2):
                    P.dma('gpsimd', vw(wq, dc * 1536 + a * 512 + b * 64, [[128, 4], [1, 64]]),
                          dr['w_qkv'][dc * 128:(dc + 1) * 128, a * 512 + b * 256:a * 512 + (b + 1) * 256].rearrange("p (i d) -> p i d", d=64), w=[('wq', dc)])
            P.dma('gpsimd', wq[:, dc, 1024:1536], dr['w_qkv'][dc * 128:(dc + 1) * 128, 1024:1536], w=[('wq', dc)])
        gbc = T("gbc", [128, 1280])
        P.dma('sync', gbc[:], dr['qkg'][0, :].partition_broadcast(128), w=['gbc'])
        xt = [T(f"xt{s}", [128, 2, 1024]) for s in range(W)]
        ssq = [T(f"ssq{s}", [128, 2]) for s in range(W)]; rstd = [T(f"rstd{s}", [128, 2]) for s in range(W)]
        xn = [T(f"xn{s}", [128, 2, 1024], BF16) for s in range(W)]; hx = [T(f"hx{s}", [128, 8, 256], BF16) for s in range(W)]
        cs = [T(f"cs{s}", [128, 2, 2, 64]) for s in range(W)]
        qk_ = [[T(f"qk{s}{j}", [128, 1536]) for j in range(2)] for s in range(W)]; sq_ = [[T(f"sq{s}{j}", [128, 1280]) for j in range(2)] for s in range(W)]
        rh_ = [[T(f"rh{s}{j}", [128, 20]) for j in range(2)] for s in range(W)]; qn_ = [[T(f"qn{s}{j}", [128, 1280]) for j in range(2)] for s in range(W)]
        t2_ = [[T(f"t2{s}{j}", [128, 1280]) for j in range(2)] for s in range(W)]; qr_ = [[T(f"qr{s}{j}", [128, 1280], BF16) for j in range(2)] for s in range(W)]
        vs = [T(f"vs{s}", [128, 2, 4, 65], BF16) for s in range(W)]
        qT = [T(f"qT{s}", [128, 10, 2, 128], BF16) for s in range(W)]
        pT = ctx.enter_context(nc.psum_tensor("q_pT", [128, 4, 512], BF16))
        pq = [ctx.enter_context(nc.psum_tensor(f"q_pq{i}", [128, 512], F32)) for i in range(3)]
        for s in range(W):
            P.add('gpsimd', lambda e, s=s: e.memset(vs[s][:], 1.0), w=[('vs', s)])
        tiles = [(0, t) for t in range(NT * NSEG)] + [(1, 0)]
        if ntiles: tiles = tiles[:ntiles - 1] + [(1, 0)]
        def tile_body(P, ti, v, t):
            s = ti % W
            qk, sq, rh, qn, t2, qr = qk_[s], sq_[s], rh_[s], qn_[s], t2_[s], qr_[s]
            src = dr['xaD'] if v == 0 else dr['caD']
            r0 = 256 * t; k0 = r0 if v == 0 else 4096 * NSEG
            P.dma('sync', xt[s][:], src[r0:r0 + 256, :].rearrange("(j p) d -> p j d", p=128), w=[('xt', s)])
            P.dma('sync', cs[s][:, :, 0, :], dr['ropeC'][k0:k0 + 256, :].rearrange("(j p) e -> p j e", p=128), w=[('cs', s)])
            P.dma('sync', cs[s][:, :, 1, :], dr['ropeS'][k0:k0 + 256, :].rearrange("(j p) e -> p j e", p=128), w=[('cs', s)])
            emit_norm_T(P, [xt[s][:, 0, :], xt[s][:, 1, :]], ('xt', s), ssq[s], rstd[s], [xn[s][:, 0, :], xn[s][:, 1, :]], eps_t, ident, pT,
                        AB[:, v, 0, 0, :], AB[:, v, 0, 1, :], hx[s], ('n', s))
            full = (v == 0 and t < NT)
            c0 = 0 if full else 1024; h0 = c0 // 64; nh = 20 - h0; W_ = 1280 - c0
            for j in range(2):
                for cb in (range(3) if full else (2,)):
                  with P.atom():
                    for dc in range(8):
                        P.add('tensor', lambda e, s=s, j=j, cb=cb, dc=dc: e.matmul(pq[cb][:], lhsT=hx[s][:, dc, j * 128:(j + 1) * 128],
                                                                                 rhs=wq[:, dc, cb * 512:(cb + 1) * 512], start=(dc == 0), stop=(dc == 7)),
                              r=[('n', s, 'hx'), ('wq', dc)], w=[('pq', cb)])
                    P.add('scalar', lambda e, j=j, cb=cb: e.activation(out=qk[j][:, cb * 512:(cb + 1) * 512], in_=pq[cb][:], func=AF.Identity),
                          r=[('pq', cb)], w=[('qk', s, j)])
                Q = ('qk', s, j)
                hd = lambda ap: ap.rearrange("p (h d) -> p h d", d=64)
                P.add('scalar', lambda e, j=j, c0=c0: e.activation(out=sq[j][:, c0:1280], in_=qk[j][:, c0:1280], func=AF.Square), r=[Q], w=[('sq', s, j)])
                P.add('vector', lambda e, j=j, c0=c0, h0=h0: e.tensor_reduce(out=rh[j][:, h0:20], in_=hd(sq[j][:, c0:1280]), axis=AX.X, op=ALU.add),
                      r=[('sq', s, j)], w=[('rh', s, j)])
                P.add('scalar', lambda e, j=j, h0=h0: e.activation(out=rh[j][:, h0:20], in_=rh[j][:, h0:20], func=AF.Sqrt, scale=1.0 / 64, bias=eps_t[:, 0:1]),
                      r=[('rh', s, j), 'eps'], w=[('rh', s, j)])
                P.add('vector', lambda e, j=j, h0=h0: e.reciprocal(out=rh[j][:, h0:20], in_=rh[j][:, h0:20]), r=[('rh', s, j)], w=[('rh', s, j)])
                P.add('vector', lambda e, j=j, c0=c0, h0=h0, nh=nh: e.tensor_tensor(out=hd(qn[j][:, c0:1280]), in0=hd(qk[j][:, c0:1280]),
                                                                               in1=vw(rh[j], h0, [[1, nh], [0, 64]]), op=ALU.mult), r=[Q, ('rh', s, j)], w=[('qn', s, j)])
                P.add('vector', lambda e, j=j, c0=c0: e.tensor_tensor(out=qn[j][:, c0:1280], in0=qn[j][:, c0:1280], in1=gbc[:, c0:1280], op=ALU.mult),
                      r=['gbc'], w=[('qn', s, j)])
                cofs = j * 128; sofs = j * 128 + 64
                P.add('vector', lambda e, j=j, s=s, sofs=sofs, c0=c0, nh=nh: e.tensor_tensor(out=vw(t2[j], c0, [[64, nh], [32, 2], [1, 16]]),
                                                                                       in0=vw(qn[j], c0 + 16, [[64, nh], [32, 2], [1, 16]]),
                                                                                       in1=vw(cs[s], sofs, [[0, nh], [32, 2], [1, 16]]), op=ALU.mult),
                      r=[('qn', s, j), ('cs', s)], w=[('t2', s, j)])
                P.add('vector', lambda e, j=j, s=s, sofs=sofs, c0=c0, nh=nh: e.tensor_tensor(out=vw(t2[j], c0 + 16, [[64, nh], [32, 2], [1, 16]]),
                                                                                       in0=vw(qn[j], c0, [[64, nh], [32, 2], [1, 16]]),
                                                                                       in1=vw(cs[s], sofs + 16, [[0, nh], [32, 2], [1, 16]]), op=ALU.mult),
                      r=[('qn', s, j), ('cs', s)], w=[('t2', s, j)])
                P.add('vector', lambda e, j=j, s=s, cofs=cofs, c0=c0, nh=nh: e.tensor_tensor(out=hd(qn[j][:, c0:1280]), in0=hd(qn[j][:, c0:1280]),
                                                                                       in1=vw(cs[s], cofs, [[0, nh], [1, 64]]), op=ALU.mult),
                      r=[('cs', s), ('t2', s, j)], w=[('qn', s, j)])
                P.add('vector', lambda e, j=j, c0=c0: e.tensor_tensor(out=qr[j][:, c0:1280], in0=qn[j][:, c0:1280], in1=t2[j][:, c0:1280], op=ALU.add),
                      r=[('t2', s, j)], w=[('qn', s, j), ('qr', s, j)])
                P.add('scalar', lambda e, j=j, s=s: e.activation(out=vs[s][:, j, :, 0:64], in_=qk[j][:, 1280:1536].rearrange("p (g d) -> p g d", d=64), func=AF.Identity),
                      r=[Q], w=[('vs', s)])
                for grp in (range(3) if full else (2,)):
                  with P.atom():
                    rg = (0, 2, 1)[grp]; bk = rg // 2; n = 4 if grp < 2 else 2
                    for u in range(n):
                        sl = grp * 4 + u
                        P.add('tensor', lambda e, j=j, sl=sl, rg=rg, u=u: e.transpose(out=pT[:, rg, u * 128:(u + 1) * 128], in_=qr[j][:, sl * 128:(sl + 1) * 128],
                                                                                identity=ident[:]), r=[('qr', s, j), 'ident'], w=[('pT', bk)])
                    P.add('vector', lambda e, j=j, s=s, grp=grp, rg=rg, n=n: e.tensor_copy(out=qT[s][:, grp * 4:grp * 4 + n, j, :],
                                                                               in_=pT[:, rg, 0:n * 128].rearrange("p (u t) -> p u t", t=128)),
                          r=[('pT', bk)], w=[('qT', s)])
            for j in range(2):
                if v == 0 and t < NT:
                    P.dma('sync', dr['QTD'][:, :, r0 + j * 128:r0 + (j + 1) * 128].rearrange("(hp two) d t -> (two d) hp t", two=2), qT[s][:, 0:8, j, :],
                          r=[('qT', s)], w=['QTD'])
                P.dma('sync', dr['KTD'][:, :, k0 + j * 128:k0 + (j + 1) * 128].rearrange("(a b) d t -> (b d) a t", b=2), qT[s][:, 8:10, j, :],
                      r=[('qT', s)], w=['KTD'])
            P.dma('sync', dr['VD'][k0:k0 + 256, :, :].rearrange("(j p) g e -> p j g e", p=128), vs[s][:], r=[('vs', s)], w=['VD'])
        recs = []
        for ti, (v, t) in enumerate(tiles):
            R = Rec(); recs.append(R); tile_body(R, ti, v, t)
        interleave(P, recs, W)
        P.emit(ctx)


def stage3b(k, ntiles=NT, W=2):
    nc = k.nc; dr = k.dr
    with ExitStack() as ctx:
        T = lambda n, sh, dt=F32: ctx.enter_context(nc.sbuf_tensor("r_" + n, sh, dt))
        P = Prog(nc)
        ident, eps_t = consts(k, ctx, P, "r_")
        AB = load_AB(k, ctx, P, 1, "r_")
        RW = T("RW", [128, 8, 1024]); Abc = T("Abc", [128, 1024]); Bbc = T("Bbc", [128, 1024]); gbc = T("gbc", [128, 1024])
        cbc = T("cbc", [128, 8]); junk = T("junk", [128, 1024])
        P.dma('sync', Abc[:], dr['modD'][1, 0, 4096:5120].partition_broadcast(128), w=['Abc'])
        P.dma('sync', Bbc[:], dr['modD'][1, 0, 3072:4096].partition_broadcast(128), w=['Bbc'])
        P.dma('sync', gbc[:], dr['norm_g'][1, 1, :].partition_broadcast(128), w=['gbc'])
        P.add('vector', lambda e: e.scalar_tensor_tensor(out=Abc[:], in0=Abc[:], scalar=1.0, in1=gbc[:], op0=ALU.add, op1=ALU.mult), r=['gbc'], w=['Abc'])
        P.add('gpsimd', lambda e: e.memset(cbc[:], 0.0), w=['cbc'])
        for ex in range(8):
            P.dma('sync', RW[:, ex, :], dr['rwT'][ex, :].partition_broadcast(128), w=[('RW', ex)])
            P.add('vector', lambda e, ex=ex: e.scalar_tensor_tensor(out=junk[:], in0=Bbc[:], scalar=1.0, in1=RW[:, ex, :], op0=ALU.mult, op1=ALU.mult,
                                                                    accum_out=cbc[:, ex:ex + 1]), r=['Bbc', ('RW', ex), 'cbc'], w=['junk', 'cbc'])
            P.add('vector', lambda e, ex=ex: e.tensor_tensor(out=RW[:, ex, :], in0=RW[:, ex, :], in1=Abc[:], op=ALU.mult), r=['Abc'], w=[('RW', ex)])
        xt = [T(f"xt{s}", [128, 2, 1024]) for s in range(W)]
        ssq = [T(f"ssq{s}", [128, 2]) for s in range(W)]; rstd = [T(f"rstd{s}", [128, 2]) for s in range(W)]
        xn = [T(f"xn{s}", [128, 2, 1024], BF16) for s in range(W)]; hx = [T(f"hx{s}", [128, 8, 256], BF16) for s in range(W)]
        raw = [T(f"raw{s}", [128, 2, 8]) for s in range(W)]
        sm = [{n: T(f"{n}{s}", [128, 2, 8]) for n in ("lg", "lg2", "mk1", "mk2")} for s in range(W)]
        for s in range(W):
            sm[s].update({n: T(f"{n}{s}", [128, 2]) for n in ("m1", "m2", "e2", "g1v", "g2v")}); sm[s]["junk"] = T(f"junkt{s}", [128, 1024])
        gates = T("gates", [128, 32, 8])
        pT = ctx.enter_context(nc.psum_tensor("r_pT", [128, 4, 512], BF16))
        for s in range(W):
            P.add('gpsimd', lambda e, s=s: e.memset(raw[s][:], 0.0), w=[('raw', s)])
        P0 = P

        def tile_body(P, t):
            s = t % W; r0 = 256 * t
            lg, lg2, mk1, mk2, m1, m2, e2, g1v, g2v, junk = [sm[s][n] for n in ('lg', 'lg2', 'mk1', 'mk2', 'm1', 'm2', 'e2', 'g1v', 'g2v', 'junk')]
            K_ = lambda n: (n, s)
            P.dma('sync', xt[s][:], dr['xaD'][r0:r0 + 256, :].rearrange("(j p) d -> p j d", p=128), w=[('xt', s)])
            emit_norm_T(P, [xt[s][:, 0, :], xt[s][:, 1, :]], ('xt', s), ssq[s], rstd[s], [xn[s][:, 0, :], xn[s][:, 1, :]], eps_t, ident, pT,
                        AB[:, 0, 1, 0, :], AB[:, 0, 1, 1, :], hx[s], ('n', s))
            P.dma('sync', dr['hxD'][t], hx[s][:].rearrange("p c t -> p (c t)"), r=[('n', s, 'hx')], w=['hxD'])
            for j in range(2):
                for ex in range(8):
                    P.add('vector', lambda e, s=s, j=j, ex=ex: e.scalar_tensor_tensor(out=junk[:], in0=xt[s][:, j, :], scalar=1.0, in1=RW[:, ex, :],
                                                                                    op0=ALU.mult, op1=ALU.mult, accum_out=raw[s][:, j, ex:ex + 1]),
                          r=[('xt', s), ('RW', ex), ('raw', s)], w=[K_('junk'), ('raw', s)])
            V = 'vector'
            P.add(V, lambda e, s=s: e.tensor_tensor(out=lg[:], in0=raw[s][:], in1=vw(rstd[s], 0, [[1, 2], [0, 8]]), op=ALU.mult), r=[('raw', s), ('n', s, 'rstd')], w=[K_('lg')])
            P.add('gpsimd', lambda e, s=s: e.memset(raw[s][:], 0.0), r=[K_('lg')], w=[('raw', s)])
            P.add(V, lambda e: e.tensor_tensor(out=lg[:], in0=lg[:], in1=vw(cbc, 0, [[0, 2], [1, 8]]), op=ALU.add), r=['cbc'], w=[K_('lg')])
            P.add(V, lambda e: e.tensor_reduce(out=m1[:], in_=lg[:], axis=AX.X, op=ALU.max), r=[K_('lg')], w=[K_('m1')])
            P.add(V, lambda e: e.tensor_tensor(out=mk1[:], in0=lg[:], in1=vw(m1, 0, [[1, 2], [0, 8]]), op=ALU.is_equal), r=[K_('lg'), K_('m1')], w=[K_('mk1')])
            P.add(V, lambda e: e.scalar_tensor_tensor(out=lg2[:], in0=mk1[:], scalar=-1e30, in1=lg[:], op0=ALU.mult, op1=ALU.add), r=[K_('mk1'), K_('lg')], w=[K_('lg2')])
            P.add(V, lambda e: e.tensor_reduce(out=m2[:], in_=lg2[:], axis=AX.X, op=ALU.max), r=[K_('lg2')], w=[K_('m2')])
            P.add(V, lambda e: e.tensor_tensor(out=mk2[:], in0=lg2[:], in1=vw(m2, 0, [[1, 2], [0, 8]]), op=ALU.is_equal), r=[K_('lg2'), K_('m2')], w=[K_('mk2')])
            P.add(V, lambda e: e.tensor_tensor(out=e2[:], in0=m2[:], in1=m1[:], op=ALU.subtract), r=[K_('m1'), K_('m2')], w=[K_('e2')])
            P.add('scalar', lambda e: e.activation(out=e2[:], in_=e2[:], func=AF.Exp), r=[K_('e2')], w=[K_('e2')])
            P.add(V, lambda e: e.tensor_scalar(out=g1v[:], in0=e2[:], scalar1=1.0, scalar2=None, op0=ALU.add), r=[K_('e2')], w=[K_('g1v')])
            P.add(V, lambda e: e.reciprocal(out=g1v[:], in_=g1v[:]), r=[K_('g1v')], w=[K_('g1v')])
            P.add(V, lambda e: e.tensor_tensor(out=g2v[:], in0=e2[:], in1=g1v[:], op=ALU.mult), r=[K_('e2'), K_('g1v')], w=[K_('g2v')])
            P.add(V, lambda e: e.tensor_tensor(out=mk1[:], in0=mk1[:], in1=vw(g1v, 0, [[1, 2], [0, 8]]), op=ALU.mult), r=[K_('g1v')], w=[K_('mk1')])
            P.add(V, lambda e: e.tensor_tensor(out=mk2[:], in0=mk2[:], in1=vw(g2v, 0, [[1, 2], [0, 8]]), op=ALU.mult), r=[K_('g2v')], w=[K_('mk2')])
            P.add(V, lambda e, t=t: e.tensor_tensor(out=gates[:, 2 * t:2 * t + 2, :], in0=mk1[:], in1=mk2[:], op=ALU.add), r=[K_('mk1'), K_('mk2')], w=['gates'])
        recs = []
        for t in range(ntiles):
            R = Rec(); recs.append(R); tile_body(R, t)
        interleave(P0, recs, W)
        P = P0
        P.dma('sync', dr['gatesD'][0:ntiles * 256, :].rearrange("(c p) e -> p c e", p=128), gates[:, 0:2 * ntiles, :], r=['gates'], w=['gatesD'])
        P.emit(ctx)


def stage4(k, nexp=8):
    dr = k.dr
    units = [(dr['moe_w1'][ex][:, h * 1408:(h + 1) * 1408], dr['moe_w3'][ex][:, h * 1408:(h + 1) * 1408], dr['moe_w2'][ex][h * 1408:(h + 1) * 1408, :], ex)
             for ex in range(nexp) for h in range(2)]
    tiles = [(dr['hxD'][t], dr['xaD'][256 * t:256 * (t + 1), :], 0, 2 * t) for t in range(NT)]
    ffn_pass(k, "m_", units, tiles, {0: dr['modD'][1, 0, 5120:6144]}, gatesD=dr['gatesD'])


def stage5(k, ntiles=NT):
    nc = k.nc; dr = k.dr
    with ExitStack() as ctx:
        T = lambda n, sh, dt=F32: ctx.enter_context(nc.sbuf_tensor("f_" + n, sh, dt))
        P = Prog(nc)
        eps_t = T("eps", [128, 1]); fg = T("fg", [128, 1024])
        P.add('gpsimd', lambda e: e.memset(eps_t[:], 1e-6), w=['eps'])
        P.dma('sync', fg[:], dr['final_g'][0, :].partition_broadcast(128), w=['fg'])
        xt = [T(f"xt{s}", [128, 2, 1024]) for s in range(2)]; ot = [T(f"ot{s}", [128, 2, 1024]) for s in range(2)]
        ssq = [T(f"ssq{s}", [128, 2]) for s in range(2)]; rstd = [T(f"rstd{s}", [128, 2]) for s in range(2)]
        for t in range(ntiles):
            s = t % 2; r0 = 256 * t
            P.dma('sync', xt[s][:], dr['xaD'][r0:r0 + 256, :].rearrange("(j p) d -> p j d", p=128), w=[('xt', s)])
            for j in range(2):
                P.add('scalar', lambda e, s=s, j=j: e.activation(out=ot[s][:, j, :], in_=xt[s][:, j, :], func=AF.Square, accum_out=ssq[s][:, j:j + 1]),
                      r=[('xt', s)], w=[('ot', s), ('ssq', s)])
            P.add('scalar', lambda e, s=s: e.activation(out=rstd[s][:], in_=ssq[s][:], func=AF.Sqrt, scale=1.0 / D, bias=eps_t[:, 0:1]),
                  r=[('ssq', s), 'eps'], w=[('rstd', s)])
            P.add('vector', lambda e, s=s: e.reciprocal(out=rstd[s][:], in_=rstd[s][:]), r=[('rstd', s)], w=[('rstd', s)])
            for j in range(2):
                P.add('vector', lambda e, s=s, j=j: e.scalar_tensor_tensor(out=ot[s][:, j, :], in0=xt[s][:, j, :], scalar=rstd[s][:, j:j + 1], in1=fg[:],
                                                                                              op0=ALU.mult, op1=ALU.mult),
                      r=[('xt', s), ('rstd', s), 'fg'], w=[('ot', s)])
            P.dma('sync', dr['out'][r0:r0 + 256, :].rearrange("(j p) d -> p j d", p=128), ot[s][:], r=[('ot', s)], w=['out'])
        P.emit(ctx)


POOLW=(2,4,8,16)
def edge_row(L, realfirst, reallast):
    inv=np.zeros((2,4,2,8),np.float32)
    for g,w in enumerate(POOLW):
        half=w//2
        for i in range(8):
            t=i; cnt=(min(t+half,L)-max(t-half,0)) if realfirst else w
            inv[0,g,:,i]=1.0/cnt
            t=L-8+i; cnt=(min(t+half,L)-max(t-half,0)) if reallast else w
            inv[1,g,:,i]=1.0/cnt
    return inv.reshape(128), [0.0 if realfirst else 1.0, 0.0 if reallast else 1.0]
def seg_quarters(r, NSEG):
    q=r%4
    if NSEG==1: return [q]
    if NSEG==4: return [q]+[o for o in range(4) if o!=q]
    m=r%2; p=(r%4)//2
    return [2*m+p, 2*m+1-p]
def core_inputs(r, inp, NSEG=1):
    b=r//4; qs=seg_quarters(r,NSEG); x=inp['x'][b]
    xin=np.zeros((NSEG,4112,1024),np.float32); inv=np.zeros((NSEG+1,128),np.float32); msk=np.zeros((1,2*(NSEG+1)),np.float32)
    for sg,q in enumerate(qs):
        lo=q*4096-8; hi=q*4096+4096+8; a=max(lo,0); z=min(hi,16384)
        xin[sg,a-lo:a-lo+(z-a)]=x[a:z]
        inv[sg],msk[0,2*sg:2*sg+2]=edge_row(16384,q==0,q==3)
    inv[NSEG],msk[0,2*NSEG:]=edge_row(256,True,True)
    ctxin=np.zeros((272,1024),np.float32); ctxin[8:264]=inp['ctx'][b]
    cvec=np.stack([inp['c'][b], inp['c_ctx']]).astype(np.float32)
    return dict(xin=xin, ctxin=ctxin, cvec=cvec, inv=inv, msk=msk)
def rope_gain(inp):
    return np.concatenate([np.tile(inp['q_norm_g'][0],16), np.tile(inp['k_norm_g'][0],4)]).astype(np.float32)[None,:]
def rope_tables(r, NSEG=1):
    n=NSEG*4096
    C=np.ones((n+256,64),np.float32); S=np.zeros((n+256,64),np.float32)
    fr=(10000.0**(-np.arange(16,dtype=np.float32)/16)).astype(np.float32)
    for sg,q in enumerate(seg_quarters(r,NSEG)):
        t=np.arange(q*4096,(q+1)*4096); row=(t//64).astype(np.float32); col=(t%64).astype(np.float32)
        ar=row[:,None]*fr; ac=col[:,None]*fr
        C[sg*4096:(sg+1)*4096]=np.concatenate([np.cos(ar),np.cos(ar),np.cos(ac),np.cos(ac)],1)
        S[sg*4096:(sg+1)*4096]=np.concatenate([-np.sin(ar),np.sin(ar),-np.sin(ac),np.sin(ac)],1)
    return {'ropeC':C,'ropeS':S}


NSEG = 4


def _build():
    k = K()
    k.din('xin', [NSEG, 4112, 1024]); k.din('ctxin', [272, 1024]); k.din('cvec', [2, 1024]); k.din('inv', [NSEG + 1, 128]); k.din('msk', [1, 2 * (NSEG + 1)])
    k.din('ada_w', [2, 1024, 6144]); k.din('ada_b', [2, 6144]); k.din('norm_g', [2, 2, 1024]); k.din('pool_w', [4, 256, 256]); k.din('pool_scale', [1, 1024])
    k.din('ffn_w1', [1024, 2816]); k.din('ffn_w3', [1024, 2816]); k.din('ffn_w2', [2816, 1024])
    k.din('w_qkv', [1024, 1536]); k.din('qkg', [1, 1280]); k.din('ropeC', [NSEG * 4096 + 256, 64]); k.din('ropeS', [NSEG * 4096 + 256, 64])
    k.din('w_o', [1024, 1024]); k.din('rwT', [8, 1024]); k.din('final_g', [1, 1024])
    k.din('moe_w1', [8, 1024, 2816]); k.din('moe_w3', [8, 1024, 2816]); k.din('moe_w2', [8, 2816, 1024])
    k.dint('modD', [2, 2, 6144]); k.dint('xaD', [NSEG * 4096, 1024]); k.dint('caD', [256, 1024])
    k.dint('hxD', [NSEG * 16, 128, 2048], BF16); k.dint('hcD', [1, 128, 2048], BF16)
    k.dint('QTD', [16, 64, 4096], BF16); k.dint('KTD', [4, 64, NSEG * 4096 + 256], BF16); k.dint('VD', [NSEG * 4096 + 256, 4, 65], BF16)
    k.dint('gatesD', [4096, 8]); k.dout('out', [4096, 1024])
    stage0(k); stage1a(k, NSEG=NSEG); stage1b(k, NSEG=NSEG); stage2(k, NSEG=NSEG)
    stage3(k, NKC=(NSEG * 4096 + 256) // 128); stage3b(k); stage4(k); stage5(k)
    return k.nc


def kernel(x, c, ctx, c_ctx, ada_w, ada_b, norm_g, pool_w, pool_scale, ffn_w1, ffn_w3, ffn_w2,
           w_qkv, w_o, q_norm_g, k_norm_g, router_w, moe_w1, moe_w3, moe_w2, final_g):
    f = lambda a: np.ascontiguousarray(np.asarray(a, dtype=np.float32))
    inp = dict(x=f(x), c=f(c), ctx=f(ctx), c_ctx=f(c_ctx), q_norm_g=f(q_norm_g), k_norm_g=f(k_norm_g))
    shared = dict(ada_w=f(ada_w), ada_b=f(ada_b), norm_g=f(norm_g), pool_w=f(pool_w)[0], pool_scale=f(pool_scale),
                  ffn_w1=f(ffn_w1)[0], ffn_w3=f(ffn_w3)[0], ffn_w2=f(ffn_w2)[0], w_qkv=f(w_qkv)[0], qkg=rope_gain(inp),
                  w_o=f(w_o)[0], rwT=np.ascontiguousarray(f(router_w)[0].T), final_g=f(final_g)[None, :],
                  moe_w1=f(moe_w1)[0], moe_w3=f(moe_w3)[0], moe_w2=f(moe_w2)[0])
    maps = [{**core_inputs(r, inp, NSEG), **rope_tables(r, NSEG), **shared} for r in range(8)]
    res = run_bass_kernel_spmd(_build(), maps, core_ids=list(range(8))).results
    out = np.stack([np.concatenate([np.asarray(res[4 * b + q]['out']) for q in range(4)], axis=0) for b in range(2)])
    return out.astype(np.float32)
```
